# Optimizing a Trainium2 kernel written in Bass

```python
import math
import jax, jax.numpy as jnp
from jax import lax
import numpy as np

D_MODEL = 1024
BATCH = 16
SEQ = 2048
DEPTH = 2

CHUNK = 64
HEAD_DIM = 64
M_HEADS = 4
R_HEADS = 6
W_HEADS = 6
M_WIDTH = M_HEADS * HEAD_DIM
R_WIDTH = R_HEADS * HEAD_DIM
W_WIDTH = W_HEADS * HEAD_DIM
MIX_WIDTH = M_WIDTH + R_WIDTH + W_WIDTH
CONV_WIDTH = 4
W_LORA = 64
A_LORA = 64
G_LORA = 128
D_FF = -(-8 * D_MODEL // (3 * 256)) * 256
M_COLS = 4 * M_WIDTH + 2 * M_HEADS
R_COLS = 4 * R_WIDTH
W_COLS = 3 * W_WIDTH + W_LORA + A_LORA + G_LORA
P_COLS = M_COLS + R_COLS + W_COLS
RMS_EPS = 1e-6
HEAD_LN_EPS = 1e-5
RWKV_LN_EPS = 64e-5
ROPE_BASE = 10000.0
M_INIT = -1e30

kernel_name = "hybrid_mlstm_retention_rwkv7_trunk"


def rmsnorm(x, g):
    xf = x.astype(jnp.float32)
    y = xf * lax.rsqrt(jnp.mean(xf * xf, axis=-1, keepdims=True) + RMS_EPS) * g.astype(jnp.float32)
    return y.astype(x.dtype)


def head_layernorm(x, n_heads, eps):
    B, T, C = x.shape
    xh = x.astype(jnp.float32).reshape(B, T, n_heads, C // n_heads)
    mu = jnp.mean(xh, axis=-1, keepdims=True)
    var = jnp.mean(jnp.square(xh - mu), axis=-1, keepdims=True)
    return ((xh - mu) * lax.rsqrt(var + eps)).reshape(B, T, C)


def causal_depthwise_conv(x, w):
    K, C = w.shape
    return lax.conv_general_dilated(
        x, w.astype(x.dtype)[:, None, :], window_strides=(1,), padding=[(K - 1, 0)],
        dimension_numbers=("NWC", "WIO", "NWC"), feature_group_count=C)


def to_chunks(x, n_heads):
    B, T, C = x.shape
    return x.reshape(B, T // CHUNK, CHUNK, n_heads, C // n_heads).transpose(1, 0, 3, 2, 4)


def from_chunks(y):
    NC, B, H, L, d = y.shape
    return y.transpose(1, 0, 3, 2, 4).reshape(B, NC * L, H * d)


def gate_chunks(g):
    B, T, H = g.shape
    return g.reshape(B, T // CHUNK, CHUNK, H).transpose(1, 0, 3, 2)


def mlstm_chunkwise(q, k, v, i_pre, log_f):
    B, T, _ = q.shape
    H, d = M_HEADS, HEAD_DIM
    causal = jnp.tril(jnp.ones((CHUNK, CHUNK), dtype=bool))

    def step(carry, inp):
        C, n, m = carry
        qc, kc, vc, ic, lfc = inp
        b = jnp.cumsum(lfc, axis=-1)
        dlog = jnp.where(causal, b[..., :, None] - b[..., None, :] + ic[..., None, :], -jnp.inf)
        inter = b + m[..., None]
        m_row = jnp.maximum(inter, jnp.max(dlog, axis=-1))
        s = jnp.einsum('bhjd,bhsd->bhjs', qc, kc) * jnp.exp(dlog - m_row[..., None])
        inter_w = jnp.exp(inter - m_row)
        num = jnp.einsum('bhjs,bhse->bhje', s, vc) + inter_w[..., None] * jnp.einsum('bhjd,bhde->bhje', qc, C)
        den = jnp.sum(s, axis=-1) + inter_w * jnp.einsum('bhjd,bhd->bhj', qc, n)
        h = num / jnp.maximum(jnp.abs(den), jnp.exp(-m_row))[..., None]
        bL = b[..., -1]
        src = bL[..., None] - b + ic
        m_new = jnp.maximum(bL + m, jnp.max(src, axis=-1))
        sw = jnp.exp(src - m_new[..., None])
        dec = jnp.exp(bL + m - m_new)
        C_new = dec[..., None, None] * C + jnp.einsum('bhs,bhsd,bhse->bhde', sw, kc, vc)
        n_new = dec[..., None] * n + jnp.einsum('bhs,bhsd->bhd', sw, kc)
        return (C_new, n_new, m_new), h

    init = (jnp.zeros((B, H, d, d), jnp.float32), jnp.zeros((B, H, d), jnp.float32),
            jnp.full((B, H), M_INIT, jnp.float32))
    xs = (to_chunks(q, H), to_chunks(k, H), to_chunks(v, H), gate_chunks(i_pre), gate_chunks(log_f))
    _, h = lax.scan(step, init, xs)
    return from_chunks(h)


def mlstm_group(p, conv_w, b_i, b_f, ln_g):
    p = p.astype(jnp.float32)
    qk, v, o, gi, gf = jnp.split(p, [2 * M_WIDTH, 3 * M_WIDTH, 4 * M_WIDTH, 4 * M_WIDTH + M_HEADS], axis=-1)
    qk = jax.nn.silu(causal_depthwise_conv(qk, conv_w))
    q, k = jnp.split(qk, 2, axis=-1)
    i_pre = gi + b_i.astype(jnp.float32)
    log_f = jax.nn.log_sigmoid(gf + b_f.astype(jnp.float32))
    h = mlstm_chunkwise(q, k * HEAD_DIM ** -0.5, v, i_pre, log_f)
    h = head_layernorm(h, M_HEADS, HEAD_LN_EPS) * ln_g.astype(jnp.float32)
    return jax.nn.sigmoid(o) * h


def rotary(x, cos, sin):
    x1, x2 = jnp.split(x, 2, axis=-1)
    c, s = cos[None, :, None, :], sin[None, :, None, :]
    return jnp.concatenate([x1 * c - x2 * s, x1 * s + x2 * c], axis=-1)


def retention_group(p):
    p = p.astype(jnp.float32)
    B, T, _ = p.shape
    H, d = R_HEADS, HEAD_DIM
    q, k, v, g = jnp.split(p, 4, axis=-1)
    pos = jnp.arange(T, dtype=jnp.float32)
    theta = 1.0 / (ROPE_BASE ** jnp.linspace(0.0, 1.0, d // 2, dtype=jnp.float32))
    ang = pos[:, None] * theta[None, :]
    cos, sin = jnp.cos(ang), jnp.sin(ang)
    q = rotary(q.reshape(B, T, H, d), cos, sin).reshape(B, T, H * d)
    k = rotary(k.reshape(B, T, H, d), cos, sin).reshape(B, T, H * d) * d ** -0.5
    log_g = jnp.log(1.0 - 2.0 ** (-5.0 - jnp.arange(H, dtype=jnp.float32)))
    idx = jnp.arange(CHUNK, dtype=jnp.float32)
    diff = idx[:, None] - idx[None, :]
    dmat = jnp.where(diff >= 0, jnp.exp(jnp.maximum(diff, 0.0)[None] * log_g[:, None, None]), 0.0)
    q_dec = jnp.exp((idx[None, :] + 1.0) * log_g[:, None])[None, :, :, None]
    k_dec = jnp.exp((CHUNK - 1.0 - idx[None, :]) * log_g[:, None])[None, :, :, None]
    c_dec = jnp.exp(CHUNK * log_g)[None, :, None, None]

    def step(R, inp):
        qc, kc, vc = inp
        s = jnp.einsum('bhjd,bhsd->bhjs', qc, kc) * dmat
        out = jnp.einsum('bhjs,bhse->bhje', s, vc) + jnp.einsum('bhjd,bhde->bhje', qc, R) * q_dec
        R_new = c_dec * R + jnp.einsum('bhsd,bhse->bhde', kc * k_dec, vc)
        return R_new, out

    R0 = jnp.zeros((B, H, d, d), jnp.float32)
    _, y = lax.scan(step, R0, (to_chunks(q, H), to_chunks(k, H), to_chunks(v, H)))
    y = head_layernorm(from_chunks(y), H, HEAD_LN_EPS)
    return jax.nn.silu(g) * y


def rwkv7_group(p, mu, w0, w_up, a0, a_up, g_up, k_k, k_a, r_k, ln_g, ln_b):
    p = p.astype(jnp.float32)
    B, T, _ = p.shape
    H, d = W_HEADS, HEAD_DIM
    p_prev = jnp.pad(p, ((0, 0), (1, 0), (0, 0)))[:, :-1]
    p = p + mu.astype(jnp.float32) * (p_prev - p)
    r, k, v, wl, al, gl = jnp.split(
        p, [W_WIDTH, 2 * W_WIDTH, 3 * W_WIDTH, 3 * W_WIDTH + W_LORA, 3 * W_WIDTH + W_LORA + A_LORA], axis=-1)
    w_log = -jax.nn.softplus(-(w0 + jnp.tanh(wl) @ w_up.astype(jnp.float32))) - 0.5
    decay = jnp.exp(-jnp.exp(w_log))
    a = jax.nn.sigmoid(a0 + al @ a_up.astype(jnp.float32))
    g = jax.nn.sigmoid(gl) @ g_up.astype(jnp.float32)
    kk = (k * k_k).reshape(B, T, H, d)
    kk = kk / jnp.maximum(jnp.sqrt(jnp.sum(kk * kk, axis=-1, keepdims=True)), 1e-12)
    k = k * (1.0 + (a - 1.0) * k_a)

    def heads_t(z):
        return z.reshape(B, T, H, d).transpose(1, 0, 2, 3)

    def step(S, inp):
        r_t, w_t, k_t, v_t, kk_t, a_t = inp
        S = (S * w_t[:, :, None, :]
             - jnp.einsum('bhvk,bhk->bhv', S, kk_t)[..., None] * (kk_t * a_t)[:, :, None, :]
             + v_t[..., :, None] * k_t[..., None, :])
        return S, jnp.einsum('bhvk,bhk->bhv', S, r_t)

    S0 = jnp.zeros((B, H, d, d), jnp.float32)
    xs = (heads_t(r), heads_t(decay), heads_t(k), heads_t(v), kk.transpose(1, 0, 2, 3), heads_t(a))
    _, y = lax.scan(step, S0, xs)
    y = y.transpose(1, 0, 2, 3).reshape(B, T, H * d)
    y = head_layernorm(y, H, RWKV_LN_EPS) * ln_g + ln_b
    bonus = jnp.sum(r.reshape(B, T, H, d) * k.reshape(B, T, H, d) * r_k.reshape(H, d), axis=-1, keepdims=True)
    y = y + (bonus * v.reshape(B, T, H, d)).reshape(B, T, H * d)
    return y * g


def setup_inputs(seed: int = 0) -> dict:
    key = jax.random.key(seed)
    ks = jax.random.split(key, 24)
    f32 = jnp.float32
    nrm = lambda k, shape, s: jax.random.normal(k, shape, f32) * s
    w0_base = jnp.linspace(-5.5, 0.5, W_WIDTH, dtype=f32)
    bf_base = jnp.linspace(3.0, 6.0, M_HEADS, dtype=f32)
    return {
        "x": nrm(ks[0], (BATCH, SEQ, D_MODEL), 1.0),
        "w_in": nrm(ks[1], (DEPTH, D_MODEL, P_COLS), D_MODEL ** -0.5),
        "ln1_g": 1.0 + nrm(ks[2], (DEPTH, D_MODEL), 0.02),
        "ln2_g": 1.0 + nrm(ks[3], (DEPTH, D_MODEL), 0.02),
        "lnf_g": 1.0 + nrm(ks[4], (D_MODEL,), 0.02),
        "m_conv": nrm(ks[5], (DEPTH, CONV_WIDTH, 2 * M_WIDTH), CONV_WIDTH ** -0.5),
        "m_b_i": nrm(ks[6], (DEPTH, M_HEADS), 0.1),
        "m_b_f": bf_base + nrm(ks[7], (DEPTH, M_HEADS), 0.1),
        "m_ln_g": 1.0 + nrm(ks[8], (DEPTH, M_WIDTH), 0.02),
        "rw_mu": jax.random.uniform(ks[9], (DEPTH, W_COLS), f32),
        "rw_w0": w0_base + nrm(ks[10], (DEPTH, W_WIDTH), 0.1),
        "rw_w_up": nrm(ks[11], (DEPTH, W_LORA, W_WIDTH), 0.1),
        "rw_a0": nrm(ks[12], (DEPTH, W_WIDTH), 0.1),
        "rw_a_up": nrm(ks[13], (DEPTH, A_LORA, W_WIDTH), 0.1),
        "rw_g_up": nrm(ks[14], (DEPTH, G_LORA, W_WIDTH), G_LORA ** -0.5),
        "rw_k_k": 0.85 + nrm(ks[15], (DEPTH, W_WIDTH), 0.02),
        "rw_k_a": 1.0 + nrm(ks[16], (DEPTH, W_WIDTH), 0.02),
        "rw_r_k": nrm(ks[17], (DEPTH, W_WIDTH), 0.1),
        "rw_ln_g": 1.0 + nrm(ks[18], (DEPTH, W_WIDTH), 0.02),
        "rw_ln_b": nrm(ks[19], (DEPTH, W_WIDTH), 0.01),
        "w_out": nrm(ks[20], (DEPTH, MIX_WIDTH, D_MODEL), MIX_WIDTH ** -0.5),
        "w_gate": nrm(ks[21], (DEPTH, D_MODEL, D_FF), D_MODEL ** -0.5),
        "w_up": nrm(ks[22], (DEPTH, D_MODEL, D_FF), D_MODEL ** -0.5),
        "w_down": nrm(ks[23], (DEPTH, D_FF, D_MODEL), D_FF ** -0.5),
    }


def reference(x, w_in, ln1_g, ln2_g, lnf_g, m_conv, m_b_i, m_b_f, m_ln_g, rw_mu, rw_w0, rw_w_up,
              rw_a0, rw_a_up, rw_g_up, rw_k_k, rw_k_a, rw_r_k, rw_ln_g, rw_ln_b, w_out, w_gate,
              w_up, w_down):
    for l in range(DEPTH):
        h = rmsnorm(x, ln1_g[l])
        p = h @ w_in[l]
        pm, pr, pw = jnp.split(p, [M_COLS, M_COLS + R_COLS], axis=-1)
        ym = mlstm_group(pm, m_conv[l], m_b_i[l], m_b_f[l], m_ln_g[l])
        yr = retention_group(pr)
        yw = rwkv7_group(pw, rw_mu[l], rw_w0[l], rw_w_up[l], rw_a0[l], rw_a_up[l], rw_g_up[l],
                         rw_k_k[l], rw_k_a[l], rw_r_k[l], rw_ln_g[l], rw_ln_b[l])
        y = jnp.concatenate([ym, yr, yw], axis=-1).astype(x.dtype)
        x = x + y @ w_out[l]
        h = rmsnorm(x, ln2_g[l])
        x = x + (jax.nn.silu(h @ w_gate[l]) * (h @ w_up[l])) @ w_down[l]
    return rmsnorm(x, lnf_g)
```

```python
import math
DBG = 99
VAR = ''
from contextlib import ExitStack
import numpy as np
import concourse.bass as bass
import concourse.mybir as mybir
from concourse.alu_op_type import AluOpType as ALU
from concourse.bass_utils import run_bass_kernel_spmd

F32 = mybir.dt.float32
BF16 = mybir.dt.bfloat16
AF = mybir.ActivationFunctionType
AX = mybir.AxisListType

ENGS = ['pe', 'dve', 'act', 'pool', 'sp']
NDMASEM = 8
HALO = 4
DMASCR = 16384
SAME_ENGINE_SYNC = True
PIPE_OFF = 9
L = 128


class _Rec:
    def __init__(self):
        self.call = None

    def __getattr__(self, name):
        def f(*a, **kw):
            self.call = (name, a, kw)
            return self
        return f


class TS:
    def __init__(self):
        object.__setattr__(self, 'sets', [{}, {}])
        object.__setattr__(self, 'par', 0)

    def add(self, name, t0, t1):
        self.sets[0][name] = t0
        self.sets[1][name] = t1

    def set(self, par):
        object.__setattr__(self, 'par', par)

    def __getattr__(self, name):
        return self.sets[self.par][name]

    def k(self, name):
        return f'{name}@{self.par}'


class KB:
    def __init__(self, nc):
        self.nc = nc
        self.q = {e: [] for e in ENGS}
        self.cnt = {e: 0 for e in ENGS}
        self.lastw = {}
        self.readers = {}
        self.seen = {e: {} for e in ENGS}
        self.dma_i = {e: 0 for e in ENGS}
        self.dma_cnt = {}
        self.out_events = []
        self.stack = ExitStack()
        self.ntile = 0
        self.bank_i = 0

    def sb(self, shape, dt=F32, name=None):
        self.ntile += 1
        return self.stack.enter_context(self.nc.sbuf_tensor(name or f"t{self.ntile}", list(shape), dt))

    def ps(self, shape, dt=F32, name=None):
        self.ntile += 1
        return self.stack.enter_context(self.nc.psum_tensor(name or f"p{self.ntile}", list(shape), dt))

    def _deps(self, reads, writes):
        ev = []
        for k in reads:
            if k in self.lastw:
                ev.append(self.lastw[k])
        for k in writes:
            if k in self.lastw:
                ev.append(self.lastw[k])
            ev.extend(self.readers.get(k, []))
        return ev

    def _filter(self, eng, evs):
        best = {}
        for (s, v) in evs:
            if s[0] == 'eng' and s[1] == eng and (eng == 'pe' or not SAME_ENGINE_SYNC):
                continue
            if self.seen[eng].get(s, 0) >= v:
                continue
            if best.get(s, 0) < v:
                best[s] = v
        for s, v in best.items():
            self.seen[eng][s] = v
        return list(best.items())

    def _record(self, ev, reads, writes):
        for k in writes:
            self.lastw[k] = ev
            self.readers[k] = []
        for k in reads:
            self.readers.setdefault(k, []).append(ev)

    def op(self, eng, fn, reads=(), writes=()):
        waits = self._filter(eng, self._deps(reads, writes))
        self.cnt[eng] += 1
        ev = (('eng', eng), self.cnt[eng])
        rec = _Rec()
        fn(rec)
        call = rec.call
        self.q[eng].append(('op', waits, lambda e, call=call: getattr(e, call[0])(*call[1], **call[2])))
        self._record(ev, reads, writes)
        return ev

    def dma(self, eng, out, in_, reads=(), writes=(), is_output=False):
        i = self.dma_i[eng] % NDMASEM
        self.dma_i[eng] += 1
        s = ('dma', eng, i)
        prev = self.dma_cnt.get(s, 0)
        evs = self._deps(reads, writes)
        if prev:
            evs.append((s, prev * 16))
        waits = self._filter(eng, evs)
        self.dma_cnt[s] = prev + 1
        ev = (s, (prev + 1) * 16)
        self.q[eng].append(('dma', waits, (out, in_), s))
        self._record(ev, reads, writes)
        if is_output:
            self.out_events.append(ev)
        return ev

    def emit(self):
        nc = self.nc
        semh = {}
        with ExitStack() as st:
            for e in ENGS:
                semh[('eng', e)] = st.enter_context(nc.semaphore(f"s_{e}"))
            for s in self.dma_cnt:
                semh[s] = st.enter_context(nc.semaphore(f"d_{s[1]}_{s[2]}"))
            fin = self._filter('sp', self.out_events)
            with nc.Block() as block:
                def run(eng_name, engine):
                    me = semh[('eng', eng_name)]
                    for item in self.q[eng_name]:
                        if item[0] == 'op':
                            _, waits, fn = item
                            for s, v in waits:
                                engine.wait_ge(semh[s], v)
                            fn(engine).then_inc(me, 1)
                        else:
                            _, waits, (out, in_), s = item
                            for ss, v in waits:
                                engine.wait_ge(semh[ss], v)
                            engine.dma_start(out=out, in_=in_).then_inc(semh[s], 16)
                    if eng_name == 'sp':
                        for s, v in fin:
                            engine.wait_ge(semh[s], v)

                @block.tensor
                def _(e):
                    run('pe', e)

                @block.vector
                def _(e):
                    run('dve', e)

                @block.scalar
                def _(e):
                    run('act', e)

                @block.gpsimd
                def _(e):
                    run('pool', e)

                @block.sync
                def _(e):
                    run('sp', e)
        self.stack.close()


class Cfg:
    def __init__(s, D=1024, SEQ=2048, NSEQ=2, BLK=1024, DEPTH=2, MH=4, RH=6, WH=6, DFF=2816,
                 WL=64, AL=64, GL=128):
        s.D, s.SEQ, s.NSEQ, s.BLK, s.DEPTH = D, SEQ, NSEQ, BLK, DEPTH
        s.MH, s.RH, s.WH, s.DFF, s.WL, s.AL, s.GL = MH, RH, WH, DFF, WL, AL, GL
        s.KD = D // 128
        s.NBLK = SEQ // BLK
        s.NCH = BLK // L
        s.TT = min(512, BLK)
        s.NT = BLK // s.TT
        s.MP, s.RP, s.WP = MH // 2, RH // 2, WH // 2
        s.NF = DFF // 128
        s.MW, s.RW, s.WW = MH * 64, RH * 64, WH * 64
        s.MIX = s.MW + s.RW + s.WW
        s.KM = s.MIX // 128
        s.MCOLS = 4 * s.MW + 2 * MH
        s.RCOLS = 4 * s.RW
        s.WCOLS = 3 * s.WW + WL + AL + GL
        g = []
        mb = 0
        for p in range(s.MP):
            for nm, base in (('mq', 0), ('mk', s.MW), ('mv', 2 * s.MW), ('mo', 3 * s.MW)):
                g.append(((nm, p), [mb + base + 128 * p + i for i in range(128)]))
        g.append((('mgi', 0), [mb + 4 * s.MW + i for i in range(MH)]))
        g.append((('mgf', 0), [mb + 4 * s.MW + MH + i for i in range(MH)]))
        rb = s.MCOLS
        swap = [h * 64 + ((j + 32) % 64) for h in range(2) for j in range(64)]
        for p in range(s.RP):
            for nm, base in (('rq', 0), ('rk', s.RW), ('rv', 2 * s.RW), ('rg', 3 * s.RW)):
                g.append(((nm, p), [rb + base + 128 * p + i for i in range(128)]))
                if nm in ('rq', 'rk'):
                    g.append(((nm + 's', p), [rb + base + 128 * p + swap[i] for i in range(128)]))
        wb = s.MCOLS + s.RCOLS
        g.append((('wwl', 0), [wb + 3 * s.WW + i for i in range(WL)]))
        g.append((('wal', 0), [wb + 3 * s.WW + WL + i for i in range(AL)]))
        g.append((('wgl', 0), [wb + 3 * s.WW + WL + AL + i for i in range(GL)]))
        for p in range(s.WP):
            for nm, base in (('wr', 0), ('wk', s.WW), ('wv', 2 * s.WW)):
                g.append(((nm, p), [wb + base + 128 * p + i for i in range(128)]))
        s.groups = g
        s.gidx = {k: i for i, (k, _) in enumerate(g)}
        s.NG = len(g)
        pc = {}
        n = 0

        def add(name, w=1):
            nonlocal n
            pc[name] = n
            n += w
        add('ln1', s.KD)
        add('ln2', s.KD)
        for p in range(s.MP):
            add(('cq', p), 4)
            add(('ck', p), 4)
            add(('mlg', p))
        add('bi')
        add('bf')
        for key, _ in g:
            if key[0][0] == 'w':
                add(('mu', key))
        for p in range(s.WP):
            for nm in ('w0', 'a0', 'kk', 'ka', 'rk', 'lg', 'lb'):
                add((nm, p))
        s.pc = pc
        s.NPAR = n
        cc = {}
        n = 0

        def addc(name, w):
            nonlocal n
            cc[name] = n
            n += w
        addc('ident', 128)
        addc('mincl', 128)
        addc('mstrict', 128)
        addc('mN', 128)
        addc('bones', 128)
        addc('es_r', RH)
        addc('ft_r', RH)
        addc('dec_r', s.RP)
        addc('sel', s.MP * 128)
        addc('lnf', s.KD)
        s.cc = cc
        s.NC = n


def pack_host(cfg, inp):
    c = cfg
    f32 = np.float32
    D, KD = c.D, c.KD
    w_in = np.asarray(inp['w_in'], f32)
    win = np.zeros((c.DEPTH, c.NG, 128, KD, 128), f32)
    for gi, (key, cols) in enumerate(c.groups):
        sub = w_in[:, :, cols]
        sub = sub.reshape(c.DEPTH, KD, 128, len(cols)).transpose(0, 2, 1, 3)
        win[:, gi, :, :, :len(cols)] = sub
    wout = np.asarray(inp['w_out'], f32).reshape(c.DEPTH, c.KM, 128, KD, 128).transpose(0, 3, 2, 1, 4)
    wg = np.asarray(inp['w_gate'], f32).reshape(c.DEPTH, KD, 128, c.NF, 128).transpose(0, 3, 2, 1, 4)
    wu = np.asarray(inp['w_up'], f32).reshape(c.DEPTH, KD, 128, c.NF, 128).transpose(0, 3, 2, 1, 4)
    wgu = np.stack([wg, wu], axis=3)
    wdn = np.asarray(inp['w_down'], f32).reshape(c.DEPTH, c.NF, 128, KD, 128).transpose(0, 3, 2, 1, 4)
    lora = np.zeros((c.DEPTH, 3, 128, c.WW), f32)
    lora[:, 0, :c.WL] = inp['rw_w_up']
    lora[:, 1, :c.AL] = inp['rw_a_up']
    lora[:, 2, :c.GL] = inp['rw_g_up']
    par = np.zeros((c.DEPTH, 128, c.NPAR), f32)
    pc = c.pc
    par[:, :, pc['ln1']:pc['ln1'] + KD] = np.asarray(inp['ln1_g'], f32).reshape(c.DEPTH, KD, 128).transpose(0, 2, 1)
    par[:, :, pc['ln2']:pc['ln2'] + KD] = np.asarray(inp['ln2_g'], f32).reshape(c.DEPTH, KD, 128).transpose(0, 2, 1)
    mconv = np.asarray(inp['m_conv'], f32)
    for p in range(c.MP):
        par[:, :, pc[('cq', p)]:pc[('cq', p)] + 4] = mconv[:, :, 128 * p:128 * p + 128].transpose(0, 2, 1)
        par[:, :, pc[('ck', p)]:pc[('ck', p)] + 4] = mconv[:, :, c.MW + 128 * p:c.MW + 128 * p + 128].transpose(0, 2, 1)
        par[:, :, pc[('mlg', p)]] = np.asarray(inp['m_ln_g'], f32)[:, 128 * p:128 * p + 128]
    par[:, :c.MH, pc['bi']] = inp['m_b_i']
    par[:, :c.MH, pc['bf']] = inp['m_b_f']
    mu = np.asarray(inp['rw_mu'], f32)
    wbase = c.MCOLS + c.RCOLS
    for key, cols in c.groups:
        if key[0][0] == 'w':
            par[:, :len(cols), pc[('mu', key)]] = mu[:, [cc - wbase for cc in cols]]
    for p in range(c.WP):
        sl = slice(128 * p, 128 * p + 128)
        for nm, src in (('w0', 'rw_w0'), ('a0', 'rw_a0'), ('kk', 'rw_k_k'), ('ka', 'rw_k_a'),
                        ('rk', 'rw_r_k'), ('lg', 'rw_ln_g'), ('lb', 'rw_ln_b')):
            par[:, :, pc[(nm, p)]] = np.asarray(inp[src], f32)[:, sl]
    cst = np.zeros((128, c.NC), f32)
    cc = c.cc
    i = np.arange(128)
    cst[:, cc['ident']:cc['ident'] + 128] = np.eye(128)
    cst[:, cc['mincl']:cc['mincl'] + 128] = (i[:, None] <= i[None, :])
    cst[:, cc['mstrict']:cc['mstrict'] + 128] = (i[:, None] < i[None, :])
    cst[:, cc['mN']:cc['mN'] + 128] = (i[None, :] < i[:, None])
    cst[:, cc['bones']:cc['bones'] + 128] = (i[:, None] // 64 == i[None, :] // 64)
    gam = 1.0 - 2.0 ** (-5.0 - np.arange(c.RH, dtype=np.float64))
    cst[:, cc['es_r']:cc['es_r'] + c.RH] = gam[None, :] ** (L - 1.0 - i[:, None])
    cst[:, cc['ft_r']:cc['ft_r'] + c.RH] = gam[None, :] ** (i[:, None] - (L - 1.0))
    for p in range(c.RP):
        cst[:64, cc['dec_r'] + p] = gam[2 * p] ** L
        cst[64:, cc['dec_r'] + p] = gam[2 * p + 1] ** L
    for p in range(c.MP):
        for m in range(128):
            cst[2 * p + m // 64, cc['sel'] + p * 128 + m] = 1.0
    cst[:, cc['lnf']:cc['lnf'] + KD] = np.asarray(inp['lnf_g'], f32).reshape(KD, 128).T
    theta = 1.0 / (10000.0 ** np.linspace(0.0, 1.0, 32, dtype=np.float32))
    ang = np.arange(c.SEQ, dtype=np.float32)[None, :] * theta.astype(np.float32)[:, None]
    cs, sn = np.cos(ang).astype(f32), np.sin(ang).astype(f32)
    rot = np.zeros((2, 128, c.SEQ), f32)
    for h in range(2):
        rot[0, h * 64:h * 64 + 32] = cs
        rot[0, h * 64 + 32:h * 64 + 64] = cs
        rot[1, h * 64:h * 64 + 32] = -sn
        rot[1, h * 64 + 32:h * 64 + 64] = sn
    return dict(win=win, wout=np.ascontiguousarray(wout), wgu=np.ascontiguousarray(wgu),
                wdn=np.ascontiguousarray(wdn), lora=lora, par=par, cst=cst, rot=rot)


def build(cfg):
    c = cfg
    nc = bass.Bass("TRN2", target_bir_lowering=False, dynamic_dma_scratch_size=DMASCR)
    KD, BLK, TT, NT, NCH, NF, KM = c.KD, c.BLK, c.TT, c.NT, c.NCH, c.NF, c.KM
    x_d = nc.dram_tensor("x", [c.NSEQ, c.SEQ, c.D], F32, kind="ExternalInput").ap()
    win_d = nc.dram_tensor("win", [c.DEPTH, c.NG, 128, KD * 128], F32, kind="ExternalInput").ap()
    wout_d = nc.dram_tensor("wout", [c.DEPTH, KD, 128, KM * 128], F32, kind="ExternalInput").ap()
    wgu_d = nc.dram_tensor("wgu", [c.DEPTH, NF, 128, 2 * KD * 128], F32, kind="ExternalInput").ap()
    wdn_d = nc.dram_tensor("wdn", [c.DEPTH, KD, 128, NF * 128], F32, kind="ExternalInput").ap()
    lora_d = nc.dram_tensor("lora", [c.DEPTH, 3, 128, c.WW], F32, kind="ExternalInput").ap()
    par_d = nc.dram_tensor("par", [c.DEPTH, 128, c.NPAR], F32, kind="ExternalInput").ap()
    cst_d = nc.dram_tensor("cst", [128, c.NC], F32, kind="ExternalInput").ap()
    rot_d = nc.dram_tensor("rot", [2, 128, c.SEQ], F32, kind="ExternalInput").ap()
    out_d = nc.dram_tensor("out", [c.NSEQ, c.SEQ, c.D], F32, kind="ExternalOutput").ap()

    k = KB(nc)
    op, dma = k.op, k.dma
    xT = k.sb([128, KD, BLK], F32, "xT")
    hT = k.sb([128, KD, BLK], BF16, "hT")
    yT = k.sb([128, KM, BLK], BF16, "yT")
    NFH = (NF + 1) // 2
    SLABW = max(KD * 128, KM * 128, 2 * KD * 128, NFH * 128)
    NSLAB = 5
    slabs = [k.sb([128, SLABW], BF16, f"slab{i}") for i in range(NSLAB)]
    NFT = 12
    FW = max(HALO + BLK, c.D)
    Ft = [k.sb([128, FW], F32, f"F{i}") for i in range(NFT)]
    Bt = {i: k.sb([128, BLK], BF16, f"B{i}") for i in (0, 3, 4, 9, 10, 11)}
    cst = k.sb([128, c.NC], F32, "cst_sb")
    par = [k.sb([128, c.NPAR], F32, f"par_sb{l}") for l in range(c.DEPTH)]
    lorab = [k.sb([128, 3, c.WW], BF16, f"lora_sb{l}") for l in range(c.DEPTH)]
    identb = k.sb([128, 128], BF16, "identb")
    bonesb = k.sb([128, 128], BF16, "bonesb")
    mask4 = k.sb([128, 4, 128], BF16, "mask4")
    xin = k.sb([128, c.D], F32, "xin")
    TN = min(256, TT)
    rstd2 = [k.sb([128, TN], F32, f"rstd{i}") for i in range(2)]
    tok3 = k.sb([128, 3, 128], BF16, "tok3")
    vaug = k.sb([128, 2, 66], BF16, "vaug")
    ATt = [k.sb([128, 128], BF16, f"AT{h}") for h in range(2)]
    SC2 = k.sb([128, 2, 4, 128], BF16, "SC2")
    Q0t = k.sb([128, 2, 128], BF16, "Q0t")
    PQ = [k.sb([128, 2, 2, 128], BF16, f"PQ{i}") for i in range(2)]
    Tb2 = [k.sb([128, 2, 128], BF16, f"Tb2_{i}") for i in range(2)]
    mN2 = k.sb([128, 2, 128], BF16, "mN2")
    id2 = k.sb([128, 2, 128], BF16, "id2")
    XU = k.sb([128, 2, 2, 64], BF16, "XU")
    hpre = k.sb([128, 2, 64], F32, "hpre")
    ndsb = k.sb([128, 2, 66], F32, "ndsb")
    hn = k.sb([128, 128], BF16, "hn")
    st6 = k.sb([128, 2, 6], F32, "st6")
    mv = k.sb([128, 2, 2], F32, "mv")
    sm = k.sb([128, 8], F32, "sm")
    ytmp = k.sb([128, 128], F32, "ytmp")
    omka = k.sb([128, c.WP], F32, "omka")
    Cf = [[k.sb([128, 65], F32, f"Cf{l}_{p}") for p in range(c.MP)] for l in range(c.DEPTH)]
    Rf = [[k.sb([128, 64], F32, f"Rf{l}_{p}") for p in range(c.RP)] for l in range(c.DEPTH)]
    Mf = [[k.sb([128, 64], F32, f"Mf{l}_{p}") for p in range(c.WP)] for l in range(c.DEPTH)]
    Sz = k.sb([128, 2, 66], BF16, "Sz")
    halo_keys = [key for key, _ in c.groups if key[0] in ('mq', 'mk') or key[0][0] == 'w']
    halo = {(l, key): k.sb([128, HALO], F32, f"halo{l}_{key[0]}{key[1]}") for l in range(c.DEPTH) for key in halo_keys}
    gcar = [k.sb([c.MH, 2], F32, f"gcar{l}") for l in range(c.DEPTH)]
    Rall = k.sb([c.MH, NCH + 1], F32, "Rall")
    decrow = k.sb([c.MH, NCH], F32, "decrow")
    decb = k.sb([128, NCH], F32, "decb")
    est = k.sb([128, NCH, c.MH], F32, "est")
    thrt = k.sb([128, NCH, c.MH], F32, "thrt")
    WLt = k.sb([128, NCH], F32, "WLt")
    S = TS()
    _shapes = dict(tok3=([128, 3, 128], BF16), Sz=([128, 2, 66], BF16), SC2=([128, 2, 4, 128], BF16), Q0t=([128, 2, 128], BF16),
                   XU=([128, 2, 2, 64], BF16), hpre=([128, 2, 64], F32), hn=([128, 128], BF16), st6=([128, 2, 6], F32),
                   mv=([128, 2, 2], F32), sm=([128, 8], F32), ytmp=([128, 128], F32), vaug=([128, 2, 66], BF16), ndsb=([128, 2, 66], F32))
    _first = dict(tok3=tok3, Sz=Sz, SC2=SC2, Q0t=Q0t, XU=XU, hpre=hpre, hn=hn, st6=st6, mv=mv, sm=sm, ytmp=ytmp, vaug=vaug, ndsb=ndsb)
    for _n, (_sh, _dt) in _shapes.items():
        S.add(_n, _first[_n], k.sb(_sh, _dt, _n + "_b"))
    S.add('PQ', PQ, [k.sb([128, 2, 2, 128], BF16, f"PQb{i}") for i in range(2)])
    S.add('Tb2', Tb2, [k.sb([128, 2, 128], BF16, f"Tb2b_{i}") for i in range(2)])
    S.add('ATt', ATt, [k.sb([128, 128], BF16, f"ATb{h}") for h in range(2)])
    banks = [k.ps([128, 512], F32, f"bank{i}") for i in range(8)]

    def bank():
        i = k.bank_i % 8
        k.bank_i += 1
        return i

    def bk(i):
        return ('bank', i)

    def bbf(i):
        return banks[i][:].bitcast(BF16)

    cc = c.cc
    pc = c.pc

    def C(name, w=1, off=0):
        return cst[:, cc[name] + off:cc[name] + off + w]

    dma('sp', cst[:], cst_d, writes=['cst'])
    for l in range(c.DEPTH):
        dma('sp', par[l][:], par_d[l], writes=[f'par{l}'])
        dma('pool', lorab[l][:], lora_d[l].rearrange("a p w -> p a w"), writes=[f'lora{l}'])
    op('dve', lambda e: e.tensor_copy(out=identb[:], in_=C('ident', 128)), reads=['cst'], writes=['identb'])
    op('dve', lambda e: e.tensor_copy(out=bonesb[:], in_=C('bones', 128)), reads=['cst'], writes=['bonesb'])
    for i, nm in enumerate(('mstrict', 'mincl', 'mstrict', 'mincl')):
        op('dve', lambda e, i=i, nm=nm: e.tensor_copy(out=mask4[:, i, :], in_=C(nm, 128)), reads=['cst'], writes=['mask4'])

    for h in range(2):
        op('dve', lambda e, h=h: e.tensor_copy(out=mN2[:, h, :], in_=C('mN', 128)), reads=['cst'], writes=['mN2'])
        op('dve', lambda e, h=h: e.tensor_copy(out=id2[:, h, :], in_=C('ident', 128)), reads=['cst'], writes=['id2'])
    slab_i = [0]

    def load_slab(src_ap, width):
        i = slab_i[0] % NSLAB
        slab_i[0] += 1
        dma('pool', slabs[i][:, 0:width], src_ap, writes=[f'slab{i}'])
        return i

    def P(l, name, w=1):
        return par[l][:, pc[name]:pc[name] + w]

    def rstd_tile(t0w):
        j = t0w // TT
        ri = (t0w // TN) % 2
        rstd, rkey = rstd2[ri], f'rstd{ri}'
        sq = Ft[11][:].bitcast(BF16)[:, 0:KD * TN].rearrange("p (a b) -> p a b", b=TN)
        op('act', lambda e: e.activation(out=sq, in_=xT[:, :, t0w:t0w + TN], func=AF.Square),
           reads=[('xT', j)], writes=['F11'])
        b = bank()
        for kk in range(KD):
            op('pe', lambda e, b=b, kk=kk: e.matmul(banks[b][:, 0:TN], lhsT=bonesall[:], rhs=sq[:, kk, :],
                                                     start=(kk == 0), stop=(kk == KD - 1)),
               reads=['F11', 'bonesall'], writes=[bk(b)])
        op('act', lambda e, b=b: e.activation(out=rstd[:], in_=banks[b][:, 0:TN], func=AF.Sqrt,
                                              scale=1.0 / c.D, bias=epsc[:, 0:1]),
           reads=[bk(b), 'epsc'], writes=[rkey])
        op('dve', lambda e: e.reciprocal(out=rstd[:], in_=rstd[:]), reads=[rkey], writes=[rkey])
        return rstd, rkey

    def rmsnorm(l, gname):
        for n in range(BLK // TN):
            t0w = n * TN
            j = t0w // TT
            ts = slice(t0w, t0w + TN)
            rstd, rkey = rstd_tile(t0w)
            for kk in range(KD):
                gap = par[l][:, pc[gname] + kk:pc[gname] + kk + 1]
                op('dve', lambda e, kk=kk, ts=ts, gap=gap, rstd=rstd: e.scalar_tensor_tensor(
                    out=hT[:, kk, ts], in0=xT[:, kk, ts], scalar=gap, in1=rstd[:], op0=ALU.mult, op1=ALU.mult),
                   reads=[('xT', j), rkey, f'par{l}'], writes=[('hT', j)])

    bonesall = k.sb([128, 128], BF16, "bonesall")
    epsc = k.sb([128, 4], F32, "epsc")
    op('dve', lambda e: e.memset(bonesall[:], 1.0), writes=['bonesall'])
    op('dve', lambda e: e.memset(epsc[:, 0:1], 1e-6), writes=['epsc'])
    op('dve', lambda e: e.memset(epsc[:, 1:2], 1e-5), writes=['epsc'])
    op('dve', lambda e: e.memset(epsc[:, 2:3], 64e-5), writes=['epsc'])
    op('dve', lambda e: e.memset(epsc[:, 3:4], 1.0), writes=['epsc'])

    def project(l, key, evac):
        gi = c.gidx[key]
        si = load_slab(win_d[l, gi], KD * 128)
        for j in range(NT):
            b = bank()
            for kk in range(KD):
                op('pe', lambda e, b=b, kk=kk, j=j, si=si: e.matmul(
                    banks[b][:, 0:TT], lhsT=slabs[si][:, kk * 128:(kk + 1) * 128], rhs=hT[:, kk, j * TT:(j + 1) * TT],
                    start=(kk == 0), stop=(kk == KD - 1)),
                   reads=[f'slab{si}', ('hT', j)], writes=[bk(b)])
            evac(b, j)

    def raw_evac(dst, dkey, l, key, first):
        def prep():
            if first:
                op('dve', lambda e: e.memset(dst[:, 0:HALO], 0.0), writes=[dkey])
            else:
                op('dve', lambda e: e.tensor_copy(out=dst[:, 0:HALO], in_=halo[(l, key)][:]),
                   reads=[('halo', l, key)], writes=[dkey])

        def ev(b, j):
            op('act', lambda e: e.activation(out=dst[:, HALO + j * TT:HALO + (j + 1) * TT], in_=banks[b][:, 0:TT],
                                             func=AF.Copy), reads=[bk(b)], writes=[dkey])

        def fin():
            op('dve', lambda e: e.tensor_copy(out=halo[(l, key)][:], in_=dst[:, BLK:BLK + HALO]),
               reads=[dkey], writes=[('halo', l, key)])
        return prep, ev, fin

    def proj_raw(l, key, dst, dkey, first):
        prep, ev, fin = raw_evac(dst, dkey, l, key, first)
        prep()
        project(l, key, ev)
        fin()

    def proj_simple(l, key, dst_ap_fn, dkey, func=AF.Copy):
        def ev(b, j):
            op('act', lambda e: e.activation(out=dst_ap_fn(j), in_=banks[b][:, 0:TT], func=func),
               reads=[bk(b)], writes=[dkey])
        project(l, key, ev)

    def tok_ln(eps_col):
        for h in range(2):
            op('dve', lambda e, h=h: e.bn_stats(out=S.st6[:, h, :], in_=S.hpre[:, h, :]), reads=[S.k('hpre')], writes=[S.k('st6')])
            op('dve', lambda e, h=h: e.bn_aggr(out=S.mv[:, h, :], in_=S.st6[:, h, :]), reads=[S.k('st6')], writes=[S.k('mv')])
        op('act', lambda e: e.activation(out=S.sm[:, 0:2], in_=S.mv[:, :, 1], func=AF.Sqrt, bias=epsc[:, eps_col:eps_col + 1]),
           reads=[S.k('mv'), 'epsc'], writes=[S.k('sm')])
        op('dve', lambda e: e.reciprocal(out=S.sm[:, 2:4], in_=S.sm[:, 0:2]), reads=[S.k('sm')], writes=[S.k('sm')])
        for h in range(2):
            op('dve', lambda e, h=h: e.tensor_scalar(out=S.hn[:, h * 64:(h + 1) * 64], in0=S.hpre[:, h, :],
                                                      scalar1=S.mv[:, h, 0:1], scalar2=S.sm[:, 2 + h:3 + h],
                                                      op0=ALU.subtract, op1=ALU.mult),
               reads=[S.k('hpre'), S.k('mv'), S.k('sm')], writes=[S.k('hn')])

    def transpose_to(bi, col, src_ap, rkeys):
        op('pe', lambda e: e.transpose(out=bbf(bi)[:, col:col + 128], in_=src_ap, identity=identb[:]),
           reads=list(rkeys) + ['identb'], writes=[bk(bi)])

    def linattn_chunk(ci, qc, qkey, kz, kzkey, vT, vkey, es_fn, es_keys, Sf, Sfkey, dec_ap, dec_keys, naug,
                      post):
        cs = slice(ci * L, (ci + 1) * L)
        W = 64 + naug
        bt_ = bank()
        transpose_to(bt_, 0, vT[:, cs], [vkey])
        transpose_to(bt_, 128, kz[:, 0, cs], [kzkey])
        transpose_to(bt_, 256, kz[:, 1, cs], [kzkey])
        op('act', lambda e: e.activation(out=S.tok3[:].rearrange("p a b -> p (a b)"), in_=bbf(bt_)[:, 0:384], func=AF.Copy),
           reads=[bk(bt_)], writes=[S.k('tok3')])
        for h in range(2):
            op('dve', lambda e, h=h: e.tensor_scalar(out=S.vaug[:, h, 0:64], in0=S.tok3[:, 0, h * 64:(h + 1) * 64],
                                                      scalar1=es_fn(h), scalar2=None, op0=ALU.mult),
               reads=[S.k('tok3')] + es_keys, writes=[S.k('vaug')])
            if naug:
                op('act', lambda e, h=h: e.activation(out=S.vaug[:, h, 64:65], in_=es_fn(h), func=AF.Copy),
                   reads=es_keys, writes=[S.k('vaug')])
        if DBG <= 2:
            return
        op('dve', lambda e: e.tensor_scalar(out=Sf[:, 0:W], in0=Sf[:, 0:W], scalar1=dec_ap, scalar2=None, op0=ALU.mult),
           reads=[Sfkey] + dec_keys, writes=[Sfkey])
        for h in range(2):
            op('act', lambda e, h=h: e.activation(out=S.Sz[h * 64:(h + 1) * 64, h, 0:W], in_=Sf[h * 64:(h + 1) * 64, 0:W],
                                                  func=AF.Copy), reads=[Sfkey], writes=[S.k('Sz')])
        if DBG <= 3:
            return
        for h in range(2):
            bs = bank()
            op('pe', lambda e, h=h, bs=bs: e.matmul(banks[bs][:, 0:128], lhsT=kz[:, h, cs], rhs=qc[:, cs], start=True, stop=True),
               reads=[kzkey, qkey], writes=[bk(bs)])
            op('dve', lambda e, h=h, bs=bs: e.tensor_tensor(out=S.ATt[h][:], in0=banks[bs][:, 0:128], in1=C('mincl', 128), op=ALU.mult),
               reads=[bk(bs), 'cst'], writes=[S.k(f'AT{h}')])
        bo = bank()
        for h in range(2):
            op('pe', lambda e, h=h: e.matmul(banks[bo][:, h * 128:h * 128 + W], lhsT=S.ATt[h][:], rhs=S.vaug[:, h, 0:W], start=True, stop=False),
               reads=[S.k(f'AT{h}'), S.k('vaug')], writes=[bk(bo)])
            op('pe', lambda e, h=h: e.matmul(banks[bo][:, h * 128:h * 128 + W], lhsT=qc[:, cs], rhs=S.Sz[:, h, 0:W], start=False, stop=True),
               reads=[qkey, S.k('Sz')], writes=[bk(bo)])
        if DBG <= 4 or DBG in (41, 42):
            return
        bu = bank()
        for h in range(2):
            op('pe', lambda e, h=h: e.matmul(banks[bu][:, 0:W], lhsT=S.tok3[:, 1 + h, :], rhs=S.vaug[:, h, 0:W], start=(h == 0), stop=(h == 1)),
               reads=[S.k('tok3'), S.k('vaug')], writes=[bk(bu)])
        op('dve', lambda e: e.tensor_tensor(out=Sf[:, 0:W], in0=Sf[:, 0:W], in1=banks[bu][:, 0:W], op=ALU.add),
           reads=[Sfkey, bk(bu)], writes=[Sfkey])
        if DBG <= 5:
            return
        post(bo)

    def finish_chunk(ci, pair_idx, eps_col, fin_fn):
        tok_ln(eps_col)
        bt2 = bank()
        transpose_to(bt2, 0, S.hn[:], [S.k('hn')])
        fin_fn(bt2, slice(ci * L, (ci + 1) * L))

    def mlstm(l, first):
        onesF = Ft[11]
        MH = c.MH
        ipre, lt, Bc, gt, Gt, esr, thr = Ft[4], Ft[5], Ft[6], Ft[7], Ft[8], Ft[9], Ft[10]

        def ev_gi(b, j):
            op('act', lambda e: e.activation(out=ipre[0:MH, j * TT:(j + 1) * TT], in_=banks[b][0:MH, 0:TT], func=AF.Identity,
                                             bias=par[l][0:MH, pc['bi']:pc['bi'] + 1]), reads=[bk(b), f'par{l}'], writes=['F4'])
        project(l, ('mgi', 0), ev_gi)

        def ev_gf(b, j):
            sl = slice(j * TT, (j + 1) * TT)
            op('act', lambda e: e.activation(out=lt[0:MH, sl], in_=banks[b][0:MH, 0:TT], func=AF.Identity,
                                             bias=par[l][0:MH, pc['bf']:pc['bf'] + 1]), reads=[bk(b), f'par{l}'], writes=['F5'])
            op('act', lambda e: e.activation(out=lt[0:MH, sl], in_=lt[0:MH, sl], func=AF.Exp, scale=-1.0), reads=['F5'], writes=['F5'])
            op('act', lambda e: e.activation(out=lt[0:MH, sl], in_=lt[0:MH, sl], func=AF.Ln, bias=epsc[0:MH, 3:4]),
               reads=['F5', 'epsc'], writes=['F5'])
        project(l, ('mgf', 0), ev_gf)
        if first:
            op('dve', lambda e: e.memset(gcar[l][:, 0:1], 0.0), writes=[f'gcar{l}'])
            op('dve', lambda e: e.memset(gcar[l][:, 1:2], -1e30), writes=[f'gcar{l}'])
        op('dve', lambda e: e.memset(onesF[0:MH, 0:BLK], 1.0), writes=['F11'])
        op('dve', lambda e: e.tensor_tensor_scan(out=Bc[0:MH, 0:BLK], data0=onesF[0:MH, 0:BLK], data1=lt[0:MH, 0:BLK],
                                                 initial=gcar[l][:, 0:1], op0=ALU.mult, op1=ALU.subtract),
           reads=['F11', 'F5', f'gcar{l}'], writes=['F6'])
        op('dve', lambda e: e.tensor_tensor(out=gt[0:MH, 0:BLK], in0=ipre[0:MH, 0:BLK], in1=Bc[0:MH, 0:BLK], op=ALU.subtract),
           reads=['F4', 'F6'], writes=['F7'])
        op('dve', lambda e: e.tensor_tensor_scan(out=Gt[0:MH, 0:BLK], data0=gt[0:MH, 0:BLK], data1=gt[0:MH, 0:BLK],
                                                 initial=gcar[l][:, 1:2], op0=ALU.max, op1=ALU.max),
           reads=['F7', f'gcar{l}'], writes=['F8'])
        op('dve', lambda e: e.tensor_copy(out=Rall[:, 0:1], in_=gcar[l][:, 1:2]), reads=[f'gcar{l}'], writes=['Rall'])
        op('dve', lambda e: e.tensor_copy(out=Rall[:, 1:NCH + 1], in_=Gt[0:MH, L - 1:BLK:L]), reads=['F8'], writes=['Rall'])
        op('dve', lambda e: e.tensor_copy(out=gcar[l][:, 0:1], in_=Bc[0:MH, BLK - 1:BLK]), reads=['F6'], writes=[f'gcar{l}'])
        op('dve', lambda e: e.tensor_copy(out=gcar[l][:, 1:2], in_=Gt[0:MH, BLK - 1:BLK]), reads=['F8'], writes=[f'gcar{l}'])
        op('dve', lambda e: e.tensor_tensor(out=decrow[:], in0=Rall[:, 0:NCH], in1=Rall[:, 1:NCH + 1], op=ALU.subtract),
           reads=['Rall'], writes=['decrow'])
        op('act', lambda e: e.activation(out=decrow[:], in_=decrow[:], func=AF.Exp), reads=['decrow'], writes=['decrow'])
        rcb = Rall[:, 1:NCH + 1].unsqueeze(2).to_broadcast([MH, NCH, L])
        op('dve', lambda e: e.tensor_tensor(out=esr[0:MH, 0:BLK].rearrange("p (a b) -> p a b", b=L),
                                            in0=gt[0:MH, 0:BLK].rearrange("p (a b) -> p a b", b=L), in1=rcb, op=ALU.subtract),
           reads=['F7', 'Rall'], writes=['F9'])
        op('act', lambda e: e.activation(out=esr[0:MH, 0:BLK], in_=esr[0:MH, 0:BLK], func=AF.Exp), reads=['F9'], writes=['F9'])
        op('dve', lambda e: e.tensor_tensor(out=thr[0:MH, 0:BLK].rearrange("p (a b) -> p a b", b=L),
                                            in0=Bc[0:MH, 0:BLK].rearrange("p (a b) -> p a b", b=L), in1=rcb, op=ALU.add),
           reads=['F6', 'Rall'], writes=['F10'])
        op('act', lambda e: e.activation(out=thr[0:MH, 0:BLK], in_=thr[0:MH, 0:BLK], func=AF.Exp, scale=-1.0), reads=['F10'], writes=['F10'])
        for src, skey, dst, dkey in ((esr, 'F9', est, 'est'), (thr, 'F10', thrt, 'thrt')):
            b = bank()
            for ci in range(NCH):
                op('pe', lambda e, ci=ci, src=src, b=b: e.transpose(out=banks[b][:, ci * MH:(ci + 1) * MH], in_=src[0:MH, ci * L:(ci + 1) * L],
                                                                     identity=C('ident', MH)[0:MH, :]),
                   reads=[skey, 'cst'], writes=[bk(b)])
            op('act', lambda e, b=b, dst=dst: e.activation(out=dst[:].rearrange("p a b -> p (a b)"), in_=banks[b][:, 0:NCH * MH], func=AF.Copy),
               reads=[bk(b)], writes=[dkey])
        for p in range(c.MP):
            praw_q, praw_k, acc = Ft[0], Ft[1], Ft[3]
            qc, vT_, sgo = Bt[0], Bt[3], Bt[4]
            kz = kzt_view[0]
            b = bank()
            op('pe', lambda e, b=b, p=p: e.matmul(banks[b][:, 0:NCH], lhsT=C('sel', 128, p * 128)[0:MH, :], rhs=decrow[:], start=True, stop=True),
               reads=['cst', 'decrow'], writes=[bk(b)])
            op('act', lambda e, b=b: e.activation(out=decb[:], in_=banks[b][:, 0:NCH], func=AF.Copy), reads=[bk(b)], writes=['decb'])
            for nm, praw, pk, cname in (('mq', praw_q, 'F0', 'cq'), ('mk', praw_k, 'F1', 'ck')):
                proj_raw(l, (nm, p), praw, pk, first)
                cw = pc[(cname, p)]
                op('dve', lambda e, praw=praw, cw=cw: e.tensor_scalar(out=acc[:, 0:BLK], in0=praw[:, HALO - 3:HALO - 3 + BLK],
                                                                     scalar1=par[l][:, cw:cw + 1], scalar2=None, op0=ALU.mult),
                   reads=[pk, f'par{l}'], writes=['F3'])
                for jj in range(1, 4):
                    op('dve', lambda e, praw=praw, cw=cw, jj=jj: e.scalar_tensor_tensor(
                        out=acc[:, 0:BLK], in0=praw[:, HALO - 3 + jj:HALO - 3 + jj + BLK], scalar=par[l][:, cw + jj:cw + jj + 1],
                        in1=acc[:, 0:BLK], op0=ALU.mult, op1=ALU.add), reads=[pk, f'par{l}', 'F3'], writes=['F3'])
                if nm == 'mq':
                    op('act', lambda e: e.activation(out=qc[:], in_=acc[:, 0:BLK], func=AF.Silu), reads=['F3'], writes=['B0'])
                else:
                    op('act', lambda e: e.activation(out=acc[:, 0:BLK], in_=acc[:, 0:BLK], func=AF.Silu), reads=['F3'], writes=['F3'])
                    for h in range(2):
                        op('dve', lambda e, h=h: e.tensor_scalar(out=kz[h * 64:(h + 1) * 64, h, :], in0=acc[h * 64:(h + 1) * 64, 0:BLK],
                                                                  scalar1=0.125, scalar2=None, op0=ALU.mult),
                           reads=['F3'], writes=['BZ'])
            proj_simple(l, ('mv', p), lambda j: vT_[:, j * TT:(j + 1) * TT], 'B3')
            proj_simple(l, ('mo', p), lambda j: sgo[:, j * TT:(j + 1) * TT], 'B4', func=AF.Sigmoid)
            if first:
                op('dve', lambda e, p=p: e.memset(Cf[l][p][:], 0.0), writes=[f'Cf{l}_{p}'])
            for ci in range(NCH):
                def post(bo, ci=ci, p=p):
                    op('act', lambda e: e.activation(out=S.ndsb[:, :, 0:65], in_=banks[bo][:, 0:256].rearrange("p (a b) -> p a b", a=2)[:, :, 0:65], func=AF.Copy),
                       reads=[bk(bo)], writes=[S.k('ndsb')])
                    op('act', lambda e: e.activation(out=S.sm[:, 4:6], in_=S.ndsb[:, :, 64], func=AF.Abs), reads=[S.k('ndsb')], writes=[S.k('sm')])
                    op('dve', lambda e: e.tensor_tensor(out=S.sm[:, 4:6], in0=S.sm[:, 4:6], in1=thrt[:, ci, 2 * p:2 * p + 2], op=ALU.max),
                       reads=[S.k('sm'), 'thrt'], writes=[S.k('sm')])
                    op('dve', lambda e: e.reciprocal(out=S.sm[:, 6:8], in_=S.sm[:, 4:6]), reads=[S.k('sm')], writes=[S.k('sm')])
                    for h in range(2):
                        op('dve', lambda e, h=h: e.tensor_scalar(out=S.hpre[:, h, :], in0=S.ndsb[:, h, 0:64],
                                                                  scalar1=S.sm[:, 6 + h:7 + h], scalar2=None, op0=ALU.mult),
                           reads=[S.k('ndsb'), S.k('sm')], writes=[S.k('hpre')])

                    def fin(bt2, cs):
                        op('dve', lambda e: e.scalar_tensor_tensor(out=yT[:, p, cs], in0=bbf(bt2)[:, 0:128],
                                                                   scalar=par[l][:, pc[('mlg', p)]:pc[('mlg', p)] + 1],
                                                                   in1=sgo[:, cs], op0=ALU.mult, op1=ALU.mult),
                           reads=[bk(bt2), f'par{l}', 'B4'], writes=[('yT', p)])
                    finish_chunk(ci, p, 1, fin)
                linattn_chunk(ci, qc, 'B0', kz, 'BZ', vT_, 'B3',
                              lambda h, ci=ci, p=p: est[:, ci, 2 * p + h:2 * p + h + 1], ['est'],
                              Cf[l][p], f'Cf{l}_{p}', decb[:, ci:ci + 1], ['decb'], 1, post)

    kzt_view = [None]

    def retention(l, first, blk):
        cosT, sinT = Ft[11], Ft[10]
        pos = slice(blk * BLK, (blk + 1) * BLK)
        dma('sp', cosT[:, 0:BLK], rot_d[0][:, pos], writes=['F11'])
        dma('sp', sinT[:, 0:BLK], rot_d[1][:, pos], writes=['F10'])
        kz = kzt_view[0]
        for p in range(c.RP):
            t1, t2 = Ft[0], Ft[1]
            qr, vT_, sg = Bt[0], Bt[3], Bt[4]
            for nm in ('rq', 'rk'):
                def ev1(b, j):
                    sl = slice(j * TT, (j + 1) * TT)
                    op('dve', lambda e: e.tensor_tensor(out=t1[:, sl], in0=banks[b][:, 0:TT], in1=cosT[:, sl], op=ALU.mult),
                       reads=[bk(b), 'F11'], writes=['F0'])
                project(l, (nm, p), ev1)

                def ev2(b, j):
                    sl = slice(j * TT, (j + 1) * TT)
                    op('dve', lambda e: e.tensor_tensor(out=t2[:, sl], in0=banks[b][:, 0:TT], in1=sinT[:, sl], op=ALU.mult),
                       reads=[bk(b), 'F10'], writes=['F1'])
                project(l, (nm + 's', p), ev2)
                if nm == 'rq':
                    op('dve', lambda e: e.tensor_tensor(out=qr[:], in0=t1[:, 0:BLK], in1=t2[:, 0:BLK], op=ALU.add),
                       reads=['F0', 'F1'], writes=['B0'])
                else:
                    op('dve', lambda e: e.tensor_tensor(out=t1[:, 0:BLK], in0=t1[:, 0:BLK], in1=t2[:, 0:BLK], op=ALU.add),
                       reads=['F0', 'F1'], writes=['F0'])
                    for h in range(2):
                        op('dve', lambda e, h=h: e.tensor_scalar(out=kz[h * 64:(h + 1) * 64, h, :], in0=t1[h * 64:(h + 1) * 64, 0:BLK],
                                                                  scalar1=0.125, scalar2=None, op0=ALU.mult),
                           reads=['F0'], writes=['BZ'])
            proj_simple(l, ('rv', p), lambda j: vT_[:, j * TT:(j + 1) * TT], 'B3')
            proj_simple(l, ('rg', p), lambda j: sg[:, j * TT:(j + 1) * TT], 'B4', func=AF.Silu)
            if first:
                op('dve', lambda e, p=p: e.memset(Rf[l][p][:], 0.0), writes=[f'Rf{l}_{p}'])
            for ci in range(NCH if DBG > 1 else 0):
                def post(bo, ci=ci, p=p):
                    for h in range(2):
                        op('dve', lambda e, h=h: e.tensor_scalar(out=S.hpre[:, h, :], in0=banks[bo][:, h * 128:h * 128 + 64],
                                                                  scalar1=C('ft_r', 1, 2 * p + h), scalar2=None, op0=ALU.mult),
                           reads=[bk(bo), 'cst'], writes=[S.k('hpre')])

                    def fin(bt2, cs):
                        op('dve', lambda e: e.tensor_tensor(out=yT[:, c.MP + p, cs], in0=bbf(bt2)[:, 0:128], in1=sg[:, cs], op=ALU.mult),
                           reads=[bk(bt2), 'B4'], writes=[('yT', c.MP + p)])
                    finish_chunk(ci, c.MP + p, 1, fin)
                linattn_chunk(ci, qr, 'B0', kz, 'BZ', vT_, 'B3',
                              lambda h, p=p: C('es_r', 1, 2 * p + h), ['cst'],
                              Rf[l][p], f'Rf{l}_{p}', C('dec_r', 1, p), ['cst'], 0, post)

    def rwkv(l, first):
        tw, alb, sgl = Bt[9], Bt[10], Bt[11]
        tmp = Ft[3]

        def shifted(key, dst, dkey):
            proj_raw(l, key, dst, dkey, first)
            mcol = pc[('mu', key)]
            op('dve', lambda e: e.tensor_tensor(out=tmp[:, 0:BLK], in0=dst[:, HALO - 1:HALO - 1 + BLK], in1=dst[:, HALO:HALO + BLK], op=ALU.subtract),
               reads=[dkey], writes=['F3'])
            op('dve', lambda e: e.scalar_tensor_tensor(out=dst[:, HALO:HALO + BLK], in0=tmp[:, 0:BLK], scalar=par[l][:, mcol:mcol + 1],
                                                       in1=dst[:, HALO:HALO + BLK], op0=ALU.mult, op1=ALU.add),
               reads=['F3', dkey, f'par{l}'], writes=[dkey])
        shifted(('wwl', 0), Ft[0], 'F0')
        op('act', lambda e: e.activation(out=tw[:], in_=Ft[0][:, HALO:HALO + BLK], func=AF.Tanh), reads=['F0'], writes=['B9'])
        shifted(('wal', 0), Ft[0], 'F0')
        op('act', lambda e: e.activation(out=alb[:], in_=Ft[0][:, HALO:HALO + BLK], func=AF.Copy), reads=['F0'], writes=['B10'])
        shifted(('wgl', 0), Ft[0], 'F0')
        op('act', lambda e: e.activation(out=sgl[:], in_=Ft[0][:, HALO:HALO + BLK], func=AF.Sigmoid), reads=['F0'], writes=['B11'])
        for p in range(c.WP):
            ka = pc[('ka', p)]
            op('dve', lambda e, p=p, ka=ka: e.tensor_scalar(out=omka[:, p:p + 1], in0=par[l][:, ka:ka + 1], scalar1=-1.0, scalar2=1.0,
                                                             op0=ALU.mult, op1=ALU.add), reads=[f'par{l}'], writes=['omka'])
        def pair_body(p):
            rs, ks, vs = Ft[0], Ft[1], Ft[2]
            lw, cl, at, kap, kmod, ak, Et, gT, bv = Ft[4], Ft[5], Ft[6], Ft[7], Ft[8], Ft[9], Ft[10], Ft[11], Ft[2]
            vTb, bh, kh = Bt[0], Bt[3], Bt[4]
            AR = ARv[0]
            BZ = BZv[0]
            KZ = KZv[0]
            H0 = slice(HALO, HALO + BLK)
            if p == 0:
                shifted(('wr', p), rs, 'F0')
                shifted(('wk', p), ks, 'F1')
            shifted(('wv', p), vs, 'F2')
            op('act', lambda e: e.activation(out=vTb[:], in_=vs[:, H0], func=AF.Copy), reads=['F2'], writes=['B0'])
            cols = slice(p * 128, (p + 1) * 128)
            for j in range(NT):
                sl = slice(j * TT, (j + 1) * TT)
                b1 = bank()
                op('pe', lambda e, b1=b1, sl=sl: e.matmul(banks[b1][:, 0:TT], lhsT=lorab[l][:, 0, cols], rhs=tw[:, sl], start=True, stop=True),
                   reads=[f'lora{l}', 'B9'], writes=[bk(b1)])
                op('act', lambda e, b1=b1, sl=sl: e.activation(out=lw[:, sl], in_=banks[b1][:, 0:TT], func=AF.Sigmoid,
                                                               bias=P(l, ('w0', p))), reads=[bk(b1), f'par{l}'], writes=['F4'])
                b2 = bank()
                op('pe', lambda e, b2=b2, sl=sl: e.matmul(banks[b2][:, 0:TT], lhsT=lorab[l][:, 1, cols], rhs=alb[:, sl], start=True, stop=True),
                   reads=[f'lora{l}', 'B10'], writes=[bk(b2)])
                op('act', lambda e, b2=b2, sl=sl: e.activation(out=at[:, sl], in_=banks[b2][:, 0:TT], func=AF.Sigmoid,
                                                               bias=P(l, ('a0', p))), reads=[bk(b2), f'par{l}'], writes=['F6'])
                b3 = bank()
                op('pe', lambda e, b3=b3, sl=sl: e.matmul(banks[b3][:, 0:TT], lhsT=lorab[l][:, 2, cols], rhs=sgl[:, sl], start=True, stop=True),
                   reads=[f'lora{l}', 'B11'], writes=[bk(b3)])
                op('act', lambda e, b3=b3, sl=sl: e.activation(out=gT[:, sl], in_=banks[b3][:, 0:TT], func=AF.Copy), reads=[bk(b3)], writes=['F11'])
            op('dve', lambda e: e.tensor_scalar(out=lw[:, 0:BLK], in0=lw[:, 0:BLK], scalar1=-math.exp(-0.5), scalar2=None, op0=ALU.mult),
               reads=['F4'], writes=['F4'])
            op('dve', lambda e: e.tensor_scalar(out=kap[:, 0:BLK], in0=ks[:, H0], scalar1=P(l, ('kk', p)), scalar2=None, op0=ALU.mult),
               reads=['F1', f'par{l}'], writes=['F7'])
            op('act', lambda e: e.activation(out=bh[:], in_=kap[:, 0:BLK], func=AF.Square), reads=['F7'], writes=['B3'])
            for j in range(NT):
                sl = slice(j * TT, (j + 1) * TT)
                b1 = bank()
                op('pe', lambda e, b1=b1, sl=sl: e.matmul(banks[b1][:, 0:TT], lhsT=bonesb[:], rhs=bh[:, sl], start=True, stop=True),
                   reads=['bonesb', 'B3'], writes=[bk(b1)])
                op('act', lambda e, b1=b1, sl=sl: e.activation(out=tmp[:, sl], in_=banks[b1][:, 0:TT], func=AF.Sqrt), reads=[bk(b1)], writes=['F3'])
            op('dve', lambda e: e.tensor_scalar(out=tmp[:, 0:BLK], in0=tmp[:, 0:BLK], scalar1=1e-12, scalar2=None, op0=ALU.max), reads=['F3'], writes=['F3'])
            op('dve', lambda e: e.reciprocal(out=tmp[:, 0:BLK], in_=tmp[:, 0:BLK]), reads=['F3'], writes=['F3'])
            op('dve', lambda e: e.tensor_tensor(out=kap[:, 0:BLK], in0=kap[:, 0:BLK], in1=tmp[:, 0:BLK], op=ALU.mult), reads=['F7', 'F3'], writes=['F7'])
            op('dve', lambda e: e.tensor_scalar(out=tmp[:, 0:BLK], in0=at[:, 0:BLK], scalar1=P(l, ('ka', p)), scalar2=omka[:, p:p + 1],
                                                op0=ALU.mult, op1=ALU.add), reads=['F6', f'par{l}', 'omka'], writes=['F3'])
            op('dve', lambda e: e.tensor_tensor(out=kmod[:, 0:BLK], in0=ks[:, H0], in1=tmp[:, 0:BLK], op=ALU.mult), reads=['F1', 'F3'], writes=['F8'])
            op('dve', lambda e: e.scalar_tensor_tensor(out=kh[:], in0=rs[:, H0], scalar=P(l, ('rk', p)), in1=kmod[:, 0:BLK],
                                                       op0=ALU.mult, op1=ALU.mult), reads=['F0', 'F8', f'par{l}'], writes=['B4'])
            for j in range(NT):
                sl = slice(j * TT, (j + 1) * TT)
                b1 = bank()
                op('pe', lambda e, b1=b1, sl=sl: e.matmul(banks[b1][:, 0:TT], lhsT=bonesb[:], rhs=kh[:, sl], start=True, stop=True),
                   reads=['bonesb', 'B4'], writes=[bk(b1)])
                op('dve', lambda e, b1=b1, j=j: e.tensor_tensor(out=bv[:, HALO + j * TT:HALO + (j + 1) * TT], in0=banks[b1][:, 0:TT], in1=vs[:, HALO + j * TT:HALO + (j + 1) * TT], op=ALU.mult),
                   reads=[bk(b1), 'F2'], writes=['F2'])
            op('dve', lambda e: e.tensor_tensor_scan(out=cl[:, 0:BLK], data0=resetm[:], data1=lw[:, 0:BLK], initial=0.0,
                                                     op0=ALU.mult, op1=ALU.add), reads=['resetm', 'F4'], writes=['F5'])
            op('dve', lambda e: e.tensor_tensor(out=ak[:, 0:BLK], in0=at[:, 0:BLK], in1=kap[:, 0:BLK], op=ALU.mult), reads=['F6', 'F7'], writes=['F9'])
            op('dve', lambda e: e.tensor_tensor(out=tmp[:, 0:BLK], in0=cl[:, 0:BLK], in1=lw[:, 0:BLK], op=ALU.subtract), reads=['F5', 'F4'], writes=['F3'])
            op('act', lambda e: e.activation(out=Et[:, 0:BLK], in_=tmp[:, 0:BLK], func=AF.Exp), reads=['F3'], writes=['F10'])
            op('dve', lambda e: e.scalar_tensor_tensor(out=AR[:, :, 0, :], in0=kap[:, 0:BLK].rearrange("p (a b) -> p a b", b=L), scalar=-1.0,
                                                       in1=Et[:, 0:BLK].rearrange("p (a b) -> p a b", b=L), op0=ALU.mult, op1=ALU.mult),
               reads=['F7', 'F10'], writes=['AR'])
            op('act', lambda e: e.activation(out=Et[:, 0:BLK], in_=cl[:, 0:BLK], func=AF.Exp), reads=['F5'], writes=['F10'])
            op('dve', lambda e: e.tensor_tensor(out=AR[:, :, 1, :], in0=rs[:, H0].rearrange("p (a b) -> p a b", b=L),
                                                in1=Et[:, 0:BLK].rearrange("p (a b) -> p a b", b=L), op=ALU.mult),
               reads=['F0', 'F10'], writes=['AR'])
            op('dve', lambda e: e.tensor_copy(out=WLt[:], in_=Et[:, L - 1:BLK:L]), reads=['F10'], writes=['WLt'])
            op('act', lambda e: e.activation(out=Et[:, 0:BLK], in_=cl[:, 0:BLK], func=AF.Exp, scale=-1.0), reads=['F5'], writes=['F10'])
            for h in range(2):
                hs = slice(h * 64, (h + 1) * 64)
                op('dve', lambda e, h=h, hs=hs: e.tensor_tensor(out=BZ[hs, h, :], in0=ak[hs, 0:BLK], in1=Et[hs, 0:BLK], op=ALU.mult),
                   reads=['F9', 'F10'], writes=['BZ'])
                op('dve', lambda e, h=h, hs=hs: e.tensor_tensor(out=KZ[hs, h, :], in0=kmod[hs, 0:BLK], in1=Et[hs, 0:BLK], op=ALU.mult),
                   reads=['F8', 'F10'], writes=['KZ'])
            clL = cl[:, L - 1:BLK:L].unsqueeze(2).to_broadcast([128, NCH, L])
            op('dve', lambda e: e.tensor_tensor(out=tmp[:, 0:BLK].rearrange("p (a b) -> p a b", b=L), in0=clL,
                                                in1=cl[:, 0:BLK].rearrange("p (a b) -> p a b", b=L), op=ALU.subtract), reads=['F5'], writes=['F3'])
            op('act', lambda e: e.activation(out=Et[:, 0:BLK], in_=tmp[:, 0:BLK], func=AF.Exp), reads=['F3'], writes=['F10'])
            op('dve', lambda e: e.tensor_tensor(out=bh[:], in0=ak[:, 0:BLK], in1=Et[:, 0:BLK], op=ALU.mult), reads=['F9', 'F10'], writes=['B3'])
            op('dve', lambda e: e.tensor_tensor(out=kh[:], in0=kmod[:, 0:BLK], in1=Et[:, 0:BLK], op=ALU.mult), reads=['F8', 'F10'], writes=['B4'])
            if first:
                op('dve', lambda e, p=p: e.memset(Mf[l][p][:], 0.0), writes=[f'Mf{l}_{p}'])
            Mfp, Mkey = Mf[l][p], f'Mf{l}_{p}'
            def chunk_body(ci):
                par = ci % 2
                S.set(par)
                cs = slice(ci * L, (ci + 1) * L)
                bt_ = bank()
                transpose_to(bt_, 0, vTb[:, cs], ['B0'])
                transpose_to(bt_, 128, bh[:, cs], ['B3'])
                transpose_to(bt_, 256, kh[:, cs], ['B4'])
                op('act', lambda e: e.activation(out=S.tok3[:].rearrange("p a b -> p (a b)"), in_=bbf(bt_)[:, 0:384], func=AF.Copy),
                   reads=[bk(bt_)], writes=[S.k('tok3')])
                yield
                S.set(par)
                ARc = AR[:, ci, :, :].rearrange("p a b -> p (a b)")
                bn = bank()
                for h in range(2):
                    bs = bank()
                    op('pe', lambda e, h=h, bs=bs: e.matmul(banks[bs][:, 0:256], lhsT=BZ[:, h, cs], rhs=ARc, start=True, stop=True),
                       reads=['BZ', 'AR'], writes=[bk(bs)])
                    op('pe', lambda e, h=h, bs=bs: e.matmul(banks[bs][:, 256:512], lhsT=KZ[:, h, cs], rhs=ARc, start=True, stop=True),
                       reads=['KZ', 'AR'], writes=[bk(bs)])
                    op('dve', lambda e, h=h, bs=bs: e.tensor_tensor(out=S.SC2[:, h, :, :].rearrange("p a b -> p (a b)"), in0=banks[bs][:, 0:512],
                                                                     in1=mask4[:].rearrange("p a b -> p (a b)"), op=ALU.mult),
                       reads=[bk(bs), 'mask4'], writes=[S.k('SC2')])
                    op('pe', lambda e, h=h: e.matmul(banks[bn][:, h * 128:(h + 1) * 128], lhsT=AR[:, ci, 0, :], rhs=BZ[:, h, cs], start=True, stop=True),
                       reads=['AR', 'BZ'], writes=[bk(bn)])
                op('dve', lambda e: e.tensor_tensor(out=S.Q0t[:].rearrange("p a b -> p (a b)"), in0=banks[bn][:, 0:256],
                                                    in1=mN2[:].rearrange("p a b -> p (a b)"), op=ALU.mult),
                   reads=[bk(bn), 'mN2'], writes=[S.k('Q0t')])
                op('dve', lambda e: e.tensor_tensor(out=S.Tb2[0][:], in0=S.SC2[:, :, 0, :], in1=id2[:], op=ALU.add),
                   reads=[S.k('SC2'), 'id2'], writes=[S.k('Tb2_0')])
                yield
                S.set(par)
                NLEV = 7
                tcur = 0
                for lev in range(NLEV - 1):
                    nxt = lev % 2
                    bp = bank()
                    for h in range(2):
                        if lev == 0:
                            pc_, pk_ = S.SC2[:, h, 0, :], S.k('SC2')
                            qc_, qk_ = S.Q0t[:, h, :], S.k('Q0t')
                        else:
                            pc_, pk_ = S.PQ[1 - nxt][:, h, 0, :], S.k(f'PQ{1 - nxt}')
                            qc_, qk_ = S.PQ[1 - nxt][:, h, 1, :], S.k(f'PQ{1 - nxt}')
                        op('pe', lambda e, h=h, bp=bp, pc_=pc_, qc_=qc_: e.matmul(banks[bp][:, h * 256:h * 256 + 128], lhsT=qc_, rhs=pc_, start=True, stop=True),
                           reads=[pk_, qk_], writes=[bk(bp)])
                        op('pe', lambda e, h=h, bp=bp, pc_=pc_, qc_=qc_: e.matmul(banks[bp][:, h * 256 + 128:h * 256 + 256], lhsT=pc_, rhs=qc_, start=True, stop=True),
                           reads=[pk_, qk_], writes=[bk(bp)])
                    op('act', lambda e, bp=bp, nxt=nxt: e.activation(out=S.PQ[nxt][:].rearrange("p a b c -> p (a b c)"), in_=banks[bp][:, 0:512], func=AF.Copy),
                       reads=[bk(bp)], writes=[S.k(f'PQ{nxt}')])
                    yield
                    S.set(par)
                    bt3 = bank()
                    for h in range(2):
                        op('pe', lambda e, h=h, bt3=bt3, nxt=nxt, tcur=tcur: e.matmul(banks[bt3][:, h * 128:(h + 1) * 128], lhsT=S.PQ[nxt][:, h, 1, :],
                                                                                 rhs=S.Tb2[tcur][:, h, :], start=True, stop=True),
                           reads=[S.k(f'PQ{nxt}'), S.k(f'Tb2_{tcur}')], writes=[bk(bt3)])
                    op('dve', lambda e, bt3=bt3, tcur=tcur: e.tensor_tensor(out=S.Tb2[1 - tcur][:].rearrange("p a b -> p (a b)"),
                                                                           in0=S.Tb2[tcur][:].rearrange("p a b -> p (a b)"), in1=banks[bt3][:, 0:256], op=ALU.add),
                       reads=[S.k(f'Tb2_{tcur}'), bk(bt3)], writes=[S.k(f'Tb2_{1 - tcur}')])
                    tcur = 1 - tcur
                    yield
                    S.set(par)
                tfin = tcur
                for h in range(2):
                    op('act', lambda e, h=h: e.activation(out=S.Sz[h * 64:(h + 1) * 64, h, 0:64], in_=Mfp[h * 64:(h + 1) * 64, :], func=AF.Copy),
                       reads=[Mkey], writes=[S.k('Sz')])
                bx = bank()
                for h in range(2):
                    op('pe', lambda e, h=h: e.matmul(banks[bx][:, h * 64:(h + 1) * 64], lhsT=AR[:, ci, 0, :], rhs=S.Sz[:, h, 0:64], start=True, stop=False),
                       reads=['AR', S.k('Sz')], writes=[bk(bx)])
                    op('pe', lambda e, h=h: e.matmul(banks[bx][:, h * 64:(h + 1) * 64], lhsT=S.SC2[:, h, 2, :], rhs=S.tok3[:, 0, h * 64:(h + 1) * 64], start=False, stop=True),
                       reads=[S.k('SC2'), S.k('tok3')], writes=[bk(bx)])
                op('act', lambda e: e.activation(out=S.XU[:, 0, :, :].rearrange("p a b -> p (a b)"), in_=banks[bx][:, 0:128], func=AF.Copy), reads=[bk(bx)], writes=[S.k('XU')])
                yield
                S.set(par)
                bu_ = bank()
                for h in range(2):
                    op('pe', lambda e, h=h: e.matmul(banks[bu_][:, h * 64:(h + 1) * 64], lhsT=S.Tb2[tfin][:, h, :], rhs=S.XU[:, 0, h, :], start=True, stop=True),
                       reads=[S.k(f'Tb2_{tfin}'), S.k('XU')], writes=[bk(bu_)])
                op('act', lambda e: e.activation(out=S.XU[:, 1, :, :].rearrange("p a b -> p (a b)"), in_=banks[bu_][:, 0:128], func=AF.Copy), reads=[bk(bu_)], writes=[S.k('XU')])
                yield
                S.set(par)
                by = bank()
                for h in range(2):
                    op('pe', lambda e, h=h: e.matmul(banks[by][:, h * 64:(h + 1) * 64], lhsT=AR[:, ci, 1, :], rhs=S.Sz[:, h, 0:64], start=True, stop=False),
                       reads=['AR', S.k('Sz')], writes=[bk(by)])
                    op('pe', lambda e, h=h: e.matmul(banks[by][:, h * 64:(h + 1) * 64], lhsT=S.SC2[:, h, 1, :], rhs=S.XU[:, 1, h, :], start=False, stop=False),
                       reads=[S.k('SC2'), S.k('XU')], writes=[bk(by)])
                    op('pe', lambda e, h=h: e.matmul(banks[by][:, h * 64:(h + 1) * 64], lhsT=S.SC2[:, h, 3, :], rhs=S.tok3[:, 0, h * 64:(h + 1) * 64], start=False, stop=True),
                       reads=[S.k('SC2'), S.k('tok3')], writes=[bk(by)])
                op('act', lambda e: e.activation(out=S.hpre[:].rearrange("p a b -> p (a b)"), in_=banks[by][:, 0:128], func=AF.Copy), reads=[bk(by)], writes=[S.k('hpre')])
                yield
                S.set(par)
                bm = bank()
                op('pe', lambda e: e.matmul(banks[bm][:, 0:128], lhsT=S.tok3[:, 1, :], rhs=S.XU[:, 1, :, :].rearrange("p a b -> p (a b)"), start=True, stop=False),
                   reads=[S.k('tok3'), S.k('XU')], writes=[bk(bm)])
                op('pe', lambda e: e.matmul(banks[bm][:, 0:128], lhsT=S.tok3[:, 2, :], rhs=S.tok3[:, 0, :], start=False, stop=True),
                   reads=[S.k('tok3')], writes=[bk(bm)])
                for h in range(2):
                    hs = slice(h * 64, (h + 1) * 64)
                    op('dve', lambda e, h=h, hs=hs: e.scalar_tensor_tensor(out=Mfp[hs, :], in0=Mfp[hs, :], scalar=WLt[hs, ci:ci + 1],
                                                                           in1=banks[bm][hs, h * 64:(h + 1) * 64], op0=ALU.mult, op1=ALU.add),
                       reads=[Mkey, 'WLt', bk(bm)], writes=[Mkey])

                def fin(bt2, cs):
                    op('dve', lambda e: e.tensor_scalar(out=S.ytmp[:], in0=bbf(bt2)[:, 0:128], scalar1=P(l, ('lg', p)), scalar2=P(l, ('lb', p)),
                                                        op0=ALU.mult, op1=ALU.add), reads=[bk(bt2), f'par{l}'], writes=[S.k('ytmp')])
                    op('dve', lambda e: e.tensor_tensor(out=S.ytmp[:], in0=S.ytmp[:], in1=bv[:, HALO + cs.start:HALO + cs.stop], op=ALU.add), reads=[S.k('ytmp'), 'F2'], writes=[S.k('ytmp')])
                    op('dve', lambda e: e.tensor_tensor(out=yT[:, c.MP + c.RP + p, cs], in0=S.ytmp[:], in1=gT[:, cs], op=ALU.mult),
                       reads=[S.k('ytmp'), 'F11'], writes=[('yT', c.MP + c.RP + p)])
                finish_chunk(ci, c.MP + c.RP + p, 2, fin)

            if p + 1 < c.WP:
                shifted(('wr', p + 1), rs, 'F0')
                shifted(('wk', p + 1), ks, 'F1')
            gens = [chunk_body(ci) for ci in range(NCH)]
            fin_ = [False] * NCH
            OFF = PIPE_OFF
            t_ = 0
            while not all(fin_):
                for ci in range(NCH):
                    if ci * OFF <= t_ and not fin_[ci]:
                        try:
                            next(gens[ci])
                        except StopIteration:
                            fin_[ci] = True
                t_ += 1
            S.set(0)

        for p in range(c.WP):
            pair_body(p)

    ARv = [k.sb([128, NCH, 2, L], BF16, "AR")]
    BZv = [k.sb([128, 2, BLK], BF16, "BZ")]
    kzt_view[0] = BZv[0]
    KZv = [k.sb([128, 2, BLK], BF16, "KZ")]
    resetm = k.sb([128, BLK], BF16, "resetm")
    op('dve', lambda e: e.memset(resetm[:], 1.0), writes=['resetm'])
    op('dve', lambda e: e.memset(resetm[:, 0:BLK:L], 0.0), writes=['resetm'])
    op('dve', lambda e: e.memset(BZv[0][:], 0.0), writes=['BZ'])
    op('dve', lambda e: e.memset(KZv[0][:], 0.0), writes=['KZ'])
    for _p in range(2):
        S.set(_p)
        op('dve', lambda e: e.memset(S.Sz[:], 0.0), writes=[S.k('Sz')])
    S.set(0)

    def wout_ffn(l):
        for o in range(KD):
            si = load_slab(wout_d[l, o], KM * 128)
            for j in range(NT):
                ts = slice(j * TT, (j + 1) * TT)
                b = bank()
                for kk in range(KM):
                    op('pe', lambda e, b=b, kk=kk, ts=ts, si=si: e.matmul(banks[b][:, 0:TT], lhsT=slabs[si][:, kk * 128:(kk + 1) * 128],
                                                                          rhs=yT[:, kk, ts], start=(kk == 0), stop=(kk == KM - 1)),
                       reads=[f'slab{si}', ('yT', kk)], writes=[bk(b)])
                op('dve', lambda e, b=b, o=o, ts=ts: e.tensor_tensor(out=xT[:, o, ts], in0=xT[:, o, ts], in1=banks[b][:, 0:TT], op=ALU.add),
                   reads=[bk(b), ('xT', j)], writes=[('xT', j)])
        rmsnorm(l, 'ln2')
        def aT(f):
            t = Ft[f // 2]
            v = t[:].bitcast(BF16)
            return v[:, (f % 2) * BLK:(f % 2) * BLK + BLK], f'F{f // 2}'
        sgt = Bt[0]
        for f in range(NF):
            si = load_slab(wgu_d[l, f], 2 * KD * 128)
            av, akey = aT(f)
            for j in range(NT):
                ts = slice(j * TT, (j + 1) * TT)
                bg, bu = bank(), bank()
                for gu, b in ((0, bg), (1, bu)):
                    for kk in range(KD):
                        off = (gu * KD + kk) * 128
                        op('pe', lambda e, b=b, kk=kk, ts=ts, si=si, off=off: e.matmul(banks[b][:, 0:TT], lhsT=slabs[si][:, off:off + 128],
                                                                                       rhs=hT[:, kk, ts], start=(kk == 0), stop=(kk == KD - 1)),
                           reads=[f'slab{si}', ('hT', j)], writes=[bk(b)])
                op('act', lambda e, bg=bg, ts=ts: e.activation(out=sgt[:, ts], in_=banks[bg][:, 0:TT], func=AF.Silu), reads=[bk(bg)], writes=['B0'])
                op('dve', lambda e, bu=bu, ts=ts, av=av: e.tensor_tensor(out=av[:, ts], in0=sgt[:, ts], in1=banks[bu][:, 0:TT], op=ALU.mult),
                   reads=['B0', bk(bu)], writes=[akey])
        for o in range(KD):
            sis = [load_slab(wdn_d[l, o][:, 0:NFH * 128], NFH * 128), load_slab(wdn_d[l, o][:, NFH * 128:NF * 128], (NF - NFH) * 128)]
            for j in range(NT):
                ts = slice(j * TT, (j + 1) * TT)
                b = bank()
                for f in range(NF):
                    av, akey = aT(f)
                    si = sis[f // NFH]
                    fo_ = (f % NFH) * 128
                    op('pe', lambda e, b=b, f=f, ts=ts, si=si, av=av, fo_=fo_: e.matmul(banks[b][:, 0:TT], lhsT=slabs[si][:, fo_:fo_ + 128],
                                                                               rhs=av[:, ts], start=(f == 0), stop=(f == NF - 1)),
                       reads=[f'slab{si}', akey], writes=[bk(b)])
                op('dve', lambda e, b=b, o=o, ts=ts: e.tensor_tensor(out=xT[:, o, ts], in0=xT[:, o, ts], in1=banks[b][:, 0:TT], op=ALU.add),
                   reads=[bk(b), ('xT', j)], writes=[('xT', j)])

    TPT = TT // 128
    for s in range(c.NSEQ):
        for blk in range(c.NBLK):
            first = (blk == 0)
            t0 = blk * BLK
            for tt in range(BLK // 128):
                dma('sp', xin[:], x_d[s, t0 + tt * 128:t0 + (tt + 1) * 128, :], writes=['xin'])
                for k4 in range(0, KD, 4):
                    b = bank()
                    n4 = min(4, KD - k4)
                    for q in range(n4):
                        op('pe', lambda e, b=b, q=q, k4=k4: e.transpose(out=banks[b][:, q * 128:(q + 1) * 128], in_=xin[:, (k4 + q) * 128:(k4 + q + 1) * 128],
                                                                         identity=C('ident', 128)), reads=['xin', 'cst'], writes=[bk(b)])
                    op('act', lambda e, b=b, k4=k4, n4=n4, tt=tt: e.activation(out=xT[:, k4:k4 + n4, tt * 128:(tt + 1) * 128],
                                                                               in_=banks[b][:, 0:n4 * 128].rearrange("p (a b) -> p a b", b=128), func=AF.Copy),
                       reads=[bk(b)], writes=[('xT', tt // TPT)])
            for l in range(c.DEPTH):
                PH = getattr(c, 'phases', 'nmrwf')
                if 'n' in PH:
                    rmsnorm(l, 'ln1')
                if 'm' in PH:
                    mlstm(l, first)
                if 'r' in PH:
                    retention(l, first, blk)
                if 'w' in PH:
                    rwkv(l, first)
                if 'f' in PH:
                    wout_ffn(l)
            fo = Ft[0]
            for n in range(BLK // TN):
                t0w = n * TN
                j = t0w // TT
                rstd, rkey = rstd_tile(t0w)
                for t8 in range(TN // 128):
                    tsl = slice(t0w + t8 * 128, t0w + (t8 + 1) * 128)
                    for kk in range(KD):
                        op('dve', lambda e, kk=kk, tsl=tsl, t8=t8: e.scalar_tensor_tensor(out=fo[:, kk * 128:(kk + 1) * 128], in0=xT[:, kk, tsl], scalar=C('lnf', 1, kk),
                                                                                          in1=rstd[:, t8 * 128:(t8 + 1) * 128], op0=ALU.mult, op1=ALU.mult),
                           reads=[('xT', j), rkey, 'cst'], writes=['F0'])
                    for k4 in range(0, KD, 4):
                        b2 = bank()
                        n4 = min(4, KD - k4)
                        for q in range(n4):
                            op('pe', lambda e, b2=b2, q=q, k4=k4: e.transpose(out=banks[b2][:, q * 128:(q + 1) * 128], in_=fo[:, (k4 + q) * 128:(k4 + q + 1) * 128],
                                                                               identity=C('ident', 128)), reads=['F0', 'cst'], writes=[bk(b2)])
                        op('act', lambda e, b2=b2, k4=k4, n4=n4: e.activation(out=xin[:, k4 * 128:(k4 + n4) * 128], in_=banks[b2][:, 0:n4 * 128], func=AF.Copy),
                           reads=[bk(b2)], writes=['xin'])
                    dma('sp', out_d[s, t0 + tsl.start:t0 + tsl.stop, :], xin[:], reads=['xin'], is_output=True)
    k.emit()
    return nc


_CACHE = {}


def kernel(**inputs):
    cfg = Cfg()
    packed = pack_host(cfg, inputs)
    if 'nc' not in _CACHE:
        _CACHE['nc'] = build(cfg)
    nc = _CACHE['nc']
    x = np.asarray(inputs['x'], np.float32)
    in_maps = []
    for i in range(8):
        m = dict(packed)
        m['x'] = np.ascontiguousarray(x[i * cfg.NSEQ:(i + 1) * cfg.NSEQ])
        m['win'] = packed['win'].reshape(cfg.DEPTH, cfg.NG, 128, cfg.KD * 128)
        m['wout'] = packed['wout'].reshape(cfg.DEPTH, cfg.KD, 128, cfg.KM * 128)
        m['wgu'] = packed['wgu'].reshape(cfg.DEPTH, cfg.NF, 128, 2 * cfg.KD * 128)
        m['wdn'] = packed['wdn'].reshape(cfg.DEPTH, cfg.KD, 128, cfg.NF * 128)
        in_maps.append(m)
    res = run_bass_kernel_spmd(nc, in_maps, core_ids=list(range(8)))
    return np.concatenate([r['out'] for r in res.results], axis=0).astype(np.float32)
```

```python
import math
DBG = 99
VAR = ''
from contextlib import ExitStack
import numpy as np
import concourse.bass as bass
import concourse.mybir as mybir
from concourse.alu_op_type import AluOpType as ALU
from concourse.bass_utils import run_bass_kernel_spmd

F32 = mybir.dt.float32
BF16 = mybir.dt.bfloat16
AF = mybir.ActivationFunctionType
AX = mybir.AxisListType

ENGS = ['pe', 'dve', 'act', 'pool', 'sp']
NDMASEM = 8
HALO = 4
DMASCR = 16384
SAME_ENGINE_SYNC = True
PIPE_OFF = 9
L = 128


class _Rec:
    def __init__(self):
        self.call = None

    def __getattr__(self, name):
        def f(*a, **kw):
            self.call = (name, a, kw)
            return self
        return f


class TS:
    def __init__(self):
        object.__setattr__(self, 'sets', [{}, {}])
        object.__setattr__(self, 'par', 0)

    def add(self, name, t0, t1):
        self.sets[0][name] = t0
        self.sets[1][name] = t1

    def set(self, par):
        object.__setattr__(self, 'par', par)

    def __getattr__(self, name):
        return self.sets[self.par][name]

    def k(self, name):
        return f'{name}@{self.par}'


class KB:
    def __init__(self, nc):
        self.nc = nc
        self.q = {e: [] for e in ENGS}
        self.cnt = {e: 0 for e in ENGS}
        self.lastw = {}
        self.readers = {}
        self.seen = {e: {} for e in ENGS}
        self.dma_i = {e: 0 for e in ENGS}
        self.dma_cnt = {}
        self.out_events = []
        self.stack = ExitStack()
        self.ntile = 0
        self.bank_i = 0

    def sb(self, shape, dt=F32, name=None):
        self.ntile += 1
        return self.stack.enter_context(self.nc.sbuf_tensor(name or f"t{self.ntile}", list(shape), dt))

    def ps(self, shape, dt=F32, name=None):
        self.ntile += 1
        return self.stack.enter_context(self.nc.psum_tensor(name or f"p{self.ntile}", list(shape), dt))

    def _deps(self, reads, writes):
        ev = []
        for k in reads:
            if k in self.lastw:
                ev.append(self.lastw[k])
        for k in writes:
            if k in self.lastw:
                ev.append(self.lastw[k])
            ev.extend(self.readers.get(k, []))
        return ev

    def _filter(self, eng, evs):
        best = {}
        for (s, v) in evs:
            if s[0] == 'eng' and s[1] == eng and (eng == 'pe' or not SAME_ENGINE_SYNC):
                continue
            if self.seen[eng].get(s, 0) >= v:
                continue
            if best.get(s, 0) < v:
                best[s] = v
        for s, v in best.items():
            self.seen[eng][s] = v
        return list(best.items())

    def _record(self, ev, reads, writes):
        for k in writes:
            self.lastw[k] = ev
            self.readers[k] = []
        for k in reads:
            self.readers.setdefault(k, []).append(ev)

    def op(self, eng, fn, reads=(), writes=()):
        waits = self._filter(eng, self._deps(reads, writes))
        self.cnt[eng] += 1
        ev = (('eng', eng), self.cnt[eng])
        rec = _Rec()
        fn(rec)
        call = rec.call
        self.q[eng].append(('op', waits, lambda e, call=call: getattr(e, call[0])(*call[1], **call[2])))
        self._record(ev, reads, writes)
        return ev

    def dma(self, eng, out, in_, reads=(), writes=(), is_output=False):
        i = self.dma_i[eng] % NDMASEM
        self.dma_i[eng] += 1
        s = ('dma', eng, i)
        prev = self.dma_cnt.get(s, 0)
        evs = self._deps(reads, writes)
        if prev:
            evs.append((s, prev * 16))
        waits = self._filter(eng, evs)
        self.dma_cnt[s] = prev + 1
        ev = (s, (prev + 1) * 16)
        self.q[eng].append(('dma', waits, (out, in_), s))
        self._record(ev, reads, writes)
        if is_output:
            self.out_events.append(ev)
        return ev

    def emit(self):
        nc = self.nc
        semh = {}
        with ExitStack() as st:
            for e in ENGS:
                semh[('eng', e)] = st.enter_context(nc.semaphore(f"s_{e}"))
            for s in self.dma_cnt:
                semh[s] = st.enter_context(nc.semaphore(f"d_{s[1]}_{s[2]}"))
            fin = self._filter('sp', self.out_events)
            with nc.Block() as block:
                def run(eng_name, engine):
                    me = semh[('eng', eng_name)]
                    for item in self.q[eng_name]:
                        if item[0] == 'op':
                            _, waits, fn = item
                            for s, v in waits:
                                engine.wait_ge(semh[s], v)
                            fn(engine).then_inc(me, 1)
                        else:
                            _, waits, (out, in_), s = item
                            for ss, v in waits:
                                engine.wait_ge(semh[ss], v)
                            engine.dma_start(out=out, in_=in_).then_inc(semh[s], 16)
                    if eng_name == 'sp':
                        for s, v in fin:
                            engine.wait_ge(semh[s], v)

                @block.tensor
                def _(e):
                    run('pe', e)

                @block.vector
                def _(e):
                    run('dve', e)

                @block.scalar
                def _(e):
                    run('act', e)

                @block.gpsimd
                def _(e):
                    run('pool', e)

                @block.sync
                def _(e):
                    run('sp', e)
        self.stack.close()


class Cfg:
    def __init__(s, D=1024, SEQ=2048, NSEQ=2, BLK=1024, DEPTH=2, MH=4, RH=6, WH=6, DFF=2816,
                 WL=64, AL=64, GL=128):
        s.D, s.SEQ, s.NSEQ, s.BLK, s.DEPTH = D, SEQ, NSEQ, BLK, DEPTH
        s.MH, s.RH, s.WH, s.DFF, s.WL, s.AL, s.GL = MH, RH, WH, DFF, WL, AL, GL
        s.KD = D // 128
        s.NBLK = SEQ // BLK
        s.NCH = BLK // L
        s.TT = min(512, BLK)
        s.NT = BLK // s.TT
        s.MP, s.RP, s.WP = MH // 2, RH // 2, WH // 2
        s.NF = DFF // 128
        s.MW, s.RW, s.WW = MH * 64, RH * 64, WH * 64
        s.MIX = s.MW + s.RW + s.WW
        s.KM = s.MIX // 128
        s.MCOLS = 4 * s.MW + 2 * MH
        s.RCOLS = 4 * s.RW
        s.WCOLS = 3 * s.WW + WL + AL + GL
        g = []
        mb = 0
        for p in range(s.MP):
            for nm, base in (('mq', 0), ('mk', s.MW), ('mv', 2 * s.MW), ('mo', 3 * s.MW)):
                g.append(((nm, p), [mb + base + 128 * p + i for i in range(128)]))
        g.append((('mgi', 0), [mb + 4 * s.MW + i for i in range(MH)]))
        g.append((('mgf', 0), [mb + 4 * s.MW + MH + i for i in range(MH)]))
        rb = s.MCOLS
        swap = [h * 64 + ((j + 32) % 64) for h in range(2) for j in range(64)]
        for p in range(s.RP):
            for nm, base in (('rq', 0), ('rk', s.RW), ('rv', 2 * s.RW), ('rg', 3 * s.RW)):
                g.append(((nm, p), [rb + base + 128 * p + i for i in range(128)]))
                if nm in ('rq', 'rk'):
                    g.append(((nm + 's', p), [rb + base + 128 * p + swap[i] for i in range(128)]))
        wb = s.MCOLS + s.RCOLS
        g.append((('wwl', 0), [wb + 3 * s.WW + i for i in range(WL)]))
        g.append((('wal', 0), [wb + 3 * s.WW + WL + i for i in range(AL)]))
        g.append((('wgl', 0), [wb + 3 * s.WW + WL + AL + i for i in range(GL)]))
        for p in range(s.WP):
            for nm, base in (('wr', 0), ('wk', s.WW), ('wv', 2 * s.WW)):
                g.append(((nm, p), [wb + base + 128 * p + i for i in range(128)]))
        s.groups = g
        s.gidx = {k: i for i, (k, _) in enumerate(g)}
        s.NG = len(g)
        pc = {}
        n = 0

        def add(name, w=1):
            nonlocal n
            pc[name] = n
            n += w
        add('ln1', s.KD)
        add('ln2', s.KD)
        for p in range(s.MP):
            add(('cq', p), 4)
            add(('ck', p), 4)
            add(('mlg', p))
        add('bi')
        add('bf')
        for key, _ in g:
            if key[0][0] == 'w':
                add(('mu', key))
        for p in range(s.WP):
            for nm in ('w0', 'a0', 'kk', 'ka', 'rk', 'lg', 'lb'):
                add((nm, p))
        s.pc = pc
        s.NPAR = n
        cc = {}
        n = 0

        def addc(name, w):
            nonlocal n
            cc[name] = n
            n += w
        addc('ident', 128)
        addc('mincl', 128)
        addc('mstrict', 128)
        addc('mN', 128)
        addc('bones', 128)
        addc('es_r', RH)
        addc('ft_r', RH)
        addc('dec_r', s.RP)
        addc('sel', s.MP * 128)
        addc('lnf', s.KD)
        s.cc = cc
        s.NC = n


def pack_host(cfg, inp):
    c = cfg
    f32 = np.float32
    D, KD = c.D, c.KD
    w_in = np.asarray(inp['w_in'], f32)
    win = np.zeros((c.DEPTH, c.NG, 128, KD, 128), f32)
    for gi, (key, cols) in enumerate(c.groups):
        sub = w_in[:, :, cols]
        sub = sub.reshape(c.DEPTH, KD, 128, len(cols)).transpose(0, 2, 1, 3)
        win[:, gi, :, :, :len(cols)] = sub
    wout = np.asarray(inp['w_out'], f32).reshape(c.DEPTH, c.KM, 128, KD, 128).transpose(0, 3, 2, 1, 4)
    wg = np.asarray(inp['w_gate'], f32).reshape(c.DEPTH, KD, 128, c.NF, 128).transpose(0, 3, 2, 1, 4)
    wu = np.asarray(inp['w_up'], f32).reshape(c.DEPTH, KD, 128, c.NF, 128).transpose(0, 3, 2, 1, 4)
    wgu = np.stack([wg, wu], axis=3)
    wdn = np.asarray(inp['w_down'], f32).reshape(c.DEPTH, c.NF, 128, KD, 128).transpose(0, 3, 2, 1, 4)
    lora = np.zeros((c.DEPTH, 3, 128, c.WW), f32)
    lora[:, 0, :c.WL] = inp['rw_w_up']
    lora[:, 1, :c.AL] = inp['rw_a_up']
    lora[:, 2, :c.GL] = inp['rw_g_up']
    par = np.zeros((c.DEPTH, 128, c.NPAR), f32)
    pc = c.pc
    par[:, :, pc['ln1']:pc['ln1'] + KD] = np.asarray(inp['ln1_g'], f32).reshape(c.DEPTH, KD, 128).transpose(0, 2, 1)
    par[:, :, pc['ln2']:pc['ln2'] + KD] = np.asarray(inp['ln2_g'], f32).reshape(c.DEPTH, KD, 128).transpose(0, 2, 1)
    mconv = np.asarray(inp['m_conv'], f32)
    for p in range(c.MP):
        par[:, :, pc[('cq', p)]:pc[('cq', p)] + 4] = mconv[:, :, 128 * p:128 * p + 128].transpose(0, 2, 1)
        par[:, :, pc[('ck', p)]:pc[('ck', p)] + 4] = mconv[:, :, c.MW + 128 * p:c.MW + 128 * p + 128].transpose(0, 2, 1)
        par[:, :, pc[('mlg', p)]] = np.asarray(inp['m_ln_g'], f32)[:, 128 * p:128 * p + 128]
    par[:, :c.MH, pc['bi']] = inp['m_b_i']
    par[:, :c.MH, pc['bf']] = inp['m_b_f']
    mu = np.asarray(inp['rw_mu'], f32)
    wbase = c.MCOLS + c.RCOLS
    for key, cols in c.groups:
        if key[0][0] == 'w':
            par[:, :len(cols), pc[('mu', key)]] = mu[:, [cc - wbase for cc in cols]]
    for p in range(c.WP):
        sl = slice(128 * p, 128 * p + 128)
        for nm, src in (('w0', 'rw_w0'), ('a0', 'rw_a0'), ('kk', 'rw_k_k'), ('ka', 'rw_k_a'),
                        ('rk', 'rw_r_k'), ('lg', 'rw_ln_g'), ('lb', 'rw_ln_b')):
            par[:, :, pc[(nm, p)]] = np.asarray(inp[src], f32)[:, sl]
    cst = np.zeros((128, c.NC), f32)
    cc = c.cc
    i = np.arange(128)
    cst[:, cc['ident']:cc['ident'] + 128] = np.eye(128)
    cst[:, cc['mincl']:cc['mincl'] + 128] = (i[:, None] <= i[None, :])
    cst[:, cc['mstrict']:cc['mstrict'] + 128] = (i[:, None] < i[None, :])
    cst[:, cc['mN']:cc['mN'] + 128] = (i[None, :] < i[:, None])
    cst[:, cc['bones']:cc['bones'] + 128] = (i[:, None] // 64 == i[None, :] // 64)
    gam = 1.0 - 2.0 ** (-5.0 - np.arange(c.RH, dtype=np.float64))
    cst[:, cc['es_r']:cc['es_r'] + c.RH] = gam[None, :] ** (L - 1.0 - i[:, None])
    cst[:, cc['ft_r']:cc['ft_r'] + c.RH] = gam[None, :] ** (i[:, None] - (L - 1.0))
    for p in range(c.RP):
        cst[:64, cc['dec_r'] + p] = gam[2 * p] ** L
        cst[64:, cc['dec_r'] + p] = gam[2 * p + 1] ** L
    for p in range(c.MP):
        for m in range(128):
            cst[2 * p + m // 64, cc['sel'] + p * 128 + m] = 1.0
    cst[:, cc['lnf']:cc['lnf'] + KD] = np.asarray(inp['lnf_g'], f32).reshape(KD, 128).T
    theta = 1.0 / (10000.0 ** np.linspace(0.0, 1.0, 32, dtype=np.float32))
    ang = np.arange(c.SEQ, dtype=np.float32)[None, :] * theta.astype(np.float32)[:, None]
    cs, sn = np.cos(ang).astype(f32), np.sin(ang).astype(f32)
    rot = np.zeros((2, 128, c.SEQ), f32)
    for h in range(2):
        rot[0, h * 64:h * 64 + 32] = cs
        rot[0, h * 64 + 32:h * 64 + 64] = cs
        rot[1, h * 64:h * 64 + 32] = -sn
        rot[1, h * 64 + 32:h * 64 + 64] = sn
    return dict(win=win, wout=np.ascontiguousarray(wout), wgu=np.ascontiguousarray(wgu),
                wdn=np.ascontiguousarray(wdn), lora=lora, par=par, cst=cst, rot=rot)


def build(cfg):
    c = cfg
    nc = bass.Bass("TRN2", target_bir_lowering=False, dynamic_dma_scratch_size=DMASCR)
    KD, BLK, TT, NT, NCH, NF, KM = c.KD, c.BLK, c.TT, c.NT, c.NCH, c.NF, c.KM
    x_d = nc.dram_tensor("x", [c.NSEQ, c.SEQ, c.D], F32, kind="ExternalInput").ap()
    win_d = nc.dram_tensor("win", [c.DEPTH, c.NG, 128, KD * 128], F32, kind="ExternalInput").ap()
    wout_d = nc.dram_tensor("wout", [c.DEPTH, KD, 128, KM * 128], F32, kind="ExternalInput").ap()
    wgu_d = nc.dram_tensor("wgu", [c.DEPTH, NF, 128, 2 * KD * 128], F32, kind="ExternalInput").ap()
    wdn_d = nc.dram_tensor("wdn", [c.DEPTH, KD, 128, NF * 128], F32, kind="ExternalInput").ap()
    lora_d = nc.dram_tensor("lora", [c.DEPTH, 3, 128, c.WW], F32, kind="ExternalInput").ap()
    par_d = nc.dram_tensor("par", [c.DEPTH, 128, c.NPAR], F32, kind="ExternalInput").ap()
    cst_d = nc.dram_tensor("cst", [128, c.NC], F32, kind="ExternalInput").ap()
    rot_d = nc.dram_tensor("rot", [2, 128, c.SEQ], F32, kind="ExternalInput").ap()
    out_d = nc.dram_tensor("out", [c.NSEQ, c.SEQ, c.D], F32, kind="ExternalOutput").ap()

    k = KB(nc)
    op, dma = k.op, k.dma
    xT = k.sb([128, KD, BLK], F32, "xT")
    hT = k.sb([128, KD, BLK], BF16, "hT")
    yT = k.sb([128, KM, BLK], BF16, "yT")
    NFH = (NF + 1) // 2
    SLABW = max(KD * 128, KM * 128, 2 * KD * 128, NFH * 128)
    NSLAB = 5
    slabs = [k.sb([128, SLABW], BF16, f"slab{i}") for i in range(NSLAB)]
    NFT = 12
    FW = max(HALO + BLK, c.D)
    Ft = [k.sb([128, FW], F32, f"F{i}") for i in range(NFT)]
    Bt = {i: k.sb([128, BLK], BF16, f"B{i}") for i in (0, 3, 4, 9, 10, 11)}
    cst = k.sb([128, c.NC], F32, "cst_sb")
    par = [k.sb([128, c.NPAR], F32, f"par_sb{l}") for l in range(c.DEPTH)]
    lorab = [k.sb([128, 3, c.WW], BF16, f"lora_sb{l}") for l in range(c.DEPTH)]
    identb = k.sb([128, 128], BF16, "identb")
    bonesb = k.sb([128, 128], BF16, "bonesb")
    mask4 = k.sb([128, 4, 128], BF16, "mask4")
    xin = k.sb([128, c.D], F32, "xin")
    TN = min(256, TT)
    rstd2 = [k.sb([128, TN], F32, f"rstd{i}") for i in range(2)]
    tok3 = k.sb([128, 3, 128], BF16, "tok3")
    vaug = k.sb([128, 2, 66], BF16, "vaug")
    ATt = [k.sb([128, 128], BF16, f"AT{h}") for h in range(2)]
    SC2 = k.sb([128, 2, 4, 128], BF16, "SC2")
    Q0t = k.sb([128, 2, 128], BF16, "Q0t")
    PQ = [k.sb([128, 2, 2, 128], BF16, f"PQ{i}") for i in range(2)]
    Tb2 = [k.sb([128, 2, 128], BF16, f"Tb2_{i}") for i in range(2)]
    mN2 = k.sb([128, 2, 128], BF16, "mN2")
    id2 = k.sb([128, 2, 128], BF16, "id2")
    XU = k.sb([128, 2, 2, 64], BF16, "XU")
    hpre = k.sb([128, 2, 64], F32, "hpre")
    ndsb = k.sb([128, 2, 66], F32, "ndsb")
    hn = k.sb([128, 128], BF16, "hn")
    st6 = k.sb([128, 2, 6], F32, "st6")
    mv = k.sb([128, 2, 2], F32, "mv")
    sm = k.sb([128, 8], F32, "sm")
    ytmp = k.sb([128, 128], F32, "ytmp")
    omka = k.sb([128, c.WP], F32, "omka")
    Cf = [[k.sb([128, 65], F32, f"Cf{l}_{p}") for p in range(c.MP)] for l in range(c.DEPTH)]
    Rf = [[k.sb([128, 64], F32, f"Rf{l}_{p}") for p in range(c.RP)] for l in range(c.DEPTH)]
    Mf = [[k.sb([128, 64], F32, f"Mf{l}_{p}") for p in range(c.WP)] for l in range(c.DEPTH)]
    Sz = k.sb([128, 2, 66], BF16, "Sz")
    halo_keys = [key for key, _ in c.groups if key[0] in ('mq', 'mk') or key[0][0] == 'w']
    halo = {(l, key): k.sb([128, HALO], F32, f"halo{l}_{key[0]}{key[1]}") for l in range(c.DEPTH) for key in halo_keys}
    gcar = [k.sb([c.MH, 2], F32, f"gcar{l}") for l in range(c.DEPTH)]
    Rall = k.sb([c.MH, NCH + 1], F32, "Rall")
    decrow = k.sb([c.MH, NCH], F32, "decrow")
    decb = k.sb([128, NCH], F32, "decb")
    est = k.sb([128, NCH, c.MH], F32, "est")
    thrt = k.sb([128, NCH, c.MH], F32, "thrt")
    WLt = k.sb([128, NCH], F32, "WLt")
    S = TS()
    _shapes = dict(tok3=([128, 3, 128], BF16), Sz=([128, 2, 66], BF16), SC2=([128, 2, 4, 128], BF16), Q0t=([128, 2, 128], BF16),
                   XU=([128, 2, 2, 64], BF16), hpre=([128, 2, 64], F32), hn=([128, 128], BF16), st6=([128, 2, 6], F32),
                   mv=([128, 2, 2], F32), sm=([128, 8], F32), ytmp=([128, 128], F32), vaug=([128, 2, 66], BF16), ndsb=([128, 2, 66], F32))
    _first = dict(tok3=tok3, Sz=Sz, SC2=SC2, Q0t=Q0t, XU=XU, hpre=hpre, hn=hn, st6=st6, mv=mv, sm=sm, ytmp=ytmp, vaug=vaug, ndsb=ndsb)
    for _n, (_sh, _dt) in _shapes.items():
        S.add(_n, _first[_n], k.sb(_sh, _dt, _n + "_b"))
    S.add('PQ', PQ, [k.sb([128, 2, 2, 128], BF16, f"PQb{i}") for i in range(2)])
    S.add('Tb2', Tb2, [k.sb([128, 2, 128], BF16, f"Tb2b_{i}") for i in range(2)])
    S.add('ATt', ATt, [k.sb([128, 128], BF16, f"ATb{h}") for h in range(2)])
    banks = [k.ps([128, 512], F32, f"bank{i}") for i in range(8)]

    def bank():
        i = k.bank_i % 8
        k.bank_i += 1
        return i

    def bk(i):
        return ('bank', i)

    def bbf(i):
        return banks[i][:].bitcast(BF16)

    cc = c.cc
    pc = c.pc

    def C(name, w=1, off=0):
        return cst[:, cc[name] + off:cc[name] + off + w]

    dma('sp', cst[:], cst_d, writes=['cst'])
    for l in range(c.DEPTH):
        dma('sp', par[l][:], par_d[l], writes=[f'par{l}'])
        dma('pool', lorab[l][:], lora_d[l].rearrange("a p w -> p a w"), writes=[f'lora{l}'])
    op('dve', lambda e: e.tensor_copy(out=identb[:], in_=C('ident', 128)), reads=['cst'], writes=['identb'])
    op('dve', lambda e: e.tensor_copy(out=bonesb[:], in_=C('bones', 128)), reads=['cst'], writes=['bonesb'])
    for i, nm in enumerate(('mstrict', 'mincl', 'mstrict', 'mincl')):
        op('dve', lambda e, i=i, nm=nm: e.tensor_copy(out=mask4[:, i, :], in_=C(nm, 128)), reads=['cst'], writes=['mask4'])

    for h in range(2):
        op('dve', lambda e, h=h: e.tensor_copy(out=mN2[:, h, :], in_=C('mN', 128)), reads=['cst'], writes=['mN2'])
        op('dve', lambda e, h=h: e.tensor_copy(out=id2[:, h, :], in_=C('ident', 128)), reads=['cst'], writes=['id2'])
    slab_i = [0]

    def load_slab(src_ap, width):
        i = slab_i[0] % NSLAB
        slab_i[0] += 1
        dma('pool', slabs[i][:, 0:width], src_ap, writes=[f'slab{i}'])
        return i

    def P(l, name, w=1):
        return par[l][:, pc[name]:pc[name] + w]

    def rstd_tile(t0w):
        j = t0w // TT
        ri = (t0w // TN) % 2
        rstd, rkey = rstd2[ri], f'rstd{ri}'
        sq = Ft[11][:].bitcast(BF16)[:, 0:KD * TN].rearrange("p (a b) -> p a b", b=TN)
        op('act', lambda e: e.activation(out=sq, in_=xT[:, :, t0w:t0w + TN], func=AF.Square),
           reads=[('xT', j)], writes=['F11'])
        b = bank()
        for kk in range(KD):
            op('pe', lambda e, b=b, kk=kk: e.matmul(banks[b][:, 0:TN], lhsT=bonesall[:], rhs=sq[:, kk, :],
                                                     start=(kk == 0), stop=(kk == KD - 1)),
               reads=['F11', 'bonesall'], writes=[bk(b)])
        op('act', lambda e, b=b: e.activation(out=rstd[:], in_=banks[b][:, 0:TN], func=AF.Sqrt,
                                              scale=1.0 / c.D, bias=epsc[:, 0:1]),
           reads=[bk(b), 'epsc'], writes=[rkey])
        op('dve', lambda e: e.reciprocal(out=rstd[:], in_=rstd[:]), reads=[rkey], writes=[rkey])
        return rstd, rkey

    def rmsnorm(l, gname):
        for n in range(BLK // TN):
            t0w = n * TN
            j = t0w // TT
            ts = slice(t0w, t0w + TN)
            rstd, rkey = rstd_tile(t0w)
            for kk in range(KD):
                gap = par[l][:, pc[gname] + kk:pc[gname] + kk + 1]
                op('dve', lambda e, kk=kk, ts=ts, gap=gap, rstd=rstd: e.scalar_tensor_tensor(
                    out=hT[:, kk, ts], in0=xT[:, kk, ts], scalar=gap, in1=rstd[:], op0=ALU.mult, op1=ALU.mult),
                   reads=[('xT', j), rkey, f'par{l}'], writes=[('hT', j)])

    bonesall = k.sb([128, 128], BF16, "bonesall")
    epsc = k.sb([128, 4], F32, "epsc")
    op('dve', lambda e: e.memset(bonesall[:], 1.0), writes=['bonesall'])
    op('dve', lambda e: e.memset(epsc[:, 0:1], 1e-6), writes=['epsc'])
    op('dve', lambda e: e.memset(epsc[:, 1:2], 1e-5), writes=['epsc'])
    op('dve', lambda e: e.memset(epsc[:, 2:3], 64e-5), writes=['epsc'])
    op('dve', lambda e: e.memset(epsc[:, 3:4], 1.0), writes=['epsc'])

    def project(l, key, evac):
        gi = c.gidx[key]
        si = load_slab(win_d[l, gi], KD * 128)
        for j in range(NT):
            b = bank()
            for kk in range(KD):
                op('pe', lambda e, b=b, kk=kk, j=j, si=si: e.matmul(
                    banks[b][:, 0:TT], lhsT=slabs[si][:, kk * 128:(kk + 1) * 128], rhs=hT[:, kk, j * TT:(j + 1) * TT],
                    start=(kk == 0), stop=(kk == KD - 1)),
                   reads=[f'slab{si}', ('hT', j)], writes=[bk(b)])
            evac(b, j)

    def raw_evac(dst, dkey, l, key, first):
        def prep():
            if first:
                op('dve', lambda e: e.memset(dst[:, 0:HALO], 0.0), writes=[dkey])
            else:
                op('dve', lambda e: e.tensor_copy(out=dst[:, 0:HALO], in_=halo[(l, key)][:]),
                   reads=[('halo', l, key)], writes=[dkey])

        def ev(b, j):
            op('act', lambda e: e.activation(out=dst[:, HALO + j * TT:HALO + (j + 1) * TT], in_=banks[b][:, 0:TT],
                                             func=AF.Copy), reads=[bk(b)], writes=[dkey])

        def fin():
            op('dve', lambda e: e.tensor_copy(out=halo[(l, key)][:], in_=dst[:, BLK:BLK + HALO]),
               reads=[dkey], writes=[('halo', l, key)])
        return prep, ev, fin

    def proj_raw(l, key, dst, dkey, first):
        prep, ev, fin = raw_evac(dst, dkey, l, key, first)
        prep()
        project(l, key, ev)
        fin()

    def proj_simple(l, key, dst_ap_fn, dkey, func=AF.Copy):
        def ev(b, j):
            op('act', lambda e: e.activation(out=dst_ap_fn(j), in_=banks[b][:, 0:TT], func=func),
               reads=[bk(b)], writes=[dkey])
        project(l, key, ev)

    def tok_ln(eps_col):
        for h in range(2):
            op('dve', lambda e, h=h: e.bn_stats(out=S.st6[:, h, :], in_=S.hpre[:, h, :]), reads=[S.k('hpre')], writes=[S.k('st6')])
            op('dve', lambda e, h=h: e.bn_aggr(out=S.mv[:, h, :], in_=S.st6[:, h, :]), reads=[S.k('st6')], writes=[S.k('mv')])
        op('act', lambda e: e.activation(out=S.sm[:, 0:2], in_=S.mv[:, :, 1], func=AF.Sqrt, bias=epsc[:, eps_col:eps_col + 1]),
           reads=[S.k('mv'), 'epsc'], writes=[S.k('sm')])
        op('dve', lambda e: e.reciprocal(out=S.sm[:, 2:4], in_=S.sm[:, 0:2]), reads=[S.k('sm')], writes=[S.k('sm')])
        for h in range(2):
            op('dve', lambda e, h=h: e.tensor_scalar(out=S.hn[:, h * 64:(h + 1) * 64], in0=S.hpre[:, h, :],
                                                      scalar1=S.mv[:, h, 0:1], scalar2=S.sm[:, 2 + h:3 + h],
                                                      op0=ALU.subtract, op1=ALU.mult),
               reads=[S.k('hpre'), S.k('mv'), S.k('sm')], writes=[S.k('hn')])

    def transpose_to(bi, col, src_ap, rkeys):
        op('pe', lambda e: e.transpose(out=bbf(bi)[:, col:col + 128], in_=src_ap, identity=identb[:]),
           reads=list(rkeys) + ['identb'], writes=[bk(bi)])

    def linattn_chunk(ci, qc, qkey, kz, kzkey, vT, vkey, es_fn, es_keys, Sf, Sfkey, dec_ap, dec_keys, naug,
                      post):
        cs = slice(ci * L, (ci + 1) * L)
        W = 64 + naug
        bt_ = bank()
        transpose_to(bt_, 0, vT[:, cs], [vkey])
        transpose_to(bt_, 128, kz[:, 0, cs], [kzkey])
        transpose_to(bt_, 256, kz[:, 1, cs], [kzkey])
        op('act', lambda e: e.activation(out=S.tok3[:].rearrange("p a b -> p (a b)"), in_=bbf(bt_)[:, 0:384], func=AF.Copy),
           reads=[bk(bt_)], writes=[S.k('tok3')])
        for h in range(2):
            op('dve', lambda e, h=h: e.tensor_scalar(out=S.vaug[:, h, 0:64], in0=S.tok3[:, 0, h * 64:(h + 1) * 64],
                                                      scalar1=es_fn(h), scalar2=None, op0=ALU.mult),
               reads=[S.k('tok3')] + es_keys, writes=[S.k('vaug')])
            if naug:
                op('act', lambda e, h=h: e.activation(out=S.vaug[:, h, 64:65], in_=es_fn(h), func=AF.Copy),
                   reads=es_keys, writes=[S.k('vaug')])
        if DBG <= 2:
            return
        op('dve', lambda e: e.tensor_scalar(out=Sf[:, 0:W], in0=Sf[:, 0:W], scalar1=dec_ap, scalar2=None, op0=ALU.mult),
           reads=[Sfkey] + dec_keys, writes=[Sfkey])
        for h in range(2):
            op('act', lambda e, h=h: e.activation(out=S.Sz[h * 64:(h + 1) * 64, h, 0:W], in_=Sf[h * 64:(h + 1) * 64, 0:W],
                                                  func=AF.Copy), reads=[Sfkey], writes=[S.k('Sz')])
        if DBG <= 3:
            return
        for h in range(2):
            bs = bank()
            op('pe', lambda e, h=h, bs=bs: e.matmul(banks[bs][:, 0:128], lhsT=kz[:, h, cs], rhs=qc[:, cs], start=True, stop=True),
               reads=[kzkey, qkey], writes=[bk(bs)])
            op('dve', lambda e, h=h, bs=bs: e.tensor_tensor(out=S.ATt[h][:], in0=banks[bs][:, 0:128], in1=C('mincl', 128), op=ALU.mult),
               reads=[bk(bs), 'cst'], writes=[S.k(f'AT{h}')])
        bo = bank()
        for h in range(2):
            op('pe', lambda e, h=h: e.matmul(banks[bo][:, h * 128:h * 128 + W], lhsT=S.ATt[h][:], rhs=S.vaug[:, h, 0:W], start=True, stop=False),
               reads=[S.k(f'AT{h}'), S.k('vaug')], writes=[bk(bo)])
            op('pe', lambda e, h=h: e.matmul(banks[bo][:, h * 128:h * 128 + W], lhsT=qc[:, cs], rhs=S.Sz[:, h, 0:W], start=False, stop=True),
               reads=[qkey, S.k('Sz')], writes=[bk(bo)])
        if DBG <= 4 or DBG in (41, 42):
            return
        bu = bank()
        for h in range(2):
            op('pe', lambda e, h=h: e.matmul(banks[bu][:, 0:W], lhsT=S.tok3[:, 1 + h, :], rhs=S.vaug[:, h, 0:W], start=(h == 0), stop=(h == 1)),
               reads=[S.k('tok3'), S.k('vaug')], writes=[bk(bu)])
        op('dve', lambda e: e.tensor_tensor(out=Sf[:, 0:W], in0=Sf[:, 0:W], in1=banks[bu][:, 0:W], op=ALU.add),
           reads=[Sfkey, bk(bu)], writes=[Sfkey])
        if DBG <= 5:
            return
        post(bo)

    def finish_chunk(ci, pair_idx, eps_col, fin_fn):
        tok_ln(eps_col)
        bt2 = bank()
        transpose_to(bt2, 0, S.hn[:], [S.k('hn')])
        fin_fn(bt2, slice(ci * L, (ci + 1) * L))

    def mlstm(l, first):
        onesF = Ft[11]
        MH = c.MH
        ipre, lt, Bc, gt, Gt, esr, thr = Ft[4], Ft[5], Ft[6], Ft[7], Ft[8], Ft[9], Ft[10]

        def ev_gi(b, j):
            op('act', lambda e: e.activation(out=ipre[0:MH, j * TT:(j + 1) * TT], in_=banks[b][0:MH, 0:TT], func=AF.Identity,
                                             bias=par[l][0:MH, pc['bi']:pc['bi'] + 1]), reads=[bk(b), f'par{l}'], writes=['F4'])
        project(l, ('mgi', 0), ev_gi)

        def ev_gf(b, j):
            sl = slice(j * TT, (j + 1) * TT)
            op('act', lambda e: e.activation(out=lt[0:MH, sl], in_=banks[b][0:MH, 0:TT], func=AF.Identity,
                                             bias=par[l][0:MH, pc['bf']:pc['bf'] + 1]), reads=[bk(b), f'par{l}'], writes=['F5'])
            op('act', lambda e: e.activation(out=lt[0:MH, sl], in_=lt[0:MH, sl], func=AF.Exp, scale=-1.0), reads=['F5'], writes=['F5'])
            op('act', lambda e: e.activation(out=lt[0:MH, sl], in_=lt[0:MH, sl], func=AF.Ln, bias=epsc[0:MH, 3:4]),
               reads=['F5', 'epsc'], writes=['F5'])
        project(l, ('mgf', 0), ev_gf)
        if first:
            op('dve', lambda e: e.memset(gcar[l][:, 0:1], 0.0), writes=[f'gcar{l}'])
            op('dve', lambda e: e.memset(gcar[l][:, 1:2], -1e30), writes=[f'gcar{l}'])
        op('dve', lambda e: e.memset(onesF[0:MH, 0:BLK], 1.0), writes=['F11'])
        op('dve', lambda e: e.tensor_tensor_scan(out=Bc[0:MH, 0:BLK], data0=onesF[0:MH, 0:BLK], data1=lt[0:MH, 0:BLK],
                                                 initial=gcar[l][:, 0:1], op0=ALU.mult, op1=ALU.subtract),
           reads=['F11', 'F5', f'gcar{l}'], writes=['F6'])
        op('dve', lambda e: e.tensor_tensor(out=gt[0:MH, 0:BLK], in0=ipre[0:MH, 0:BLK], in1=Bc[0:MH, 0:BLK], op=ALU.subtract),
           reads=['F4', 'F6'], writes=['F7'])
        op('dve', lambda e: e.tensor_tensor_scan(out=Gt[0:MH, 0:BLK], data0=gt[0:MH, 0:BLK], data1=gt[0:MH, 0:BLK],
                                                 initial=gcar[l][:, 1:2], op0=ALU.max, op1=ALU.max),
           reads=['F7', f'gcar{l}'], writes=['F8'])
        op('dve', lambda e: e.tensor_copy(out=Rall[:, 0:1], in_=gcar[l][:, 1:2]), reads=[f'gcar{l}'], writes=['Rall'])
        op('dve', lambda e: e.tensor_copy(out=Rall[:, 1:NCH + 1], in_=Gt[0:MH, L - 1:BLK:L]), reads=['F8'], writes=['Rall'])
        op('dve', lambda e: e.tensor_copy(out=gcar[l][:, 0:1], in_=Bc[0:MH, BLK - 1:BLK]), reads=['F6'], writes=[f'gcar{l}'])
        op('dve', lambda e: e.tensor_copy(out=gcar[l][:, 1:2], in_=Gt[0:MH, BLK - 1:BLK]), reads=['F8'], writes=[f'gcar{l}'])
        op('dve', lambda e: e.tensor_tensor(out=decrow[:], in0=Rall[:, 0:NCH], in1=Rall[:, 1:NCH + 1], op=ALU.subtract),
           reads=['Rall'], writes=['decrow'])
        op('act', lambda e: e.activation(out=decrow[:], in_=decrow[:], func=AF.Exp), reads=['decrow'], writes=['decrow'])
        rcb = Rall[:, 1:NCH + 1].unsqueeze(2).to_broadcast([MH, NCH, L])
        op('dve', lambda e: e.tensor_tensor(out=esr[0:MH, 0:BLK].rearrange("p (a b) -> p a b", b=L),
                                            in0=gt[0:MH, 0:BLK].rearrange("p (a b) -> p a b", b=L), in1=rcb, op=ALU.subtract),
           reads=['F7', 'Rall'], writes=['F9'])
        op('act', lambda e: e.activation(out=esr[0:MH, 0:BLK], in_=esr[0:MH, 0:BLK], func=AF.Exp), reads=['F9'], writes=['F9'])
        op('dve', lambda e: e.tensor_tensor(out=thr[0:MH, 0:BLK].rearrange("p (a b) -> p a b", b=L),
                                            in0=Bc[0:MH, 0:BLK].rearrange("p (a b) -> p a b", b=L), in1=rcb, op=ALU.add),
           reads=['F6', 'Rall'], writes=['F10'])
        op('act', lambda e: e.activation(out=thr[0:MH, 0:BLK], in_=thr[0:MH, 0:BLK], func=AF.Exp, scale=-1.0), reads=['F10'], writes=['F10'])
        for src, skey, dst, dkey in ((esr, 'F9', est, 'est'), (thr, 'F10', thrt, 'thrt')):
            b = bank()
            for ci in range(NCH):
                op('pe', lambda e, ci=ci, src=src, b=b: e.transpose(out=banks[b][:, ci * MH:(ci + 1) * MH], in_=src[0:MH, ci * L:(ci + 1) * L],
                                                                     identity=C('ident', MH)[0:MH, :]),
                   reads=[skey, 'cst'], writes=[bk(b)])
            op('act', lambda e, b=b, dst=dst: e.activation(out=dst[:].rearrange("p a b -> p (a b)"), in_=banks[b][:, 0:NCH * MH], func=AF.Copy),
               reads=[bk(b)], writes=[dkey])
        for p in range(c.MP):
            praw_q, praw_k, acc = Ft[0], Ft[1], Ft[3]
            qc, vT_, sgo = Bt[0], Bt[3], Bt[4]
            kz = kzt_view[0]
            b = bank()
            op('pe', lambda e, b=b, p=p: e.matmul(banks[b][:, 0:NCH], lhsT=C('sel', 128, p * 128)[0:MH, :], rhs=decrow[:], start=True, stop=True),
               reads=['cst', 'decrow'], writes=[bk(b)])
            op('act', lambda e, b=b: e.activation(out=decb[:], in_=banks[b][:, 0:NCH], func=AF.Copy), reads=[bk(b)], writes=['decb'])
            for nm, praw, pk, cname in (('mq', praw_q, 'F0', 'cq'), ('mk', praw_k, 'F1', 'ck')):
                proj_raw(l, (nm, p), praw, pk, first)
                cw = pc[(cname, p)]
                op('dve', lambda e, praw=praw, cw=cw: e.tensor_scalar(out=acc[:, 0:BLK], in0=praw[:, HALO - 3:HALO - 3 + BLK],
                                                                     scalar1=par[l][:, cw:cw + 1], scalar2=None, op0=ALU.mult),
                   reads=[pk, f'par{l}'], writes=['F3'])
                for jj in range(1, 4):
                    op('dve', lambda e, praw=praw, cw=cw, jj=jj: e.scalar_tensor_tensor(
                        out=acc[:, 0:BLK], in0=praw[:, HALO - 3 + jj:HALO - 3 + jj + BLK], scalar=par[l][:, cw + jj:cw + jj + 1],
                        in1=acc[:, 0:BLK], op0=ALU.mult, op1=ALU.add), reads=[pk, f'par{l}', 'F3'], writes=['F3'])
                if nm == 'mq':
                    op('act', lambda e: e.activation(out=qc[:], in_=acc[:, 0:BLK], func=AF.Silu), reads=['F3'], writes=['B0'])
                else:
                    op('act', lambda e: e.activation(out=acc[:, 0:BLK], in_=acc[:, 0:BLK], func=AF.Silu), reads=['F3'], writes=['F3'])
                    for h in range(2):
                        op('dve', lambda e, h=h: e.tensor_scalar(out=kz[h * 64:(h + 1) * 64, h, :], in0=acc[h * 64:(h + 1) * 64, 0:BLK],
                                                                  scalar1=0.125, scalar2=None, op0=ALU.mult),
                           reads=['F3'], writes=['BZ'])
            proj_simple(l, ('mv', p), lambda j: vT_[:, j * TT:(j + 1) * TT], 'B3')
            proj_simple(l, ('mo', p), lambda j: sgo[:, j * TT:(j + 1) * TT], 'B4', func=AF.Sigmoid)
            if first:
                op('dve', lambda e, p=p: e.memset(Cf[l][p][:], 0.0), writes=[f'Cf{l}_{p}'])
            for ci in range(NCH):
                def post(bo, ci=ci, p=p):
                    op('act', lambda e: e.activation(out=S.ndsb[:, :, 0:65], in_=banks[bo][:, 0:256].rearrange("p (a b) -> p a b", a=2)[:, :, 0:65], func=AF.Copy),
                       reads=[bk(bo)], writes=[S.k('ndsb')])
                    op('act', lambda e: e.activation(out=S.sm[:, 4:6], in_=S.ndsb[:, :, 64], func=AF.Abs), reads=[S.k('ndsb')], writes=[S.k('sm')])
                    op('dve', lambda e: e.tensor_tensor(out=S.sm[:, 4:6], in0=S.sm[:, 4:6], in1=thrt[:, ci, 2 * p:2 * p + 2], op=ALU.max),
                       reads=[S.k('sm'), 'thrt'], writes=[S.k('sm')])
                    op('dve', lambda e: e.reciprocal(out=S.sm[:, 6:8], in_=S.sm[:, 4:6]), reads=[S.k('sm')], writes=[S.k('sm')])
                    for h in range(2):
                        op('dve', lambda e, h=h: e.tensor_scalar(out=S.hpre[:, h, :], in0=S.ndsb[:, h, 0:64],
                                                                  scalar1=S.sm[:, 6 + h:7 + h], scalar2=None, op0=ALU.mult),
                           reads=[S.k('ndsb'), S.k('sm')], writes=[S.k('hpre')])

                    def fin(bt2, cs):
                        op('dve', lambda e: e.scalar_tensor_tensor(out=yT[:, p, cs], in0=bbf(bt2)[:, 0:128],
                                                                   scalar=par[l][:, pc[('mlg', p)]:pc[('mlg', p)] + 1],
                                                                   in1=sgo[:, cs], op0=ALU.mult, op1=ALU.mult),
                           reads=[bk(bt2), f'par{l}', 'B4'], writes=[('yT', p)])
                    finish_chunk(ci, p, 1, fin)
                linattn_chunk(ci, qc, 'B0', kz, 'BZ', vT_, 'B3',
                              lambda h, ci=ci, p=p: est[:, ci, 2 * p + h:2 * p + h + 1], ['est'],
                              Cf[l][p], f'Cf{l}_{p}', decb[:, ci:ci + 1], ['decb'], 1, post)

    kzt_view = [None]

    def retention(l, first, blk):
        cosT, sinT = Ft[11], Ft[10]
        pos = slice(blk * BLK, (blk + 1) * BLK)
        dma('sp', cosT[:, 0:BLK], rot_d[0][:, pos], writes=['F11'])
        dma('sp', sinT[:, 0:BLK], rot_d[1][:, pos], writes=['F10'])
        kz = kzt_view[0]
        for p in range(c.RP):
            t1, t2 = Ft[0], Ft[1]
            qr, vT_, sg = Bt[0], Bt[3], Bt[4]
            for nm in ('rq', 'rk'):
                def ev1(b, j):
                    sl = slice(j * TT, (j + 1) * TT)
                    op('dve', lambda e: e.tensor_tensor(out=t1[:, sl], in0=banks[b][:, 0:TT], in1=cosT[:, sl], op=ALU.mult),
                       reads=[bk(b), 'F11'], writes=['F0'])
                project(l, (nm, p), ev1)

                def ev2(b, j):
                    sl = slice(j * TT, (j + 1) * TT)
                    op('dve', lambda e: e.tensor_tensor(out=t2[:, sl], in0=banks[b][:, 0:TT], in1=sinT[:, sl], op=ALU.mult),
                       reads=[bk(b), 'F10'], writes=['F1'])
                project(l, (nm + 's', p), ev2)
                if nm == 'rq':
                    op('dve', lambda e: e.tensor_tensor(out=qr[:], in0=t1[:, 0:BLK], in1=t2[:, 0:BLK], op=ALU.add),
                       reads=['F0', 'F1'], writes=['B0'])
                else:
                    op('dve', lambda e: e.tensor_tensor(out=t1[:, 0:BLK], in0=t1[:, 0:BLK], in1=t2[:, 0:BLK], op=ALU.add),
                       reads=['F0', 'F1'], writes=['F0'])
                    for h in range(2):
                        op('dve', lambda e, h=h: e.tensor_scalar(out=kz[h * 64:(h + 1) * 64, h, :], in0=t1[h * 64:(h + 1) * 64, 0:BLK],
                                                                  scalar1=0.125, scalar2=None, op0=ALU.mult),
                           reads=['F0'], writes=['BZ'])
            proj_simple(l, ('rv', p), lambda j: vT_[:, j * TT:(j + 1) * TT], 'B3')
            proj_simple(l, ('rg', p), lambda j: sg[:, j * TT:(j + 1) * TT], 'B4', func=AF.Silu)
            if first:
                op('dve', lambda e, p=p: e.memset(Rf[l][p][:], 0.0), writes=[f'Rf{l}_{p}'])
            for ci in range(NCH if DBG > 1 else 0):
                def post(bo, ci=ci, p=p):
                    for h in range(2):
                        op('dve', lambda e, h=h: e.tensor_scalar(out=S.hpre[:, h, :], in0=banks[bo][:, h * 128:h * 128 + 64],
                                                                  scalar1=C('ft_r', 1, 2 * p + h), scalar2=None, op0=ALU.mult),
                           reads=[bk(bo), 'cst'], writes=[S.k('hpre')])

                    def fin(bt2, cs):
                        op('dve', lambda e: e.tensor_tensor(out=yT[:, c.MP + p, cs], in0=bbf(bt2)[:, 0:128], in1=sg[:, cs], op=ALU.mult),
                           reads=[bk(bt2), 'B4'], writes=[('yT', c.MP + p)])
                    finish_chunk(ci, c.MP + p, 1, fin)
                linattn_chunk(ci, qr, 'B0', kz, 'BZ', vT_, 'B3',
                              lambda h, p=p: C('es_r', 1, 2 * p + h), ['cst'],
                              Rf[l][p], f'Rf{l}_{p}', C('dec_r', 1, p), ['cst'], 0, post)

    def rwkv(l, first):
        tw, alb, sgl = Bt[9], Bt[10], Bt[11]
        tmp = Ft[3]

        def shifted(key, dst, dkey):
            proj_raw(l, key, dst, dkey, first)
            mcol = pc[('mu', key)]
            op('dve', lambda e: e.tensor_tensor(out=tmp[:, 0:BLK], in0=dst[:, HALO - 1:HALO - 1 + BLK], in1=dst[:, HALO:HALO + BLK], op=ALU.subtract),
               reads=[dkey], writes=['F3'])
            op('dve', lambda e: e.scalar_tensor_tensor(out=dst[:, HALO:HALO + BLK], in0=tmp[:, 0:BLK], scalar=par[l][:, mcol:mcol + 1],
                                                       in1=dst[:, HALO:HALO + BLK], op0=ALU.mult, op1=ALU.add),
               reads=['F3', dkey, f'par{l}'], writes=[dkey])
        shifted(('wwl', 0), Ft[0], 'F0')
        op('act', lambda e: e.activation(out=tw[:], in_=Ft[0][:, HALO:HALO + BLK], func=AF.Tanh), reads=['F0'], writes=['B9'])
        shifted(('wal', 0), Ft[0], 'F0')
        op('act', lambda e: e.activation(out=alb[:], in_=Ft[0][:, HALO:HALO + BLK], func=AF.Copy), reads=['F0'], writes=['B10'])
        shifted(('wgl', 0), Ft[0], 'F0')
        op('act', lambda e: e.activation(out=sgl[:], in_=Ft[0][:, HALO:HALO + BLK], func=AF.Sigmoid), reads=['F0'], writes=['B11'])
        for p in range(c.WP):
            ka = pc[('ka', p)]
            op('dve', lambda e, p=p, ka=ka: e.tensor_scalar(out=omka[:, p:p + 1], in0=par[l][:, ka:ka + 1], scalar1=-1.0, scalar2=1.0,
                                                             op0=ALU.mult, op1=ALU.add), reads=[f'par{l}'], writes=['omka'])
        def pair_body(p):
            rs, ks, vs = Ft[0], Ft[1], Ft[2]
            lw, cl, at, kap, kmod, ak, Et, gT, bv = Ft[4], Ft[5], Ft[6], Ft[7], Ft[8], Ft[9], Ft[10], Ft[11], Ft[2]
            vTb, bh, kh = Bt[0], Bt[3], Bt[4]
            AR = ARv[0]
            BZ = BZv[0]
            KZ = KZv[0]
            H0 = slice(HALO, HALO + BLK)
            if p == 0:
                shifted(('wr', p), rs, 'F0')
                shifted(('wk', p), ks, 'F1')
            shifted(('wv', p), vs, 'F2')
            op('act', lambda e: e.activation(out=vTb[:], in_=vs[:, H0], func=AF.Copy), reads=['F2'], writes=['B0'])
            cols = slice(p * 128, (p + 1) * 128)
            for j in range(NT):
                sl = slice(j * TT, (j + 1) * TT)
                b1 = bank()
                op('pe', lambda e, b1=b1, sl=sl: e.matmul(banks[b1][:, 0:TT], lhsT=lorab[l][:, 0, cols], rhs=tw[:, sl], start=True, stop=True),
                   reads=[f'lora{l}', 'B9'], writes=[bk(b1)])
                op('act', lambda e, b1=b1, sl=sl: e.activation(out=lw[:, sl], in_=banks[b1][:, 0:TT], func=AF.Sigmoid,
                                                               bias=P(l, ('w0', p))), reads=[bk(b1), f'par{l}'], writes=['F4'])
                b2 = bank()
                op('pe', lambda e, b2=b2, sl=sl: e.matmul(banks[b2][:, 0:TT], lhsT=lorab[l][:, 1, cols], rhs=alb[:, sl], start=True, stop=True),
                   reads=[f'lora{l}', 'B10'], writes=[bk(b2)])
                op('act', lambda e, b2=b2, sl=sl: e.activation(out=at[:, sl], in_=banks[b2][:, 0:TT], func=AF.Sigmoid,
                                                               bias=P(l, ('a0', p))), reads=[bk(b2), f'par{l}'], writes=['F6'])
                b3 = bank()
                op('pe', lambda e, b3=b3, sl=sl: e.matmul(banks[b3][:, 0:TT], lhsT=lorab[l][:, 2, cols], rhs=sgl[:, sl], start=True, stop=True),
                   reads=[f'lora{l}', 'B11'], writes=[bk(b3)])
                op('act', lambda e, b3=b3, sl=sl: e.activation(out=gT[:, sl], in_=banks[b3][:, 0:TT], func=AF.Copy), reads=[bk(b3)], writes=['F11'])
            op('dve', lambda e: e.tensor_scalar(out=lw[:, 0:BLK], in0=lw[:, 0:BLK], scalar1=-math.exp(-0.5), scalar2=None, op0=ALU.mult),
               reads=['F4'], writes=['F4'])
            op('dve', lambda e: e.tensor_scalar(out=kap[:, 0:BLK], in0=ks[:, H0], scalar1=P(l, ('kk', p)), scalar2=None, op0=ALU.mult),
               reads=['F1', f'par{l}'], writes=['F7'])
            op('act', lambda e: e.activation(out=bh[:], in_=kap[:, 0:BLK], func=AF.Square), reads=['F7'], writes=['B3'])
            for j in range(NT):
                sl = slice(j * TT, (j + 1) * TT)
                b1 = bank()
                op('pe', lambda e, b1=b1, sl=sl: e.matmul(banks[b1][:, 0:TT], lhsT=bonesb[:], rhs=bh[:, sl], start=True, stop=True),
                   reads=['bonesb', 'B3'], writes=[bk(b1)])
                op('act', lambda e, b1=b1, sl=sl: e.activation(out=tmp[:, sl], in_=banks[b1][:, 0:TT], func=AF.Sqrt), reads=[bk(b1)], writes=['F3'])
            op('dve', lambda e: e.tensor_scalar(out=tmp[:, 0:BLK], in0=tmp[:, 0:BLK], scalar1=1e-12, scalar2=None, op0=ALU.max), reads=['F3'], writes=['F3'])
            op('dve', lambda e: e.reciprocal(out=tmp[:, 0:BLK], in_=tmp[:, 0:BLK]), reads=['F3'], writes=['F3'])
            op('dve', lambda e: e.tensor_tensor(out=kap[:, 0:BLK], in0=kap[:, 0:BLK], in1=tmp[:, 0:BLK], op=ALU.mult), reads=['F7', 'F3'], writes=['F7'])
            op('dve', lambda e: e.tensor_scalar(out=tmp[:, 0:BLK], in0=at[:, 0:BLK], scalar1=P(l, ('ka', p)), scalar2=omka[:, p:p + 1],
                                                op0=ALU.mult, op1=ALU.add), reads=['F6', f'par{l}', 'omka'], writes=['F3'])
            op('dve', lambda e: e.tensor_tensor(out=kmod[:, 0:BLK], in0=ks[:, H0], in1=tmp[:, 0:BLK], op=ALU.mult), reads=['F1', 'F3'], writes=['F8'])
            op('dve', lambda e: e.scalar_tensor_tensor(out=kh[:], in0=rs[:, H0], scalar=P(l, ('rk', p)), in1=kmod[:, 0:BLK],
                                                       op0=ALU.mult, op1=ALU.mult), reads=['F0', 'F8', f'par{l}'], writes=['B4'])
            for j in range(NT):
                sl = slice(j * TT, (j + 1) * TT)
                b1 = bank()
                op('pe', lambda e, b1=b1, sl=sl: e.matmul(banks[b1][:, 0:TT], lhsT=bonesb[:], rhs=kh[:, sl], start=True, stop=True),
                   reads=['bonesb', 'B4'], writes=[bk(b1)])
                op('dve', lambda e, b1=b1, j=j: e.tensor_tensor(out=bv[:, HALO + j * TT:HALO + (j + 1) * TT], in0=banks[b1][:, 0:TT], in1=vs[:, HALO + j * TT:HALO + (j + 1) * TT], op=ALU.mult),
                   reads=[bk(b1), 'F2'], writes=['F2'])
            op('dve', lambda e: e.tensor_tensor_scan(out=cl[:, 0:BLK], data0=resetm[:], data1=lw[:, 0:BLK], initial=0.0,
                                                     op0=ALU.mult, op1=ALU.add), reads=['resetm', 'F4'], writes=['F5'])
            op('dve', lambda e: e.tensor_tensor(out=ak[:, 0:BLK], in0=at[:, 0:BLK], in1=kap[:, 0:BLK], op=ALU.mult), reads=['F6', 'F7'], writes=['F9'])
            op('dve', lambda e: e.tensor_tensor(out=tmp[:, 0:BLK], in0=cl[:, 0:BLK], in1=lw[:, 0:BLK], op=ALU.subtract), reads=['F5', 'F4'], writes=['F3'])
            op('act', lambda e: e.activation(out=Et[:, 0:BLK], in_=tmp[:, 0:BLK], func=AF.Exp), reads=['F3'], writes=['F10'])
            op('dve', lambda e: e.scalar_tensor_tensor(out=AR[:, :, 0, :], in0=kap[:, 0:BLK].rearrange("p (a b) -> p a b", b=L), scalar=-1.0,
                                                       in1=Et[:, 0:BLK].rearrange("p (a b) -> p a b", b=L), op0=ALU.mult, op1=ALU.mult),
               reads=['F7', 'F10'], writes=['AR'])
            op('act', lambda e: e.activation(out=Et[:, 0:BLK], in_=cl[:, 0:BLK], func=AF.Exp), reads=['F5'], writes=['F10'])
            op('dve', lambda e: e.tensor_tensor(out=AR[:, :, 1, :], in0=rs[:, H0].rearrange("p (a b) -> p a b", b=L),
                                                in1=Et[:, 0:BLK].rearrange("p (a b) -> p a b", b=L), op=ALU.mult),
               reads=['F0', 'F10'], writes=['AR'])
            op('dve', lambda e: e.tensor_copy(out=WLt[:], in_=Et[:, L - 1:BLK:L]), reads=['F10'], writes=['WLt'])
            op('act', lambda e: e.activation(out=Et[:, 0:BLK], in_=cl[:, 0:BLK], func=AF.Exp, scale=-1.0), reads=['F5'], writes=['F10'])
            for h in range(2):
                hs = slice(h * 64, (h + 1) * 64)
                op('dve', lambda e, h=h, hs=hs: e.tensor_tensor(out=BZ[hs, h, :], in0=ak[hs, 0:BLK], in1=Et[hs, 0:BLK], op=ALU.mult),
                   reads=['F9', 'F10'], writes=['BZ'])
                op('dve', lambda e, h=h, hs=hs: e.tensor_tensor(out=KZ[hs, h, :], in0=kmod[hs, 0:BLK], in1=Et[hs, 0:BLK], op=ALU.mult),
                   reads=['F8', 'F10'], writes=['KZ'])
            clL = cl[:, L - 1:BLK:L].unsqueeze(2).to_broadcast([128, NCH, L])
            op('dve', lambda e: e.tensor_tensor(out=tmp[:, 0:BLK].rearrange("p (a b) -> p a b", b=L), in0=clL,
                                                in1=cl[:, 0:BLK].rearrange("p (a b) -> p a b", b=L), op=ALU.subtract), reads=['F5'], writes=['F3'])
            op('act', lambda e: e.activation(out=Et[:, 0:BLK], in_=tmp[:, 0:BLK], func=AF.Exp), reads=['F3'], writes=['F10'])
            op('dve', lambda e: e.tensor_tensor(out=bh[:], in0=ak[:, 0:BLK], in1=Et[:, 0:BLK], op=ALU.mult), reads=['F9', 'F10'], writes=['B3'])
            op('dve', lambda e: e.tensor_tensor(out=kh[:], in0=kmod[:, 0:BLK], in1=Et[:, 0:BLK], op=ALU.mult), reads=['F8', 'F10'], writes=['B4'])
            if first:
                op('dve', lambda e, p=p: e.memset(Mf[l][p][:], 0.0), writes=[f'Mf{l}_{p}'])
            Mfp, Mkey = Mf[l][p], f'Mf{l}_{p}'
            def chunk_body(ci):
                par = ci % 2
                S.set(par)
                cs = slice(ci * L, (ci + 1) * L)
                bt_ = bank()
                transpose_to(bt_, 0, vTb[:, cs], ['B0'])
                transpose_to(bt_, 128, bh[:, cs], ['B3'])
                transpose_to(bt_, 256, kh[:, cs], ['B4'])
                op('act', lambda e: e.activation(out=S.tok3[:].rearrange("p a b -> p (a b)"), in_=bbf(bt_)[:, 0:384], func=AF.Copy),
                   reads=[bk(bt_)], writes=[S.k('tok3')])
                yield
                S.set(par)
                ARc = AR[:, ci, :, :].rearrange("p a b -> p (a b)")
                bn = bank()
                for h in range(2):
                    bs = bank()
                    op('pe', lambda e, h=h, bs=bs: e.matmul(banks[bs][:, 0:256], lhsT=BZ[:, h, cs], rhs=ARc, start=True, stop=True),
                       reads=['BZ', 'AR'], writes=[bk(bs)])
                    op('pe', lambda e, h=h, bs=bs: e.matmul(banks[bs][:, 256:512], lhsT=KZ[:, h, cs], rhs=ARc, start=True, stop=True),
                       reads=['KZ', 'AR'], writes=[bk(bs)])
                    op('dve', lambda e, h=h, bs=bs: e.tensor_tensor(out=S.SC2[:, h, :, :].rearrange("p a b -> p (a b)"), in0=banks[bs][:, 0:512],
                                                                     in1=mask4[:].rearrange("p a b -> p (a b)"), op=ALU.mult),
                       reads=[bk(bs), 'mask4'], writes=[S.k('SC2')])
                    op('pe', lambda e, h=h: e.matmul(banks[bn][:, h * 128:(h + 1) * 128], lhsT=AR[:, ci, 0, :], rhs=BZ[:, h, cs], start=True, stop=True),
                       reads=['AR', 'BZ'], writes=[bk(bn)])
                op('dve', lambda e: e.tensor_tensor(out=S.Q0t[:].rearrange("p a b -> p (a b)"), in0=banks[bn][:, 0:256],
                                                    in1=mN2[:].rearrange("p a b -> p (a b)"), op=ALU.mult),
                   reads=[bk(bn), 'mN2'], writes=[S.k('Q0t')])
                op('dve', lambda e: e.tensor_tensor(out=S.Tb2[0][:], in0=S.SC2[:, :, 0, :], in1=id2[:], op=ALU.add),
                   reads=[S.k('SC2'), 'id2'], writes=[S.k('Tb2_0')])
                yield
                S.set(par)
                NLEV = 7
                tcur = 0
                for lev in range(NLEV - 1):
                    nxt = lev % 2
                    bp = bank()
                    for h in range(2):
                        if lev == 0:
                            pc_, pk_ = S.SC2[:, h, 0, :], S.k('SC2')
                            qc_, qk_ = S.Q0t[:, h, :], S.k('Q0t')
                        else:
                            pc_, pk_ = S.PQ[1 - nxt][:, h, 0, :], S.k(f'PQ{1 - nxt}')
                            qc_, qk_ = S.PQ[1 - nxt][:, h, 1, :], S.k(f'PQ{1 - nxt}')
                        if lev < NLEV - 2:
                            op('pe', lambda e, h=h, bp=bp, pc_=pc_, qc_=qc_: e.matmul(banks[bp][:, h * 256:h * 256 + 128], lhsT=qc_, rhs=pc_, start=True, stop=True),
                               reads=[pk_, qk_], writes=[bk(bp)])
                        op('pe', lambda e, h=h, bp=bp, pc_=pc_, qc_=qc_: e.matmul(banks[bp][:, h * 256 + 128:h * 256 + 256], lhsT=pc_, rhs=qc_, start=True, stop=True),
                           reads=[pk_, qk_], writes=[bk(bp)])
                    if lev < NLEV - 2:
                        op('act', lambda e, bp=bp, nxt=nxt: e.activation(out=S.PQ[nxt][:].rearrange("p a b c -> p (a b c)"), in_=banks[bp][:, 0:512], func=AF.Copy),
                           reads=[bk(bp)], writes=[S.k(f'PQ{nxt}')])
                    else:
                        op('act', lambda e, bp=bp, nxt=nxt: e.activation(out=S.PQ[nxt][:, :, 1, :], in_=banks[bp][:, 0:512].rearrange("p (a b c) -> p a b c", a=2, b=2)[:, :, 1, :], func=AF.Copy),
                           reads=[bk(bp)], writes=[S.k(f'PQ{nxt}')])
                    yield
                    S.set(par)
                    bt3 = bank()
                    for h in range(2):
                        op('pe', lambda e, h=h, bt3=bt3, nxt=nxt, tcur=tcur: e.matmul(banks[bt3][:, h * 128:(h + 1) * 128], lhsT=S.PQ[nxt][:, h, 1, :],
                                                                                 rhs=S.Tb2[tcur][:, h, :], start=True, stop=True),
                           reads=[S.k(f'PQ{nxt}'), S.k(f'Tb2_{tcur}')], writes=[bk(bt3)])
                    op('dve', lambda e, bt3=bt3, tcur=tcur: e.tensor_tensor(out=S.Tb2[1 - tcur][:].rearrange("p a b -> p (a b)"),
                                                                           in0=S.Tb2[tcur][:].rearrange("p a b -> p (a b)"), in1=banks[bt3][:, 0:256], op=ALU.add),
                       reads=[S.k(f'Tb2_{tcur}'), bk(bt3)], writes=[S.k(f'Tb2_{1 - tcur}')])
                    tcur = 1 - tcur
                    yield
                    S.set(par)
                tfin = tcur
                for h in range(2):
                    op('act', lambda e, h=h: e.activation(out=S.Sz[h * 64:(h + 1) * 64, h, 0:64], in_=Mfp[h * 64:(h + 1) * 64, :], func=AF.Copy),
                       reads=[Mkey], writes=[S.k('Sz')])
                bx = bank()
                for h in range(2):
                    op('pe', lambda e, h=h: e.matmul(banks[bx][:, h * 64:(h + 1) * 64], lhsT=AR[:, ci, 0, :], rhs=S.Sz[:, h, 0:64], start=True, stop=False),
                       reads=['AR', S.k('Sz')], writes=[bk(bx)])
                    op('pe', lambda e, h=h: e.matmul(banks[bx][:, h * 64:(h + 1) * 64], lhsT=S.SC2[:, h, 2, :], rhs=S.tok3[:, 0, h * 64:(h + 1) * 64], start=False, stop=True),
                       reads=[S.k('SC2'), S.k('tok3')], writes=[bk(bx)])
                op('act', lambda e: e.activation(out=S.XU[:, 0, :, :].rearrange("p a b -> p (a b)"), in_=banks[bx][:, 0:128], func=AF.Copy), reads=[bk(bx)], writes=[S.k('XU')])
                yield
                S.set(par)
                bu_ = bank()
                for h in range(2):
                    op('pe', lambda e, h=h: e.matmul(banks[bu_][:, h * 64:(h + 1) * 64], lhsT=S.Tb2[tfin][:, h, :], rhs=S.XU[:, 0, h, :], start=True, stop=True),
                       reads=[S.k(f'Tb2_{tfin}'), S.k('XU')], writes=[bk(bu_)])
                op('act', lambda e: e.activation(out=S.XU[:, 1, :, :].rearrange("p a b -> p (a b)"), in_=banks[bu_][:, 0:128], func=AF.Copy), reads=[bk(bu_)], writes=[S.k('XU')])
                yield
                S.set(par)
                by = bank()
                for h in range(2):
                    op('pe', lambda e, h=h: e.matmul(banks[by][:, h * 64:(h + 1) * 64], lhsT=AR[:, ci, 1, :], rhs=S.Sz[:, h, 0:64], start=True, stop=False),
                       reads=['AR', S.k('Sz')], writes=[bk(by)])
                    op('pe', lambda e, h=h: e.matmul(banks[by][:, h * 64:(h + 1) * 64], lhsT=S.SC2[:, h, 1, :], rhs=S.XU[:, 1, h, :], start=False, stop=False),
                       reads=[S.k('SC2'), S.k('XU')], writes=[bk(by)])
                    op('pe', lambda e, h=h: e.matmul(banks[by][:, h * 64:(h + 1) * 64], lhsT=S.SC2[:, h, 3, :], rhs=S.tok3[:, 0, h * 64:(h + 1) * 64], start=False, stop=True),
                       reads=[S.k('SC2'), S.k('tok3')], writes=[bk(by)])
                op('act', lambda e: e.activation(out=S.hpre[:].rearrange("p a b -> p (a b)"), in_=banks[by][:, 0:128], func=AF.Copy), reads=[bk(by)], writes=[S.k('hpre')])
                yield
                S.set(par)
                bm = bank()
                op('pe', lambda e: e.matmul(banks[bm][:, 0:128], lhsT=S.tok3[:, 1, :], rhs=S.XU[:, 1, :, :].rearrange("p a b -> p (a b)"), start=True, stop=False),
                   reads=[S.k('tok3'), S.k('XU')], writes=[bk(bm)])
                op('pe', lambda e: e.matmul(banks[bm][:, 0:128], lhsT=S.tok3[:, 2, :], rhs=S.tok3[:, 0, :], start=False, stop=True),
                   reads=[S.k('tok3')], writes=[bk(bm)])
                for h in range(2):
                    hs = slice(h * 64, (h + 1) * 64)
                    op('dve', lambda e, h=h, hs=hs: e.scalar_tensor_tensor(out=Mfp[hs, :], in0=Mfp[hs, :], scalar=WLt[hs, ci:ci + 1],
                                                                           in1=banks[bm][hs, h * 64:(h + 1) * 64], op0=ALU.mult, op1=ALU.add),
                       reads=[Mkey, 'WLt', bk(bm)], writes=[Mkey])

                def fin(bt2, cs):
                    op('dve', lambda e: e.tensor_scalar(out=S.ytmp[:], in0=bbf(bt2)[:, 0:128], scalar1=P(l, ('lg', p)), scalar2=P(l, ('lb', p)),
                                                        op0=ALU.mult, op1=ALU.add), reads=[bk(bt2), f'par{l}'], writes=[S.k('ytmp')])
                    op('dve', lambda e: e.tensor_tensor(out=S.ytmp[:], in0=S.ytmp[:], in1=bv[:, HALO + cs.start:HALO + cs.stop], op=ALU.add), reads=[S.k('ytmp'), 'F2'], writes=[S.k('ytmp')])
                    op('dve', lambda e: e.tensor_tensor(out=yT[:, c.MP + c.RP + p, cs], in0=S.ytmp[:], in1=gT[:, cs], op=ALU.mult),
                       reads=[S.k('ytmp'), 'F11'], writes=[('yT', c.MP + c.RP + p)])
                finish_chunk(ci, c.MP + c.RP + p, 2, fin)

            if p + 1 < c.WP:
                shifted(('wr', p + 1), rs, 'F0')
                shifted(('wk', p + 1), ks, 'F1')
            gens = [chunk_body(ci) for ci in range(NCH)]
            fin_ = [False] * NCH
            OFF = PIPE_OFF
            t_ = 0
            while not all(fin_):
                for ci in range(NCH):
                    if ci * OFF <= t_ and not fin_[ci]:
                        try:
                            next(gens[ci])
                        except StopIteration:
                            fin_[ci] = True
                t_ += 1
            S.set(0)

        for p in range(c.WP):
            pair_body(p)

    ARv = [k.sb([128, NCH, 2, L], BF16, "AR")]
    BZv = [k.sb([128, 2, BLK], BF16, "BZ")]
    kzt_view[0] = BZv[0]
    KZv = [k.sb([128, 2, BLK], BF16, "KZ")]
    resetm = k.sb([128, BLK], BF16, "resetm")
    op('dve', lambda e: e.memset(resetm[:], 1.0), writes=['resetm'])
    op('dve', lambda e: e.memset(resetm[:, 0:BLK:L], 0.0), writes=['resetm'])
    op('dve', lambda e: e.memset(BZv[0][:], 0.0), writes=['BZ'])
    op('dve', lambda e: e.memset(KZv[0][:], 0.0), writes=['KZ'])
    for _p in range(2):
        S.set(_p)
        op('dve', lambda e: e.memset(S.Sz[:], 0.0), writes=[S.k('Sz')])
    S.set(0)

    def wout_ffn(l):
        for o in range(KD):
            si = load_slab(wout_d[l, o], KM * 128)
            for j in range(NT):
                ts = slice(j * TT, (j + 1) * TT)
                b = bank()
                for kk in range(KM):
                    op('pe', lambda e, b=b, kk=kk, ts=ts, si=si: e.matmul(banks[b][:, 0:TT], lhsT=slabs[si][:, kk * 128:(kk + 1) * 128],
                                                                          rhs=yT[:, kk, ts], start=(kk == 0), stop=(kk == KM - 1)),
                       reads=[f'slab{si}', ('yT', kk)], writes=[bk(b)])
                op('dve', lambda e, b=b, o=o, ts=ts: e.tensor_tensor(out=xT[:, o, ts], in0=xT[:, o, ts], in1=banks[b][:, 0:TT], op=ALU.add),
                   reads=[bk(b), ('xT', j)], writes=[('xT', j)])
        rmsnorm(l, 'ln2')
        def aT(f):
            t = Ft[f // 2]
            v = t[:].bitcast(BF16)
            return v[:, (f % 2) * BLK:(f % 2) * BLK + BLK], f'F{f // 2}'
        sgt = Bt[0]
        for f in range(NF):
            si = load_slab(wgu_d[l, f], 2 * KD * 128)
            av, akey = aT(f)
            for j in range(NT):
                ts = slice(j * TT, (j + 1) * TT)
                bg, bu = bank(), bank()
                for gu, b in ((0, bg), (1, bu)):
                    for kk in range(KD):
                        off = (gu * KD + kk) * 128
                        op('pe', lambda e, b=b, kk=kk, ts=ts, si=si, off=off: e.matmul(banks[b][:, 0:TT], lhsT=slabs[si][:, off:off + 128],
                                                                                       rhs=hT[:, kk, ts], start=(kk == 0), stop=(kk == KD - 1)),
                           reads=[f'slab{si}', ('hT', j)], writes=[bk(b)])
                op('act', lambda e, bg=bg, ts=ts: e.activation(out=sgt[:, ts], in_=banks[bg][:, 0:TT], func=AF.Silu), reads=[bk(bg)], writes=['B0'])
                op('dve', lambda e, bu=bu, ts=ts, av=av: e.tensor_tensor(out=av[:, ts], in0=sgt[:, ts], in1=banks[bu][:, 0:TT], op=ALU.mult),
                   reads=['B0', bk(bu)], writes=[akey])
        for o in range(KD):
            sis = [load_slab(wdn_d[l, o][:, 0:NFH * 128], NFH * 128), load_slab(wdn_d[l, o][:, NFH * 128:NF * 128], (NF - NFH) * 128)]
            for j in range(NT):
                ts = slice(j * TT, (j + 1) * TT)
                b = bank()
                for f in range(NF):
                    av, akey = aT(f)
                    si = sis[f // NFH]
                    fo_ = (f % NFH) * 128
                    op('pe', lambda e, b=b, f=f, ts=ts, si=si, av=av, fo_=fo_: e.matmul(banks[b][:, 0:TT], lhsT=slabs[si][:, fo_:fo_ + 128],
                                                                               rhs=av[:, ts], start=(f == 0), stop=(f == NF - 1)),
                       reads=[f'slab{si}', akey], writes=[bk(b)])
                op('dve', lambda e, b=b, o=o, ts=ts: e.tensor_tensor(out=xT[:, o, ts], in0=xT[:, o, ts], in1=banks[b][:, 0:TT], op=ALU.add),
                   reads=[bk(b), ('xT', j)], writes=[('xT', j)])

    TPT = TT // 128
    for s in range(c.NSEQ):
        for blk in range(c.NBLK):
            first = (blk == 0)
            t0 = blk * BLK
            for tt in range(BLK // 128):
                dma('sp', xin[:], x_d[s, t0 + tt * 128:t0 + (tt + 1) * 128, :], writes=['xin'])
                for k4 in range(0, KD, 4):
                    b = bank()
                    n4 = min(4, KD - k4)
                    for q in range(n4):
                        op('pe', lambda e, b=b, q=q, k4=k4: e.transpose(out=banks[b][:, q * 128:(q + 1) * 128], in_=xin[:, (k4 + q) * 128:(k4 + q + 1) * 128],
                                                                         identity=C('ident', 128)), reads=['xin', 'cst'], writes=[bk(b)])
                    op('act', lambda e, b=b, k4=k4, n4=n4, tt=tt: e.activation(out=xT[:, k4:k4 + n4, tt * 128:(tt + 1) * 128],
                                                                               in_=banks[b][:, 0:n4 * 128].rearrange("p (a b) -> p a b", b=128), func=AF.Copy),
                       reads=[bk(b)], writes=[('xT', tt // TPT)])
            for l in range(c.DEPTH):
                PH = getattr(c, 'phases', 'nmrwf')
                if 'n' in PH:
                    rmsnorm(l, 'ln1')
                if 'm' in PH:
                    mlstm(l, first)
                if 'r' in PH:
                    retention(l, first, blk)
                if 'w' in PH:
                    rwkv(l, first)
                if 'f' in PH:
                    wout_ffn(l)
            fo = Ft[0]
            for n in range(BLK // TN):
                t0w = n * TN
                j = t0w // TT
                rstd, rkey = rstd_tile(t0w)
                for t8 in range(TN // 128):
                    tsl = slice(t0w + t8 * 128, t0w + (t8 + 1) * 128)
                    for kk in range(KD):
                        op('dve', lambda e, kk=kk, tsl=tsl, t8=t8: e.scalar_tensor_tensor(out=fo[:, kk * 128:(kk + 1) * 128], in0=xT[:, kk, tsl], scalar=C('lnf', 1, kk),
                                                                                          in1=rstd[:, t8 * 128:(t8 + 1) * 128], op0=ALU.mult, op1=ALU.mult),
                           reads=[('xT', j), rkey, 'cst'], writes=['F0'])
                    for k4 in range(0, KD, 4):
                        b2 = bank()
                        n4 = min(4, KD - k4)
                        for q in range(n4):
                            op('pe', lambda e, b2=b2, q=q, k4=k4: e.transpose(out=banks[b2][:, q * 128:(q + 1) * 128], in_=fo[:, (k4 + q) * 128:(k4 + q + 1) * 128],
                                                                               identity=C('ident', 128)), reads=['F0', 'cst'], writes=[bk(b2)])
                        op('act', lambda e, b2=b2, k4=k4, n4=n4: e.activation(out=xin[:, k4 * 128:(k4 + n4) * 128], in_=banks[b2][:, 0:n4 * 128], func=AF.Copy),
                           reads=[bk(b2)], writes=['xin'])
                    dma('sp', out_d[s, t0 + tsl.start:t0 + tsl.stop, :], xin[:], reads=['xin'], is_output=True)
    k.emit()
    return nc


_CACHE = {}


def kernel(**inputs):
    cfg = Cfg()
    packed = pack_host(cfg, inputs)
    if 'nc' not in _CACHE:
        _CACHE['nc'] = build(cfg)
    nc = _CACHE['nc']
    x = np.asarray(inputs['x'], np.float32)
    in_maps = []
    for i in range(8):
        m = dict(packed)
        m['x'] = np.ascontiguousarray(x[i * cfg.NSEQ:(i + 1) * cfg.NSEQ])
        m['win'] = packed['win'].reshape(cfg.DEPTH, cfg.NG, 128, cfg.KD * 128)
        m['wout'] = packed['wout'].reshape(cfg.DEPTH, cfg.KD, 128, cfg.KM * 128)
        m['wgu'] = packed['wgu'].reshape(cfg.DEPTH, cfg.NF, 128, 2 * cfg.KD * 128)
        m['wdn'] = packed['wdn'].reshape(cfg.DEPTH, cfg.KD, 128, cfg.NF * 128)
        in_maps.append(m)
    res = run_bass_kernel_spmd(nc, in_maps, core_ids=list(range(8)))
    return np.concatenate([r['out'] for r in res.results], axis=0).astype(np.float32)
```

```python
import math
DBG = 99
VAR = ''
from contextlib import ExitStack
import numpy as np
import concourse.bass as bass
import concourse.mybir as mybir
from concourse.alu_op_type import AluOpType as ALU
from concourse.bass_utils import run_bass_kernel_spmd

F32 = mybir.dt.float32
BF16 = mybir.dt.bfloat16
AF = mybir.ActivationFunctionType
AX = mybir.AxisListType

ENGS = ['pe', 'dve', 'act', 'pool', 'sp']
NDMASEM = 8
HALO = 4
DMASCR = 16384
SAME_ENGINE_SYNC = True
PIPE_OFF = 9
L = 128


class _Rec:
    def __init__(self):
        self.call = None

    def __getattr__(self, name):
        def f(*a, **kw):
            self.call = (name, a, kw)
            return self
        return f


class TS:
    def __init__(self):
        object.__setattr__(self, 'sets', [{}, {}])
        object.__setattr__(self, 'par', 0)

    def add(self, name, t0, t1):
        self.sets[0][name] = t0
        self.sets[1][name] = t1

    def set(self, par):
        object.__setattr__(self, 'par', par)

    def __getattr__(self, name):
        return self.sets[self.par][name]

    def k(self, name):
        return f'{name}@{self.par}'


class KB:
    def __init__(self, nc):
        self.nc = nc
        self.q = {e: [] for e in ENGS}
        self.cnt = {e: 0 for e in ENGS}
        self.lastw = {}
        self.readers = {}
        self.seen = {e: {} for e in ENGS}
        self.dma_i = {e: 0 for e in ENGS}
        self.dma_cnt = {}
        self.out_events = []
        self.stack = ExitStack()
        self.ntile = 0
        self.bank_i = 0

    def sb(self, shape, dt=F32, name=None):
        self.ntile += 1
        return self.stack.enter_context(self.nc.sbuf_tensor(name or f"t{self.ntile}", list(shape), dt))

    def ps(self, shape, dt=F32, name=None):
        self.ntile += 1
        return self.stack.enter_context(self.nc.psum_tensor(name or f"p{self.ntile}", list(shape), dt))

    def _deps(self, reads, writes):
        ev = []
        for k in reads:
            if k in self.lastw:
                ev.append(self.lastw[k])
        for k in writes:
            if k in self.lastw:
                ev.append(self.lastw[k])
            ev.extend(self.readers.get(k, []))
        return ev

    def _filter(self, eng, evs):
        best = {}
        for (s, v) in evs:
            if s[0] == 'eng' and s[1] == eng and (eng == 'pe' or not SAME_ENGINE_SYNC):
                continue
            if self.seen[eng].get(s, 0) >= v:
                continue
            if best.get(s, 0) < v:
                best[s] = v
        for s, v in best.items():
            self.seen[eng][s] = v
        return list(best.items())

    def _record(self, ev, reads, writes):
        for k in writes:
            self.lastw[k] = ev
            self.readers[k] = []
        for k in reads:
            self.readers.setdefault(k, []).append(ev)

    def op(self, eng, fn, reads=(), writes=()):
        waits = self._filter(eng, self._deps(reads, writes))
        self.cnt[eng] += 1
        ev = (('eng', eng), self.cnt[eng])
        rec = _Rec()
        fn(rec)
        call = rec.call
        self.q[eng].append(('op', waits, lambda e, call=call: getattr(e, call[0])(*call[1], **call[2])))
        self._record(ev, reads, writes)
        return ev

    def dma(self, eng, out, in_, reads=(), writes=(), is_output=False):
        i = self.dma_i[eng] % NDMASEM
        self.dma_i[eng] += 1
        s = ('dma', eng, i)
        prev = self.dma_cnt.get(s, 0)
        evs = self._deps(reads, writes)
        if prev:
            evs.append((s, prev * 16))
        waits = self._filter(eng, evs)
        self.dma_cnt[s] = prev + 1
        ev = (s, (prev + 1) * 16)
        self.q[eng].append(('dma', waits, (out, in_), s))
        self._record(ev, reads, writes)
        if is_output:
            self.out_events.append(ev)
        return ev

    def emit(self):
        nc = self.nc
        semh = {}
        with ExitStack() as st:
            for e in ENGS:
                semh[('eng', e)] = st.enter_context(nc.semaphore(f"s_{e}"))
            for s in self.dma_cnt:
                semh[s] = st.enter_context(nc.semaphore(f"d_{s[1]}_{s[2]}"))
            fin = self._filter('sp', self.out_events)
            with nc.Block() as block:
                def run(eng_name, engine):
                    me = semh[('eng', eng_name)]
                    for item in self.q[eng_name]:
                        if item[0] == 'op':
                            _, waits, fn = item
                            for s, v in waits:
                                engine.wait_ge(semh[s], v)
                            fn(engine).then_inc(me, 1)
                        else:
                            _, waits, (out, in_), s = item
                            for ss, v in waits:
                                engine.wait_ge(semh[ss], v)
                            engine.dma_start(out=out, in_=in_).then_inc(semh[s], 16)
                    if eng_name == 'sp':
                        for s, v in fin:
                            engine.wait_ge(semh[s], v)

                @block.tensor
                def _(e):
                    run('pe', e)

                @block.vector
                def _(e):
                    run('dve', e)

                @block.scalar
                def _(e):
                    run('act', e)

                @block.gpsimd
                def _(e):
                    run('pool', e)

                @block.sync
                def _(e):
                    run('sp', e)
        self.stack.close()


class Cfg:
    def __init__(s, D=1024, SEQ=2048, NSEQ=2, BLK=1024, DEPTH=2, MH=4, RH=6, WH=6, DFF=2816,
                 WL=64, AL=64, GL=128):
        s.D, s.SEQ, s.NSEQ, s.BLK, s.DEPTH = D, SEQ, NSEQ, BLK, DEPTH
        s.MH, s.RH, s.WH, s.DFF, s.WL, s.AL, s.GL = MH, RH, WH, DFF, WL, AL, GL
        s.KD = D // 128
        s.NBLK = SEQ // BLK
        s.NCH = BLK // L
        s.TT = min(512, BLK)
        s.NT = BLK // s.TT
        s.MP, s.RP, s.WP = MH // 2, RH // 2, WH // 2
        s.NF = DFF // 128
        s.MW, s.RW, s.WW = MH * 64, RH * 64, WH * 64
        s.MIX = s.MW + s.RW + s.WW
        s.KM = s.MIX // 128
        s.MCOLS = 4 * s.MW + 2 * MH
        s.RCOLS = 4 * s.RW
        s.WCOLS = 3 * s.WW + WL + AL + GL
        g = []
        mb = 0
        for p in range(s.MP):
            for nm, base in (('mq', 0), ('mk', s.MW), ('mv', 2 * s.MW), ('mo', 3 * s.MW)):
                g.append(((nm, p), [mb + base + 128 * p + i for i in range(128)]))
        g.append((('mgi', 0), [mb + 4 * s.MW + i for i in range(MH)]))
        g.append((('mgf', 0), [mb + 4 * s.MW + MH + i for i in range(MH)]))
        rb = s.MCOLS
        swap = [h * 64 + ((j + 32) % 64) for h in range(2) for j in range(64)]
        for p in range(s.RP):
            for nm, base in (('rq', 0), ('rk', s.RW), ('rv', 2 * s.RW), ('rg', 3 * s.RW)):
                g.append(((nm, p), [rb + base + 128 * p + i for i in range(128)]))
                if nm in ('rq', 'rk'):
                    g.append(((nm + 's', p), [rb + base + 128 * p + swap[i] for i in range(128)]))
        wb = s.MCOLS + s.RCOLS
        g.append((('wwl', 0), [wb + 3 * s.WW + i for i in range(WL)]))
        g.append((('wal', 0), [wb + 3 * s.WW + WL + i for i in range(AL)]))
        g.append((('wgl', 0), [wb + 3 * s.WW + WL + AL + i for i in range(GL)]))
        for p in range(s.WP):
            for nm, base in (('wr', 0), ('wk', s.WW), ('wv', 2 * s.WW)):
                g.append(((nm, p), [wb + base + 128 * p + i for i in range(128)]))
        s.groups = g
        s.gidx = {k: i for i, (k, _) in enumerate(g)}
        s.NG = len(g)
        pc = {}
        n = 0

        def add(name, w=1):
            nonlocal n
            pc[name] = n
            n += w
        add('ln1', s.KD)
        add('ln2', s.KD)
        for p in range(s.MP):
            add(('cq', p), 4)
            add(('ck', p), 4)
            add(('mlg', p))
        add('bi')
        add('bf')
        for key, _ in g:
            if key[0][0] == 'w':
                add(('mu', key))
        for p in range(s.WP):
            for nm in ('w0', 'a0', 'kk', 'ka', 'rk', 'lg', 'lb'):
                add((nm, p))
        s.pc = pc
        s.NPAR = n
        cc = {}
        n = 0

        def addc(name, w):
            nonlocal n
            cc[name] = n
            n += w
        addc('ident', 128)
        addc('mincl', 128)
        addc('mstrict', 128)
        addc('mN', 128)
        addc('bones', 128)
        addc('es_r', RH)
        addc('ft_r', RH)
        addc('dec_r', s.RP)
        addc('sel', s.MP * 128)
        addc('lnf', s.KD)
        s.cc = cc
        s.NC = n


def pack_host(cfg, inp):
    c = cfg
    f32 = np.float32
    D, KD = c.D, c.KD
    w_in = np.asarray(inp['w_in'], f32)
    win = np.zeros((c.DEPTH, c.NG, 128, KD, 128), f32)
    for gi, (key, cols) in enumerate(c.groups):
        sub = w_in[:, :, cols]
        sub = sub.reshape(c.DEPTH, KD, 128, len(cols)).transpose(0, 2, 1, 3)
        win[:, gi, :, :, :len(cols)] = sub
    wout = np.asarray(inp['w_out'], f32).reshape(c.DEPTH, c.KM, 128, KD, 128).transpose(0, 3, 2, 1, 4)
    wg = np.asarray(inp['w_gate'], f32).reshape(c.DEPTH, KD, 128, c.NF, 128).transpose(0, 3, 2, 1, 4)
    wu = np.asarray(inp['w_up'], f32).reshape(c.DEPTH, KD, 128, c.NF, 128).transpose(0, 3, 2, 1, 4)
    wgu = np.stack([wg, wu], axis=3)
    wdn = np.asarray(inp['w_down'], f32).reshape(c.DEPTH, c.NF, 128, KD, 128).transpose(0, 3, 2, 1, 4)
    lora = np.zeros((c.DEPTH, 3, 128, c.WW), f32)
    lora[:, 0, :c.WL] = inp['rw_w_up']
    lora[:, 1, :c.AL] = inp['rw_a_up']
    lora[:, 2, :c.GL] = inp['rw_g_up']
    par = np.zeros((c.DEPTH, 128, c.NPAR), f32)
    pc = c.pc
    par[:, :, pc['ln1']:pc['ln1'] + KD] = np.asarray(inp['ln1_g'], f32).reshape(c.DEPTH, KD, 128).transpose(0, 2, 1)
    par[:, :, pc['ln2']:pc['ln2'] + KD] = np.asarray(inp['ln2_g'], f32).reshape(c.DEPTH, KD, 128).transpose(0, 2, 1)
    mconv = np.asarray(inp['m_conv'], f32)
    for p in range(c.MP):
        par[:, :, pc[('cq', p)]:pc[('cq', p)] + 4] = mconv[:, :, 128 * p:128 * p + 128].transpose(0, 2, 1)
        par[:, :, pc[('ck', p)]:pc[('ck', p)] + 4] = mconv[:, :, c.MW + 128 * p:c.MW + 128 * p + 128].transpose(0, 2, 1)
        par[:, :, pc[('mlg', p)]] = np.asarray(inp['m_ln_g'], f32)[:, 128 * p:128 * p + 128]
    par[:, :c.MH, pc['bi']] = inp['m_b_i']
    par[:, :c.MH, pc['bf']] = inp['m_b_f']
    mu = np.asarray(inp['rw_mu'], f32)
    wbase = c.MCOLS + c.RCOLS
    for key, cols in c.groups:
        if key[0][0] == 'w':
            par[:, :len(cols), pc[('mu', key)]] = mu[:, [cc - wbase for cc in cols]]
    for p in range(c.WP):
        sl = slice(128 * p, 128 * p + 128)
        for nm, src in (('w0', 'rw_w0'), ('a0', 'rw_a0'), ('kk', 'rw_k_k'), ('ka', 'rw_k_a'),
                        ('rk', 'rw_r_k'), ('lg', 'rw_ln_g'), ('lb', 'rw_ln_b')):
            par[:, :, pc[(nm, p)]] = np.asarray(inp[src], f32)[:, sl]
    cst = np.zeros((128, c.NC), f32)
    cc = c.cc
    i = np.arange(128)
    cst[:, cc['ident']:cc['ident'] + 128] = np.eye(128)
    cst[:, cc['mincl']:cc['mincl'] + 128] = (i[:, None] <= i[None, :])
    cst[:, cc['mstrict']:cc['mstrict'] + 128] = (i[:, None] < i[None, :])
    cst[:, cc['mN']:cc['mN'] + 128] = (i[None, :] < i[:, None])
    cst[:, cc['bones']:cc['bones'] + 128] = (i[:, None] // 64 == i[None, :] // 64)
    gam = 1.0 - 2.0 ** (-5.0 - np.arange(c.RH, dtype=np.float64))
    cst[:, cc['es_r']:cc['es_r'] + c.RH] = gam[None, :] ** (L - 1.0 - i[:, None])
    cst[:, cc['ft_r']:cc['ft_r'] + c.RH] = gam[None, :] ** (i[:, None] - (L - 1.0))
    for p in range(c.RP):
        cst[:64, cc['dec_r'] + p] = gam[2 * p] ** L
        cst[64:, cc['dec_r'] + p] = gam[2 * p + 1] ** L
    for p in range(c.MP):
        for m in range(128):
            cst[2 * p + m // 64, cc['sel'] + p * 128 + m] = 1.0
    cst[:, cc['lnf']:cc['lnf'] + KD] = np.asarray(inp['lnf_g'], f32).reshape(KD, 128).T
    theta = 1.0 / (10000.0 ** np.linspace(0.0, 1.0, 32, dtype=np.float32))
    ang = np.arange(c.SEQ, dtype=np.float32)[None, :] * theta.astype(np.float32)[:, None]
    cs, sn = np.cos(ang).astype(f32), np.sin(ang).astype(f32)
    rot = np.zeros((2, 128, c.SEQ), f32)
    for h in range(2):
        rot[0, h * 64:h * 64 + 32] = cs
        rot[0, h * 64 + 32:h * 64 + 64] = cs
        rot[1, h * 64:h * 64 + 32] = -sn
        rot[1, h * 64 + 32:h * 64 + 64] = sn
    return dict(win=win, wout=np.ascontiguousarray(wout), wgu=np.ascontiguousarray(wgu),
                wdn=np.ascontiguousarray(wdn), lora=lora, par=par, cst=cst, rot=rot)


def build(cfg):
    c = cfg
    nc = bass.Bass("TRN2", target_bir_lowering=False, dynamic_dma_scratch_size=DMASCR)
    KD, BLK, TT, NT, NCH, NF, KM = c.KD, c.BLK, c.TT, c.NT, c.NCH, c.NF, c.KM
    x_d = nc.dram_tensor("x", [c.NSEQ, c.SEQ, c.D], F32, kind="ExternalInput").ap()
    win_d = nc.dram_tensor("win", [c.DEPTH, c.NG, 128, KD * 128], F32, kind="ExternalInput").ap()
    wout_d = nc.dram_tensor("wout", [c.DEPTH, KD, 128, KM * 128], F32, kind="ExternalInput").ap()
    wgu_d = nc.dram_tensor("wgu", [c.DEPTH, NF, 128, 2 * KD * 128], F32, kind="ExternalInput").ap()
    wdn_d = nc.dram_tensor("wdn", [c.DEPTH, KD, 128, NF * 128], F32, kind="ExternalInput").ap()
    lora_d = nc.dram_tensor("lora", [c.DEPTH, 3, 128, c.WW], F32, kind="ExternalInput").ap()
    par_d = nc.dram_tensor("par", [c.DEPTH, 128, c.NPAR], F32, kind="ExternalInput").ap()
    cst_d = nc.dram_tensor("cst", [128, c.NC], F32, kind="ExternalInput").ap()
    rot_d = nc.dram_tensor("rot", [2, 128, c.SEQ], F32, kind="ExternalInput").ap()
    out_d = nc.dram_tensor("out", [c.NSEQ, c.SEQ, c.D], F32, kind="ExternalOutput").ap()

    k = KB(nc)
    op, dma = k.op, k.dma
    xT = k.sb([128, KD, BLK], F32, "xT")
    hT = k.sb([128, KD, BLK], BF16, "hT")
    yT = k.sb([128, KM, BLK], BF16, "yT")
    NFH = (NF + 1) // 2
    SLABW = max(KD * 128, KM * 128, 2 * KD * 128, NFH * 128)
    NSLAB = 5
    slabs = [k.sb([128, SLABW], BF16, f"slab{i}") for i in range(NSLAB)]
    NFT = 12
    FW = max(HALO + BLK, c.D)
    Ft = [k.sb([128, FW], F32, f"F{i}") for i in range(NFT)]
    Bt = {i: k.sb([128, BLK], BF16, f"B{i}") for i in (0, 3, 4, 9, 10, 11)}
    cst = k.sb([128, c.NC], F32, "cst_sb")
    par = [k.sb([128, c.NPAR], F32, f"par_sb{l}") for l in range(c.DEPTH)]
    lorab = [k.sb([128, 3, c.WW], BF16, f"lora_sb{l}") for l in range(c.DEPTH)]
    identb = k.sb([128, 128], BF16, "identb")
    bonesb = k.sb([128, 128], BF16, "bonesb")
    mask4 = k.sb([128, 4, 128], BF16, "mask4")
    xin = k.sb([128, c.D], F32, "xin")
    TN = min(256, TT)
    rstd2 = [k.sb([128, TN], F32, f"rstd{i}") for i in range(2)]
    tok3 = k.sb([128, 3, 128], BF16, "tok3")
    vaug = k.sb([128, 2, 66], BF16, "vaug")
    ATt = [k.sb([128, 128], BF16, f"AT{h}") for h in range(2)]
    SC2 = k.sb([128, 2, 4, 128], BF16, "SC2")
    Q0t = k.sb([128, 2, 128], BF16, "Q0t")
    PQ = [k.sb([128, 2, 2, 128], BF16, f"PQ{i}") for i in range(2)]
    Tb2 = [k.sb([128, 2, 128], BF16, f"Tb2_{i}") for i in range(2)]
    mN2 = k.sb([128, 2, 128], BF16, "mN2")
    id2 = k.sb([128, 2, 128], BF16, "id2")
    XU = k.sb([128, 2, 2, 64], BF16, "XU")
    hpre = k.sb([128, 2, 64], F32, "hpre")
    ndsb = k.sb([128, 2, 66], F32, "ndsb")
    hn = k.sb([128, 128], BF16, "hn")
    st6 = k.sb([128, 2, 6], F32, "st6")
    mv = k.sb([128, 2, 2], F32, "mv")
    sm = k.sb([128, 8], F32, "sm")
    ytmp = k.sb([128, 128], F32, "ytmp")
    omka = k.sb([128, c.WP], F32, "omka")
    Cf = [[k.sb([128, 65], F32, f"Cf{l}_{p}") for p in range(c.MP)] for l in range(c.DEPTH)]
    Rf = [[k.sb([128, 64], F32, f"Rf{l}_{p}") for p in range(c.RP)] for l in range(c.DEPTH)]
    Mf = [[k.sb([128, 64], F32, f"Mf{l}_{p}") for p in range(c.WP)] for l in range(c.DEPTH)]
    Sz = k.sb([128, 2, 66], BF16, "Sz")
    halo_keys = [key for key, _ in c.groups if key[0] in ('mq', 'mk') or key[0][0] == 'w']
    halo = {(l, key): k.sb([128, HALO], F32, f"halo{l}_{key[0]}{key[1]}") for l in range(c.DEPTH) for key in halo_keys}
    gcar = [k.sb([c.MH, 2], F32, f"gcar{l}") for l in range(c.DEPTH)]
    Rall = k.sb([c.MH, NCH + 1], F32, "Rall")
    decrow = k.sb([c.MH, NCH], F32, "decrow")
    decb = k.sb([128, NCH], F32, "decb")
    est = k.sb([128, NCH, c.MH], F32, "est")
    thrt = k.sb([128, NCH, c.MH], F32, "thrt")
    WLt = k.sb([128, NCH], F32, "WLt")
    S = TS()
    _shapes = dict(tok3=([128, 3, 128], BF16), Sz=([128, 2, 66], BF16), SC2=([128, 2, 4, 128], BF16), Q0t=([128, 2, 128], BF16),
                   XU=([128, 2, 2, 64], BF16), hpre=([128, 2, 64], F32), hn=([128, 128], BF16), st6=([128, 2, 6], F32),
                   mv=([128, 2, 2], F32), sm=([128, 8], F32), ytmp=([128, 128], F32), vaug=([128, 2, 66], BF16), ndsb=([128, 2, 66], F32))
    _first = dict(tok3=tok3, Sz=Sz, SC2=SC2, Q0t=Q0t, XU=XU, hpre=hpre, hn=hn, st6=st6, mv=mv, sm=sm, ytmp=ytmp, vaug=vaug, ndsb=ndsb)
    for _n, (_sh, _dt) in _shapes.items():
        S.add(_n, _first[_n], k.sb(_sh, _dt, _n + "_b"))
    S.add('PQ', PQ, [k.sb([128, 2, 2, 128], BF16, f"PQb{i}") for i in range(2)])
    S.add('Tb2', Tb2, [k.sb([128, 2, 128], BF16, f"Tb2b_{i}") for i in range(2)])
    S.add('ATt', ATt, [k.sb([128, 128], BF16, f"ATb{h}") for h in range(2)])
    banks = [k.ps([128, 512], F32, f"bank{i}") for i in range(8)]

    def bank():
        i = k.bank_i % 8
        k.bank_i += 1
        return i

    def bk(i):
        return ('bank', i)

    def bbf(i):
        return banks[i][:].bitcast(BF16)

    cc = c.cc
    pc = c.pc

    def C(name, w=1, off=0):
        return cst[:, cc[name] + off:cc[name] + off + w]

    dma('sp', cst[:], cst_d, writes=['cst'])
    for l in range(c.DEPTH):
        dma('sp', par[l][:], par_d[l], writes=[f'par{l}'])
        dma('pool', lorab[l][:], lora_d[l].rearrange("a p w -> p a w"), writes=[f'lora{l}'])
    op('dve', lambda e: e.tensor_copy(out=identb[:], in_=C('ident', 128)), reads=['cst'], writes=['identb'])
    op('dve', lambda e: e.tensor_copy(out=bonesb[:], in_=C('bones', 128)), reads=['cst'], writes=['bonesb'])
    for i, nm in enumerate(('mstrict', 'mincl', 'mstrict', 'mincl')):
        op('dve', lambda e, i=i, nm=nm: e.tensor_copy(out=mask4[:, i, :], in_=C(nm, 128)), reads=['cst'], writes=['mask4'])

    for h in range(2):
        op('dve', lambda e, h=h: e.tensor_copy(out=mN2[:, h, :], in_=C('mN', 128)), reads=['cst'], writes=['mN2'])
        op('dve', lambda e, h=h: e.tensor_copy(out=id2[:, h, :], in_=C('ident', 128)), reads=['cst'], writes=['id2'])
    slab_i = [0]

    def load_slab(src_ap, width):
        i = slab_i[0] % NSLAB
        slab_i[0] += 1
        dma('pool', slabs[i][:, 0:width], src_ap, writes=[f'slab{i}'])
        return i

    def P(l, name, w=1):
        return par[l][:, pc[name]:pc[name] + w]

    def rstd_tile(t0w):
        j = t0w // TT
        ri = (t0w // TN) % 2
        rstd, rkey = rstd2[ri], f'rstd{ri}'
        sq = Ft[11][:].bitcast(BF16)[:, 0:KD * TN].rearrange("p (a b) -> p a b", b=TN)
        op('act', lambda e: e.activation(out=sq, in_=xT[:, :, t0w:t0w + TN], func=AF.Square),
           reads=[('xT', j)], writes=['F11'])
        b = bank()
        for kk in range(KD):
            op('pe', lambda e, b=b, kk=kk: e.matmul(banks[b][:, 0:TN], lhsT=bonesall[:], rhs=sq[:, kk, :],
                                                     start=(kk == 0), stop=(kk == KD - 1)),
               reads=['F11', 'bonesall'], writes=[bk(b)])
        op('act', lambda e, b=b: e.activation(out=rstd[:], in_=banks[b][:, 0:TN], func=AF.Sqrt,
                                              scale=1.0 / c.D, bias=epsc[:, 0:1]),
           reads=[bk(b), 'epsc'], writes=[rkey])
        op('dve', lambda e: e.reciprocal(out=rstd[:], in_=rstd[:]), reads=[rkey], writes=[rkey])
        return rstd, rkey

    def rmsnorm(l, gname):
        for n in range(BLK // TN):
            t0w = n * TN
            j = t0w // TT
            ts = slice(t0w, t0w + TN)
            rstd, rkey = rstd_tile(t0w)
            for kk in range(KD):
                gap = par[l][:, pc[gname] + kk:pc[gname] + kk + 1]
                op('dve', lambda e, kk=kk, ts=ts, gap=gap, rstd=rstd: e.scalar_tensor_tensor(
                    out=hT[:, kk, ts], in0=xT[:, kk, ts], scalar=gap, in1=rstd[:], op0=ALU.mult, op1=ALU.mult),
                   reads=[('xT', j), rkey, f'par{l}'], writes=[('hT', j)])

    bonesall = k.sb([128, 128], BF16, "bonesall")
    epsc = k.sb([128, 4], F32, "epsc")
    op('dve', lambda e: e.memset(bonesall[:], 1.0), writes=['bonesall'])
    op('dve', lambda e: e.memset(epsc[:, 0:1], 1e-6), writes=['epsc'])
    op('dve', lambda e: e.memset(epsc[:, 1:2], 1e-5), writes=['epsc'])
    op('dve', lambda e: e.memset(epsc[:, 2:3], 64e-5), writes=['epsc'])
    op('dve', lambda e: e.memset(epsc[:, 3:4], 1.0), writes=['epsc'])

    def project(l, key, evac):
        gi = c.gidx[key]
        si = load_slab(win_d[l, gi], KD * 128)
        for j in range(NT):
            b = bank()
            for kk in range(KD):
                op('pe', lambda e, b=b, kk=kk, j=j, si=si: e.matmul(
                    banks[b][:, 0:TT], lhsT=slabs[si][:, kk * 128:(kk + 1) * 128], rhs=hT[:, kk, j * TT:(j + 1) * TT],
                    start=(kk == 0), stop=(kk == KD - 1)),
                   reads=[f'slab{si}', ('hT', j)], writes=[bk(b)])
            evac(b, j)

    def raw_evac(dst, dkey, l, key, first):
        def prep():
            if first:
                op('dve', lambda e: e.memset(dst[:, 0:HALO], 0.0), writes=[dkey])
            else:
                op('dve', lambda e: e.tensor_copy(out=dst[:, 0:HALO], in_=halo[(l, key)][:]),
                   reads=[('halo', l, key)], writes=[dkey])

        def ev(b, j):
            op('act', lambda e: e.activation(out=dst[:, HALO + j * TT:HALO + (j + 1) * TT], in_=banks[b][:, 0:TT],
                                             func=AF.Copy), reads=[bk(b)], writes=[dkey])

        def fin():
            op('dve', lambda e: e.tensor_copy(out=halo[(l, key)][:], in_=dst[:, BLK:BLK + HALO]),
               reads=[dkey], writes=[('halo', l, key)])
        return prep, ev, fin

    def proj_raw(l, key, dst, dkey, first):
        prep, ev, fin = raw_evac(dst, dkey, l, key, first)
        prep()
        project(l, key, ev)
        fin()

    def proj_simple(l, key, dst_ap_fn, dkey, func=AF.Copy):
        def ev(b, j):
            op('act', lambda e: e.activation(out=dst_ap_fn(j), in_=banks[b][:, 0:TT], func=func),
               reads=[bk(b)], writes=[dkey])
        project(l, key, ev)

    def tok_ln(eps_col):
        for h in range(2):
            op('dve', lambda e, h=h: e.bn_stats(out=S.st6[:, h, :], in_=S.hpre[:, h, :]), reads=[S.k('hpre')], writes=[S.k('st6')])
            op('dve', lambda e, h=h: e.bn_aggr(out=S.mv[:, h, :], in_=S.st6[:, h, :]), reads=[S.k('st6')], writes=[S.k('mv')])
        op('act', lambda e: e.activation(out=S.sm[:, 0:2], in_=S.mv[:, :, 1], func=AF.Sqrt, bias=epsc[:, eps_col:eps_col + 1]),
           reads=[S.k('mv'), 'epsc'], writes=[S.k('sm')])
        op('dve', lambda e: e.reciprocal(out=S.sm[:, 2:4], in_=S.sm[:, 0:2]), reads=[S.k('sm')], writes=[S.k('sm')])
        for h in range(2):
            op('dve', lambda e, h=h: e.tensor_scalar(out=S.hn[:, h * 64:(h + 1) * 64], in0=S.hpre[:, h, :],
                                                      scalar1=S.mv[:, h, 0:1], scalar2=S.sm[:, 2 + h:3 + h],
                                                      op0=ALU.subtract, op1=ALU.mult),
               reads=[S.k('hpre'), S.k('mv'), S.k('sm')], writes=[S.k('hn')])

    def transpose_to(bi, col, src_ap, rkeys):
        op('pe', lambda e: e.transpose(out=bbf(bi)[:, col:col + 128], in_=src_ap, identity=identb[:]),
           reads=list(rkeys) + ['identb'], writes=[bk(bi)])

    def linattn_chunk(ci, qc, qkey, kz, kzkey, vT, vkey, es_fn, es_keys, Sf, Sfkey, dec_ap, dec_keys, naug,
                      post):
        cs = slice(ci * L, (ci + 1) * L)
        W = 64 + naug
        bt_ = bank()
        transpose_to(bt_, 0, vT[:, cs], [vkey])
        transpose_to(bt_, 128, kz[:, 0, cs], [kzkey])
        transpose_to(bt_, 256, kz[:, 1, cs], [kzkey])
        op('act', lambda e: e.activation(out=S.tok3[:].rearrange("p a b -> p (a b)"), in_=bbf(bt_)[:, 0:384], func=AF.Copy),
           reads=[bk(bt_)], writes=[S.k('tok3')])
        for h in range(2):
            op('dve', lambda e, h=h: e.tensor_scalar(out=S.vaug[:, h, 0:64], in0=S.tok3[:, 0, h * 64:(h + 1) * 64],
                                                      scalar1=es_fn(h), scalar2=None, op0=ALU.mult),
               reads=[S.k('tok3')] + es_keys, writes=[S.k('vaug')])
            if naug:
                op('act', lambda e, h=h: e.activation(out=S.vaug[:, h, 64:65], in_=es_fn(h), func=AF.Copy),
                   reads=es_keys, writes=[S.k('vaug')])
        if DBG <= 2:
            return
        op('dve', lambda e: e.tensor_scalar(out=Sf[:, 0:W], in0=Sf[:, 0:W], scalar1=dec_ap, scalar2=None, op0=ALU.mult),
           reads=[Sfkey] + dec_keys, writes=[Sfkey])
        for h in range(2):
            op('act', lambda e, h=h: e.activation(out=S.Sz[h * 64:(h + 1) * 64, h, 0:W], in_=Sf[h * 64:(h + 1) * 64, 0:W],
                                                  func=AF.Copy), reads=[Sfkey], writes=[S.k('Sz')])
        if DBG <= 3:
            return
        for h in range(2):
            bs = bank()
            op('pe', lambda e, h=h, bs=bs: e.matmul(banks[bs][:, 0:128], lhsT=kz[:, h, cs], rhs=qc[:, cs], start=True, stop=True),
               reads=[kzkey, qkey], writes=[bk(bs)])
            op('dve', lambda e, h=h, bs=bs: e.tensor_tensor(out=S.ATt[h][:], in0=banks[bs][:, 0:128], in1=C('mincl', 128), op=ALU.mult),
               reads=[bk(bs), 'cst'], writes=[S.k(f'AT{h}')])
        bo = bank()
        for h in range(2):
            op('pe', lambda e, h=h: e.matmul(banks[bo][:, h * 128:h * 128 + W], lhsT=S.ATt[h][:], rhs=S.vaug[:, h, 0:W], start=True, stop=False),
               reads=[S.k(f'AT{h}'), S.k('vaug')], writes=[bk(bo)])
            op('pe', lambda e, h=h: e.matmul(banks[bo][:, h * 128:h * 128 + W], lhsT=qc[:, cs], rhs=S.Sz[:, h, 0:W], start=False, stop=True),
               reads=[qkey, S.k('Sz')], writes=[bk(bo)])
        if DBG <= 4 or DBG in (41, 42):
            return
        bu = bank()
        for h in range(2):
            op('pe', lambda e, h=h: e.matmul(banks[bu][:, 0:W], lhsT=S.tok3[:, 1 + h, :], rhs=S.vaug[:, h, 0:W], start=(h == 0), stop=(h == 1)),
               reads=[S.k('tok3'), S.k('vaug')], writes=[bk(bu)])
        op('dve', lambda e: e.tensor_tensor(out=Sf[:, 0:W], in0=Sf[:, 0:W], in1=banks[bu][:, 0:W], op=ALU.add),
           reads=[Sfkey, bk(bu)], writes=[Sfkey])
        if DBG <= 5:
            return
        post(bo)

    def finish_chunk(ci, pair_idx, eps_col, fin_fn):
        tok_ln(eps_col)
        bt2 = bank()
        transpose_to(bt2, 0, S.hn[:], [S.k('hn')])
        fin_fn(bt2, slice(ci * L, (ci + 1) * L))

    def mlstm(l, first):
        onesF = Ft[11]
        MH = c.MH
        ipre, lt, Bc, gt, Gt, esr, thr = Ft[4], Ft[5], Ft[6], Ft[7], Ft[8], Ft[9], Ft[10]

        def ev_gi(b, j):
            op('act', lambda e: e.activation(out=ipre[0:MH, j * TT:(j + 1) * TT], in_=banks[b][0:MH, 0:TT], func=AF.Identity,
                                             bias=par[l][0:MH, pc['bi']:pc['bi'] + 1]), reads=[bk(b), f'par{l}'], writes=['F4'])
        project(l, ('mgi', 0), ev_gi)

        def ev_gf(b, j):
            sl = slice(j * TT, (j + 1) * TT)
            op('act', lambda e: e.activation(out=lt[0:MH, sl], in_=banks[b][0:MH, 0:TT], func=AF.Identity,
                                             bias=par[l][0:MH, pc['bf']:pc['bf'] + 1]), reads=[bk(b), f'par{l}'], writes=['F5'])
            op('act', lambda e: e.activation(out=lt[0:MH, sl], in_=lt[0:MH, sl], func=AF.Exp, scale=-1.0), reads=['F5'], writes=['F5'])
            op('act', lambda e: e.activation(out=lt[0:MH, sl], in_=lt[0:MH, sl], func=AF.Ln, bias=epsc[0:MH, 3:4]),
               reads=['F5', 'epsc'], writes=['F5'])
        project(l, ('mgf', 0), ev_gf)
        if first:
            op('dve', lambda e: e.memset(gcar[l][:, 0:1], 0.0), writes=[f'gcar{l}'])
            op('dve', lambda e: e.memset(gcar[l][:, 1:2], -1e30), writes=[f'gcar{l}'])
        op('dve', lambda e: e.memset(onesF[0:MH, 0:BLK], 1.0), writes=['F11'])
        op('dve', lambda e: e.tensor_tensor_scan(out=Bc[0:MH, 0:BLK], data0=onesF[0:MH, 0:BLK], data1=lt[0:MH, 0:BLK],
                                                 initial=gcar[l][:, 0:1], op0=ALU.mult, op1=ALU.subtract),
           reads=['F11', 'F5', f'gcar{l}'], writes=['F6'])
        op('dve', lambda e: e.tensor_tensor(out=gt[0:MH, 0:BLK], in0=ipre[0:MH, 0:BLK], in1=Bc[0:MH, 0:BLK], op=ALU.subtract),
           reads=['F4', 'F6'], writes=['F7'])
        op('dve', lambda e: e.tensor_tensor_scan(out=Gt[0:MH, 0:BLK], data0=gt[0:MH, 0:BLK], data1=gt[0:MH, 0:BLK],
                                                 initial=gcar[l][:, 1:2], op0=ALU.max, op1=ALU.max),
           reads=['F7', f'gcar{l}'], writes=['F8'])
        op('dve', lambda e: e.tensor_copy(out=Rall[:, 0:1], in_=gcar[l][:, 1:2]), reads=[f'gcar{l}'], writes=['Rall'])
        op('dve', lambda e: e.tensor_copy(out=Rall[:, 1:NCH + 1], in_=Gt[0:MH, L - 1:BLK:L]), reads=['F8'], writes=['Rall'])
        op('dve', lambda e: e.tensor_copy(out=gcar[l][:, 0:1], in_=Bc[0:MH, BLK - 1:BLK]), reads=['F6'], writes=[f'gcar{l}'])
        op('dve', lambda e: e.tensor_copy(out=gcar[l][:, 1:2], in_=Gt[0:MH, BLK - 1:BLK]), reads=['F8'], writes=[f'gcar{l}'])
        op('dve', lambda e: e.tensor_tensor(out=decrow[:], in0=Rall[:, 0:NCH], in1=Rall[:, 1:NCH + 1], op=ALU.subtract),
           reads=['Rall'], writes=['decrow'])
        op('act', lambda e: e.activation(out=decrow[:], in_=decrow[:], func=AF.Exp), reads=['decrow'], writes=['decrow'])
        rcb = Rall[:, 1:NCH + 1].unsqueeze(2).to_broadcast([MH, NCH, L])
        op('dve', lambda e: e.tensor_tensor(out=esr[0:MH, 0:BLK].rearrange("p (a b) -> p a b", b=L),
                                            in0=gt[0:MH, 0:BLK].rearrange("p (a b) -> p a b", b=L), in1=rcb, op=ALU.subtract),
           reads=['F7', 'Rall'], writes=['F9'])
        op('act', lambda e: e.activation(out=esr[0:MH, 0:BLK], in_=esr[0:MH, 0:BLK], func=AF.Exp), reads=['F9'], writes=['F9'])
        op('dve', lambda e: e.tensor_tensor(out=thr[0:MH, 0:BLK].rearrange("p (a b) -> p a b", b=L),
                                            in0=Bc[0:MH, 0:BLK].rearrange("p (a b) -> p a b", b=L), in1=rcb, op=ALU.add),
           reads=['F6', 'Rall'], writes=['F10'])
        op('act', lambda e: e.activation(out=thr[0:MH, 0:BLK], in_=thr[0:MH, 0:BLK], func=AF.Exp, scale=-1.0), reads=['F10'], writes=['F10'])
        for src, skey, dst, dkey in ((esr, 'F9', est, 'est'), (thr, 'F10', thrt, 'thrt')):
            b = bank()
            for ci in range(NCH):
                op('pe', lambda e, ci=ci, src=src, b=b: e.transpose(out=banks[b][:, ci * MH:(ci + 1) * MH], in_=src[0:MH, ci * L:(ci + 1) * L],
                                                                     identity=C('ident', MH)[0:MH, :]),
                   reads=[skey, 'cst'], writes=[bk(b)])
            op('act', lambda e, b=b, dst=dst: e.activation(out=dst[:].rearrange("p a b -> p (a b)"), in_=banks[b][:, 0:NCH * MH], func=AF.Copy),
               reads=[bk(b)], writes=[dkey])
        for p in range(c.MP):
            praw_q, praw_k, acc = Ft[0], Ft[1], Ft[3]
            qc, vT_, sgo = Bt[0], Bt[3], Bt[4]
            kz = kzt_view[0]
            b = bank()
            op('pe', lambda e, b=b, p=p: e.matmul(banks[b][:, 0:NCH], lhsT=C('sel', 128, p * 128)[0:MH, :], rhs=decrow[:], start=True, stop=True),
               reads=['cst', 'decrow'], writes=[bk(b)])
            op('act', lambda e, b=b: e.activation(out=decb[:], in_=banks[b][:, 0:NCH], func=AF.Copy), reads=[bk(b)], writes=['decb'])
            for nm, praw, pk, cname in (('mq', praw_q, 'F0', 'cq'), ('mk', praw_k, 'F1', 'ck')):
                proj_raw(l, (nm, p), praw, pk, first)
                cw = pc[(cname, p)]
                op('dve', lambda e, praw=praw, cw=cw: e.tensor_scalar(out=acc[:, 0:BLK], in0=praw[:, HALO - 3:HALO - 3 + BLK],
                                                                     scalar1=par[l][:, cw:cw + 1], scalar2=None, op0=ALU.mult),
                   reads=[pk, f'par{l}'], writes=['F3'])
                for jj in range(1, 4):
                    op('dve', lambda e, praw=praw, cw=cw, jj=jj: e.scalar_tensor_tensor(
                        out=acc[:, 0:BLK], in0=praw[:, HALO - 3 + jj:HALO - 3 + jj + BLK], scalar=par[l][:, cw + jj:cw + jj + 1],
                        in1=acc[:, 0:BLK], op0=ALU.mult, op1=ALU.add), reads=[pk, f'par{l}', 'F3'], writes=['F3'])
                if nm == 'mq':
                    op('act', lambda e: e.activation(out=qc[:], in_=acc[:, 0:BLK], func=AF.Silu), reads=['F3'], writes=['B0'])
                else:
                    op('act', lambda e: e.activation(out=acc[:, 0:BLK], in_=acc[:, 0:BLK], func=AF.Silu), reads=['F3'], writes=['F3'])
                    for h in range(2):
                        op('dve', lambda e, h=h: e.tensor_scalar(out=kz[h * 64:(h + 1) * 64, h, :], in0=acc[h * 64:(h + 1) * 64, 0:BLK],
                                                                  scalar1=0.125, scalar2=None, op0=ALU.mult),
                           reads=['F3'], writes=['BZ'])
            proj_simple(l, ('mv', p), lambda j: vT_[:, j * TT:(j + 1) * TT], 'B3')
            proj_simple(l, ('mo', p), lambda j: sgo[:, j * TT:(j + 1) * TT], 'B4', func=AF.Sigmoid)
            if first:
                op('dve', lambda e, p=p: e.memset(Cf[l][p][:], 0.0), writes=[f'Cf{l}_{p}'])
            for ci in range(NCH):
                def post(bo, ci=ci, p=p):
                    op('act', lambda e: e.activation(out=S.ndsb[:, :, 0:65], in_=banks[bo][:, 0:256].rearrange("p (a b) -> p a b", a=2)[:, :, 0:65], func=AF.Copy),
                       reads=[bk(bo)], writes=[S.k('ndsb')])
                    op('act', lambda e: e.activation(out=S.sm[:, 4:6], in_=S.ndsb[:, :, 64], func=AF.Abs), reads=[S.k('ndsb')], writes=[S.k('sm')])
                    op('dve', lambda e: e.tensor_tensor(out=S.sm[:, 4:6], in0=S.sm[:, 4:6], in1=thrt[:, ci, 2 * p:2 * p + 2], op=ALU.max),
                       reads=[S.k('sm'), 'thrt'], writes=[S.k('sm')])
                    op('dve', lambda e: e.reciprocal(out=S.sm[:, 6:8], in_=S.sm[:, 4:6]), reads=[S.k('sm')], writes=[S.k('sm')])
                    for h in range(2):
                        op('dve', lambda e, h=h: e.tensor_scalar(out=S.hpre[:, h, :], in0=S.ndsb[:, h, 0:64],
                                                                  scalar1=S.sm[:, 6 + h:7 + h], scalar2=None, op0=ALU.mult),
                           reads=[S.k('ndsb'), S.k('sm')], writes=[S.k('hpre')])

                    def fin(bt2, cs):
                        op('dve', lambda e: e.scalar_tensor_tensor(out=yT[:, p, cs], in0=bbf(bt2)[:, 0:128],
                                                                   scalar=par[l][:, pc[('mlg', p)]:pc[('mlg', p)] + 1],
                                                                   in1=sgo[:, cs], op0=ALU.mult, op1=ALU.mult),
                           reads=[bk(bt2), f'par{l}', 'B4'], writes=[('yT', p)])
                    finish_chunk(ci, p, 1, fin)
                linattn_chunk(ci, qc, 'B0', kz, 'BZ', vT_, 'B3',
                              lambda h, ci=ci, p=p: est[:, ci, 2 * p + h:2 * p + h + 1], ['est'],
                              Cf[l][p], f'Cf{l}_{p}', decb[:, ci:ci + 1], ['decb'], 1, post)

    kzt_view = [None]

    def retention(l, first, blk):
        cosT, sinT = Ft[11], Ft[10]
        pos = slice(blk * BLK, (blk + 1) * BLK)
        dma('sp', cosT[:, 0:BLK], rot_d[0][:, pos], writes=['F11'])
        dma('sp', sinT[:, 0:BLK], rot_d[1][:, pos], writes=['F10'])
        kz = kzt_view[0]
        for p in range(c.RP):
            t1, t2 = Ft[0], Ft[1]
            qr, vT_, sg = Bt[0], Bt[3], Bt[4]
            for nm in ('rq', 'rk'):
                def ev1(b, j):
                    sl = slice(j * TT, (j + 1) * TT)
                    op('dve', lambda e: e.tensor_tensor(out=t1[:, sl], in0=banks[b][:, 0:TT], in1=cosT[:, sl], op=ALU.mult),
                       reads=[bk(b), 'F11'], writes=['F0'])
                project(l, (nm, p), ev1)

                def ev2(b, j):
                    sl = slice(j * TT, (j + 1) * TT)
                    op('dve', lambda e: e.tensor_tensor(out=t2[:, sl], in0=banks[b][:, 0:TT], in1=sinT[:, sl], op=ALU.mult),
                       reads=[bk(b), 'F10'], writes=['F1'])
                project(l, (nm + 's', p), ev2)
                if nm == 'rq':
                    op('dve', lambda e: e.tensor_tensor(out=qr[:], in0=t1[:, 0:BLK], in1=t2[:, 0:BLK], op=ALU.add),
                       reads=['F0', 'F1'], writes=['B0'])
                else:
                    op('dve', lambda e: e.tensor_tensor(out=t1[:, 0:BLK], in0=t1[:, 0:BLK], in1=t2[:, 0:BLK], op=ALU.add),
                       reads=['F0', 'F1'], writes=['F0'])
                    for h in range(2):
                        op('dve', lambda e, h=h: e.tensor_scalar(out=kz[h * 64:(h + 1) * 64, h, :], in0=t1[h * 64:(h + 1) * 64, 0:BLK],
                                                                  scalar1=0.125, scalar2=None, op0=ALU.mult),
                           reads=['F0'], writes=['BZ'])
            proj_simple(l, ('rv', p), lambda j: vT_[:, j * TT:(j + 1) * TT], 'B3')
            proj_simple(l, ('rg', p), lambda j: sg[:, j * TT:(j + 1) * TT], 'B4', func=AF.Silu)
            if first:
                op('dve', lambda e, p=p: e.memset(Rf[l][p][:], 0.0), writes=[f'Rf{l}_{p}'])
            for ci in range(NCH if DBG > 1 else 0):
                def post(bo, ci=ci, p=p):
                    for h in range(2):
                        op('dve', lambda e, h=h: e.tensor_scalar(out=S.hpre[:, h, :], in0=banks[bo][:, h * 128:h * 128 + 64],
                                                                  scalar1=C('ft_r', 1, 2 * p + h), scalar2=None, op0=ALU.mult),
                           reads=[bk(bo), 'cst'], writes=[S.k('hpre')])

                    def fin(bt2, cs):
                        op('dve', lambda e: e.tensor_tensor(out=yT[:, c.MP + p, cs], in0=bbf(bt2)[:, 0:128], in1=sg[:, cs], op=ALU.mult),
                           reads=[bk(bt2), 'B4'], writes=[('yT', c.MP + p)])
                    finish_chunk(ci, c.MP + p, 1, fin)
                linattn_chunk(ci, qr, 'B0', kz, 'BZ', vT_, 'B3',
                              lambda h, p=p: C('es_r', 1, 2 * p + h), ['cst'],
                              Rf[l][p], f'Rf{l}_{p}', C('dec_r', 1, p), ['cst'], 0, post)

    def rwkv(l, first):
        tw, alb, sgl = Bt[9], Bt[10], Bt[11]
        tmp = Ft[3]

        def shifted(key, dst, dkey):
            proj_raw(l, key, dst, dkey, first)
            mcol = pc[('mu', key)]
            op('dve', lambda e: e.tensor_tensor(out=tmp[:, 0:BLK], in0=dst[:, HALO - 1:HALO - 1 + BLK], in1=dst[:, HALO:HALO + BLK], op=ALU.subtract),
               reads=[dkey], writes=['F3'])
            op('dve', lambda e: e.scalar_tensor_tensor(out=dst[:, HALO:HALO + BLK], in0=tmp[:, 0:BLK], scalar=par[l][:, mcol:mcol + 1],
                                                       in1=dst[:, HALO:HALO + BLK], op0=ALU.mult, op1=ALU.add),
               reads=['F3', dkey, f'par{l}'], writes=[dkey])
        shifted(('wwl', 0), Ft[0], 'F0')
        op('act', lambda e: e.activation(out=tw[:], in_=Ft[0][:, HALO:HALO + BLK], func=AF.Tanh), reads=['F0'], writes=['B9'])
        shifted(('wal', 0), Ft[0], 'F0')
        op('act', lambda e: e.activation(out=alb[:], in_=Ft[0][:, HALO:HALO + BLK], func=AF.Copy), reads=['F0'], writes=['B10'])
        shifted(('wgl', 0), Ft[0], 'F0')
        op('act', lambda e: e.activation(out=sgl[:], in_=Ft[0][:, HALO:HALO + BLK], func=AF.Sigmoid), reads=['F0'], writes=['B11'])
        for p in range(c.WP):
            ka = pc[('ka', p)]
            op('dve', lambda e, p=p, ka=ka: e.tensor_scalar(out=omka[:, p:p + 1], in0=par[l][:, ka:ka + 1], scalar1=-1.0, scalar2=1.0,
                                                             op0=ALU.mult, op1=ALU.add), reads=[f'par{l}'], writes=['omka'])
        def pair_body(p):
            rs, ks, vs = Ft[0], Ft[1], Ft[2]
            lw, cl, at, kap, kmod, ak, Et, gT, bv = Ft[4], Ft[5], Ft[6], Ft[7], Ft[8], Ft[9], Ft[10], Ft[11], Ft[2]
            vTb, bh, kh = Bt[0], Bt[3], Bt[4]
            AR = ARv[0]
            BZ = BZv[0]
            KZ = KZv[0]
            H0 = slice(HALO, HALO + BLK)
            if p == 0:
                shifted(('wr', p), rs, 'F0')
                shifted(('wk', p), ks, 'F1')
            shifted(('wv', p), vs, 'F2')
            op('act', lambda e: e.activation(out=vTb[:], in_=vs[:, H0], func=AF.Copy), reads=['F2'], writes=['B0'])
            cols = slice(p * 128, (p + 1) * 128)
            for j in range(NT):
                sl = slice(j * TT, (j + 1) * TT)
                b1 = bank()
                op('pe', lambda e, b1=b1, sl=sl: e.matmul(banks[b1][:, 0:TT], lhsT=lorab[l][:, 0, cols], rhs=tw[:, sl], start=True, stop=True),
                   reads=[f'lora{l}', 'B9'], writes=[bk(b1)])
                op('act', lambda e, b1=b1, sl=sl: e.activation(out=lw[:, sl], in_=banks[b1][:, 0:TT], func=AF.Sigmoid,
                                                               bias=P(l, ('w0', p))), reads=[bk(b1), f'par{l}'], writes=['F4'])
                b2 = bank()
                op('pe', lambda e, b2=b2, sl=sl: e.matmul(banks[b2][:, 0:TT], lhsT=lorab[l][:, 1, cols], rhs=alb[:, sl], start=True, stop=True),
                   reads=[f'lora{l}', 'B10'], writes=[bk(b2)])
                op('act', lambda e, b2=b2, sl=sl: e.activation(out=at[:, sl], in_=banks[b2][:, 0:TT], func=AF.Sigmoid,
                                                               bias=P(l, ('a0', p))), reads=[bk(b2), f'par{l}'], writes=['F6'])
                b3 = bank()
                op('pe', lambda e, b3=b3, sl=sl: e.matmul(banks[b3][:, 0:TT], lhsT=lorab[l][:, 2, cols], rhs=sgl[:, sl], start=True, stop=True),
                   reads=[f'lora{l}', 'B11'], writes=[bk(b3)])
                op('act', lambda e, b3=b3, sl=sl: e.activation(out=gT[:, sl], in_=banks[b3][:, 0:TT], func=AF.Copy), reads=[bk(b3)], writes=['F11'])
            op('dve', lambda e: e.tensor_scalar(out=lw[:, 0:BLK], in0=lw[:, 0:BLK], scalar1=-math.exp(-0.5), scalar2=None, op0=ALU.mult),
               reads=['F4'], writes=['F4'])
            op('dve', lambda e: e.tensor_scalar(out=kap[:, 0:BLK], in0=ks[:, H0], scalar1=P(l, ('kk', p)), scalar2=None, op0=ALU.mult),
               reads=['F1', f'par{l}'], writes=['F7'])
            op('act', lambda e: e.activation(out=bh[:], in_=kap[:, 0:BLK], func=AF.Square), reads=['F7'], writes=['B3'])
            for j in range(NT):
                sl = slice(j * TT, (j + 1) * TT)
                b1 = bank()
                op('pe', lambda e, b1=b1, sl=sl: e.matmul(banks[b1][:, 0:TT], lhsT=bonesb[:], rhs=bh[:, sl], start=True, stop=True),
                   reads=['bonesb', 'B3'], writes=[bk(b1)])
                op('act', lambda e, b1=b1, sl=sl: e.activation(out=tmp[:, sl], in_=banks[b1][:, 0:TT], func=AF.Sqrt), reads=[bk(b1)], writes=['F3'])
            op('dve', lambda e: e.tensor_scalar(out=tmp[:, 0:BLK], in0=tmp[:, 0:BLK], scalar1=1e-12, scalar2=None, op0=ALU.max), reads=['F3'], writes=['F3'])
            op('dve', lambda e: e.reciprocal(out=tmp[:, 0:BLK], in_=tmp[:, 0:BLK]), reads=['F3'], writes=['F3'])
            op('dve', lambda e: e.tensor_tensor(out=kap[:, 0:BLK], in0=kap[:, 0:BLK], in1=tmp[:, 0:BLK], op=ALU.mult), reads=['F7', 'F3'], writes=['F7'])
            op('dve', lambda e: e.tensor_scalar(out=tmp[:, 0:BLK], in0=at[:, 0:BLK], scalar1=P(l, ('ka', p)), scalar2=omka[:, p:p + 1],
                                                op0=ALU.mult, op1=ALU.add), reads=['F6', f'par{l}', 'omka'], writes=['F3'])
            op('dve', lambda e: e.tensor_tensor(out=kmod[:, 0:BLK], in0=ks[:, H0], in1=tmp[:, 0:BLK], op=ALU.mult), reads=['F1', 'F3'], writes=['F8'])
            op('dve', lambda e: e.scalar_tensor_tensor(out=kh[:], in0=rs[:, H0], scalar=P(l, ('rk', p)), in1=kmod[:, 0:BLK],
                                                       op0=ALU.mult, op1=ALU.mult), reads=['F0', 'F8', f'par{l}'], writes=['B4'])
            for j in range(NT):
                sl = slice(j * TT, (j + 1) * TT)
                b1 = bank()
                op('pe', lambda e, b1=b1, sl=sl: e.matmul(banks[b1][:, 0:TT], lhsT=bonesb[:], rhs=kh[:, sl], start=True, stop=True),
                   reads=['bonesb', 'B4'], writes=[bk(b1)])
                op('dve', lambda e, b1=b1, j=j: e.tensor_tensor(out=bv[:, HALO + j * TT:HALO + (j + 1) * TT], in0=banks[b1][:, 0:TT], in1=vs[:, HALO + j * TT:HALO + (j + 1) * TT], op=ALU.mult),
                   reads=[bk(b1), 'F2'], writes=['F2'])
            op('dve', lambda e: e.tensor_tensor_scan(out=cl[:, 0:BLK], data0=resetm[:], data1=lw[:, 0:BLK], initial=0.0,
                                                     op0=ALU.mult, op1=ALU.add), reads=['resetm', 'F4'], writes=['F5'])
            op('dve', lambda e: e.tensor_tensor(out=ak[:, 0:BLK], in0=at[:, 0:BLK], in1=kap[:, 0:BLK], op=ALU.mult), reads=['F6', 'F7'], writes=['F9'])
            op('dve', lambda e: e.tensor_tensor(out=tmp[:, 0:BLK], in0=cl[:, 0:BLK], in1=lw[:, 0:BLK], op=ALU.subtract), reads=['F5', 'F4'], writes=['F3'])
            op('act', lambda e: e.activation(out=Et[:, 0:BLK], in_=tmp[:, 0:BLK], func=AF.Exp), reads=['F3'], writes=['F10'])
            op('dve', lambda e: e.scalar_tensor_tensor(out=AR[:, :, 0, :], in0=kap[:, 0:BLK].rearrange("p (a b) -> p a b", b=L), scalar=-1.0,
                                                       in1=Et[:, 0:BLK].rearrange("p (a b) -> p a b", b=L), op0=ALU.mult, op1=ALU.mult),
               reads=['F7', 'F10'], writes=['AR'])
            op('act', lambda e: e.activation(out=Et[:, 0:BLK], in_=cl[:, 0:BLK], func=AF.Exp), reads=['F5'], writes=['F10'])
            op('dve', lambda e: e.tensor_tensor(out=AR[:, :, 1, :], in0=rs[:, H0].rearrange("p (a b) -> p a b", b=L),
                                                in1=Et[:, 0:BLK].rearrange("p (a b) -> p a b", b=L), op=ALU.mult),
               reads=['F0', 'F10'], writes=['AR'])
            op('dve', lambda e: e.tensor_copy(out=WLt[:], in_=Et[:, L - 1:BLK:L]), reads=['F10'], writes=['WLt'])
            op('act', lambda e: e.activation(out=Et[:, 0:BLK], in_=cl[:, 0:BLK], func=AF.Exp, scale=-1.0), reads=['F5'], writes=['F10'])
            for h in range(2):
                hs = slice(h * 64, (h + 1) * 64)
                op('dve', lambda e, h=h, hs=hs: e.tensor_tensor(out=BZ[hs, h, :], in0=ak[hs, 0:BLK], in1=Et[hs, 0:BLK], op=ALU.mult),
                   reads=['F9', 'F10'], writes=['BZ'])
                op('dve', lambda e, h=h, hs=hs: e.tensor_tensor(out=KZ[hs, h, :], in0=kmod[hs, 0:BLK], in1=Et[hs, 0:BLK], op=ALU.mult),
                   reads=['F8', 'F10'], writes=['KZ'])
            clL = cl[:, L - 1:BLK:L].unsqueeze(2).to_broadcast([128, NCH, L])
            op('dve', lambda e: e.tensor_tensor(out=tmp[:, 0:BLK].rearrange("p (a b) -> p a b", b=L), in0=clL,
                                                in1=cl[:, 0:BLK].rearrange("p (a b) -> p a b", b=L), op=ALU.subtract), reads=['F5'], writes=['F3'])
            op('act', lambda e: e.activation(out=Et[:, 0:BLK], in_=tmp[:, 0:BLK], func=AF.Exp), reads=['F3'], writes=['F10'])
            op('dve', lambda e: e.tensor_tensor(out=bh[:], in0=ak[:, 0:BLK], in1=Et[:, 0:BLK], op=ALU.mult), reads=['F9', 'F10'], writes=['B3'])
            op('dve', lambda e: e.tensor_tensor(out=kh[:], in0=kmod[:, 0:BLK], in1=Et[:, 0:BLK], op=ALU.mult), reads=['F8', 'F10'], writes=['B4'])
            if first:
                op('dve', lambda e, p=p: e.memset(Mf[l][p][:], 0.0), writes=[f'Mf{l}_{p}'])
            Mfp, Mkey = Mf[l][p], f'Mf{l}_{p}'
            def chunk_body(ci):
                par = ci % 2
                S.set(par)
                cs = slice(ci * L, (ci + 1) * L)
                bt_ = bank()
                transpose_to(bt_, 0, vTb[:, cs], ['B0'])
                transpose_to(bt_, 128, bh[:, cs], ['B3'])
                transpose_to(bt_, 256, kh[:, cs], ['B4'])
                op('act', lambda e: e.activation(out=S.tok3[:].rearrange("p a b -> p (a b)"), in_=bbf(bt_)[:, 0:384], func=AF.Copy),
                   reads=[bk(bt_)], writes=[S.k('tok3')])
                yield
                S.set(par)
                ARc = AR[:, ci, :, :].rearrange("p a b -> p (a b)")
                bn = bank()
                for h in range(2):
                    bs = bank()
                    op('pe', lambda e, h=h, bs=bs: e.matmul(banks[bs][:, 0:256], lhsT=BZ[:, h, cs], rhs=ARc, start=True, stop=True),
                       reads=['BZ', 'AR'], writes=[bk(bs)])
                    op('pe', lambda e, h=h, bs=bs: e.matmul(banks[bs][:, 256:512], lhsT=KZ[:, h, cs], rhs=ARc, start=True, stop=True),
                       reads=['KZ', 'AR'], writes=[bk(bs)])
                    op('dve', lambda e, h=h, bs=bs: e.tensor_tensor(out=S.SC2[:, h, :, :].rearrange("p a b -> p (a b)"), in0=banks[bs][:, 0:512],
                                                                     in1=mask4[:].rearrange("p a b -> p (a b)"), op=ALU.mult),
                       reads=[bk(bs), 'mask4'], writes=[S.k('SC2')])
                    op('pe', lambda e, h=h: e.matmul(banks[bn][:, h * 128:(h + 1) * 128], lhsT=AR[:, ci, 0, :], rhs=BZ[:, h, cs], start=True, stop=True),
                       reads=['AR', 'BZ'], writes=[bk(bn)])
                op('dve', lambda e: e.tensor_tensor(out=S.Q0t[:].rearrange("p a b -> p (a b)"), in0=banks[bn][:, 0:256],
                                                    in1=mN2[:].rearrange("p a b -> p (a b)"), op=ALU.mult),
                   reads=[bk(bn), 'mN2'], writes=[S.k('Q0t')])
                op('dve', lambda e: e.tensor_tensor(out=S.Tb2[0][:], in0=S.SC2[:, :, 0, :], in1=id2[:], op=ALU.add),
                   reads=[S.k('SC2'), 'id2'], writes=[S.k('Tb2_0')])
                yield
                S.set(par)
                NLEV = 7
                tcur = 0
                for lev in range(NLEV - 1):
                    nxt = lev % 2
                    bp = bank()
                    for h in range(2):
                        if lev == 0:
                            pc_, pk_ = S.SC2[:, h, 0, :], S.k('SC2')
                            qc_, qk_ = S.Q0t[:, h, :], S.k('Q0t')
                        else:
                            pc_, pk_ = S.PQ[1 - nxt][:, h, 0, :], S.k(f'PQ{1 - nxt}')
                            qc_, qk_ = S.PQ[1 - nxt][:, h, 1, :], S.k(f'PQ{1 - nxt}')
                        if lev < NLEV - 2:
                            op('pe', lambda e, h=h, bp=bp, pc_=pc_, qc_=qc_: e.matmul(banks[bp][:, h * 256:h * 256 + 128], lhsT=qc_, rhs=pc_, start=True, stop=True),
                               reads=[pk_, qk_], writes=[bk(bp)])
                        op('pe', lambda e, h=h, bp=bp, pc_=pc_, qc_=qc_: e.matmul(banks[bp][:, h * 256 + 128:h * 256 + 256], lhsT=pc_, rhs=qc_, start=True, stop=True),
                           reads=[pk_, qk_], writes=[bk(bp)])
                    if lev < NLEV - 2:
                        op('act', lambda e, bp=bp, nxt=nxt: e.activation(out=S.PQ[nxt][:].rearrange("p a b c -> p (a b c)"), in_=banks[bp][:, 0:512], func=AF.Copy),
                           reads=[bk(bp)], writes=[S.k(f'PQ{nxt}')])
                    else:
                        op('act', lambda e, bp=bp, nxt=nxt: e.activation(out=S.PQ[nxt][:, :, 1, :], in_=banks[bp][:, 0:512].rearrange("p (a b c) -> p a b c", a=2, b=2)[:, :, 1, :], func=AF.Copy),
                           reads=[bk(bp)], writes=[S.k(f'PQ{nxt}')])
                    yield
                    S.set(par)
                    bt3 = bank()
                    for h in range(2):
                        op('pe', lambda e, h=h, bt3=bt3, nxt=nxt, tcur=tcur: e.matmul(banks[bt3][:, h * 128:(h + 1) * 128], lhsT=S.PQ[nxt][:, h, 1, :],
                                                                                 rhs=S.Tb2[tcur][:, h, :], start=True, stop=True),
                           reads=[S.k(f'PQ{nxt}'), S.k(f'Tb2_{tcur}')], writes=[bk(bt3)])
                    op('dve', lambda e, bt3=bt3, tcur=tcur: e.tensor_tensor(out=S.Tb2[1 - tcur][:].rearrange("p a b -> p (a b)"),
                                                                           in0=S.Tb2[tcur][:].rearrange("p a b -> p (a b)"), in1=banks[bt3][:, 0:256], op=ALU.add),
                       reads=[S.k(f'Tb2_{tcur}'), bk(bt3)], writes=[S.k(f'Tb2_{1 - tcur}')])
                    tcur = 1 - tcur
                    yield
                    S.set(par)
                tfin = tcur
                for h in range(2):
                    op('act', lambda e, h=h: e.activation(out=S.Sz[h * 64:(h + 1) * 64, h, 0:64], in_=Mfp[h * 64:(h + 1) * 64, :], func=AF.Copy),
                       reads=[Mkey], writes=[S.k('Sz')])
                bx = bank()
                for h in range(2):
                    op('pe', lambda e, h=h: e.matmul(banks[bx][:, h * 64:(h + 1) * 64], lhsT=AR[:, ci, 0, :], rhs=S.Sz[:, h, 0:64], start=True, stop=False),
                       reads=['AR', S.k('Sz')], writes=[bk(bx)])
                    op('pe', lambda e, h=h: e.matmul(banks[bx][:, h * 64:(h + 1) * 64], lhsT=S.SC2[:, h, 2, :], rhs=S.tok3[:, 0, h * 64:(h + 1) * 64], start=False, stop=True),
                       reads=[S.k('SC2'), S.k('tok3')], writes=[bk(bx)])
                op('act', lambda e: e.activation(out=S.XU[:, 0, :, :].rearrange("p a b -> p (a b)"), in_=banks[bx][:, 0:128], func=AF.Copy), reads=[bk(bx)], writes=[S.k('XU')])
                yield
                S.set(par)
                bu_ = bank()
                for h in range(2):
                    op('pe', lambda e, h=h: e.matmul(banks[bu_][:, h * 64:(h + 1) * 64], lhsT=S.Tb2[tfin][:, h, :], rhs=S.XU[:, 0, h, :], start=True, stop=True),
                       reads=[S.k(f'Tb2_{tfin}'), S.k('XU')], writes=[bk(bu_)])
                op('act', lambda e: e.activation(out=S.XU[:, 1, :, :].rearrange("p a b -> p (a b)"), in_=banks[bu_][:, 0:128], func=AF.Copy), reads=[bk(bu_)], writes=[S.k('XU')])
                yield
                S.set(par)
                by = bank()
                for h in range(2):
                    op('pe', lambda e, h=h: e.matmul(banks[by][:, h * 64:(h + 1) * 64], lhsT=AR[:, ci, 1, :], rhs=S.Sz[:, h, 0:64], start=True, stop=False),
                       reads=['AR', S.k('Sz')], writes=[bk(by)])
                    op('pe', lambda e, h=h: e.matmul(banks[by][:, h * 64:(h + 1) * 64], lhsT=S.SC2[:, h, 1, :], rhs=S.XU[:, 1, h, :], start=False, stop=False),
                       reads=[S.k('SC2'), S.k('XU')], writes=[bk(by)])
                    op('pe', lambda e, h=h: e.matmul(banks[by][:, h * 64:(h + 1) * 64], lhsT=S.SC2[:, h, 3, :], rhs=S.tok3[:, 0, h * 64:(h + 1) * 64], start=False, stop=True),
                       reads=[S.k('SC2'), S.k('tok3')], writes=[bk(by)])
                op('act', lambda e: e.activation(out=S.hpre[:].rearrange("p a b -> p (a b)"), in_=banks[by][:, 0:128], func=AF.Copy), reads=[bk(by)], writes=[S.k('hpre')])
                yield
                S.set(par)
                bm = bank()
                op('pe', lambda e: e.matmul(banks[bm][:, 0:128], lhsT=S.tok3[:, 1, :], rhs=S.XU[:, 1, :, :].rearrange("p a b -> p (a b)"), start=True, stop=False),
                   reads=[S.k('tok3'), S.k('XU')], writes=[bk(bm)])
                op('pe', lambda e: e.matmul(banks[bm][:, 0:128], lhsT=S.tok3[:, 2, :], rhs=S.tok3[:, 0, :], start=False, stop=True),
                   reads=[S.k('tok3')], writes=[bk(bm)])
                for h in range(2):
                    hs = slice(h * 64, (h + 1) * 64)
                    op('dve', lambda e, h=h, hs=hs: e.scalar_tensor_tensor(out=Mfp[hs, :], in0=Mfp[hs, :], scalar=WLt[hs, ci:ci + 1],
                                                                           in1=banks[bm][hs, h * 64:(h + 1) * 64], op0=ALU.mult, op1=ALU.add),
                       reads=[Mkey, 'WLt', bk(bm)], writes=[Mkey])

                def fin(bt2, cs):
                    op('dve', lambda e: e.tensor_scalar(out=S.ytmp[:], in0=bbf(bt2)[:, 0:128], scalar1=P(l, ('lg', p)), scalar2=P(l, ('lb', p)),
                                                        op0=ALU.mult, op1=ALU.add), reads=[bk(bt2), f'par{l}'], writes=[S.k('ytmp')])
                    op('dve', lambda e: e.tensor_tensor(out=S.ytmp[:], in0=S.ytmp[:], in1=bv[:, HALO + cs.start:HALO + cs.stop], op=ALU.add), reads=[S.k('ytmp'), 'F2'], writes=[S.k('ytmp')])
                    op('dve', lambda e: e.tensor_tensor(out=yT[:, c.MP + c.RP + p, cs], in0=S.ytmp[:], in1=gT[:, cs], op=ALU.mult),
                       reads=[S.k('ytmp'), 'F11'], writes=[('yT', c.MP + c.RP + p)])
                finish_chunk(ci, c.MP + c.RP + p, 2, fin)

            if p + 1 < c.WP:
                shifted(('wr', p + 1), rs, 'F0')
                shifted(('wk', p + 1), ks, 'F1')
            gens = [chunk_body(ci) for ci in range(NCH)]
            fin_ = [False] * NCH
            OFF = PIPE_OFF
            t_ = 0
            while not all(fin_):
                for ci in range(NCH):
                    if ci * OFF <= t_ and not fin_[ci]:
                        try:
                            next(gens[ci])
                        except StopIteration:
                            fin_[ci] = True
                t_ += 1
            S.set(0)

        for p in range(c.WP):
            pair_body(p)

    ARv = [k.sb([128, NCH, 2, L], BF16, "AR")]
    BZv = [k.sb([128, 2, BLK], BF16, "BZ")]
    kzt_view[0] = BZv[0]
    KZv = [k.sb([128, 2, BLK], BF16, "KZ")]
    resetm = k.sb([128, BLK], BF16, "resetm")
    op('dve', lambda e: e.memset(resetm[:], 1.0), writes=['resetm'])
    op('dve', lambda e: e.memset(resetm[:, 0:BLK:L], 0.0), writes=['resetm'])
    op('dve', lambda e: e.memset(BZv[0][:], 0.0), writes=['BZ'])
    op('dve', lambda e: e.memset(KZv[0][:], 0.0), writes=['KZ'])
    for _p in range(2):
        S.set(_p)
        op('dve', lambda e: e.memset(S.Sz[:], 0.0), writes=[S.k('Sz')])
    S.set(0)

    def wout_ffn(l):
        for o in range(KD):
            si = load_slab(wout_d[l, o], KM * 128)
            for j in range(NT):
                ts = slice(j * TT, (j + 1) * TT)
                b = bank()
                for kk in range(KM):
                    op('pe', lambda e, b=b, kk=kk, ts=ts, si=si: e.matmul(banks[b][:, 0:TT], lhsT=slabs[si][:, kk * 128:(kk + 1) * 128],
                                                                          rhs=yT[:, kk, ts], start=(kk == 0), stop=(kk == KM - 1)),
                       reads=[f'slab{si}', ('yT', kk)], writes=[bk(b)])
                op('dve', lambda e, b=b, o=o, ts=ts: e.tensor_tensor(out=xT[:, o, ts], in0=xT[:, o, ts], in1=banks[b][:, 0:TT], op=ALU.add),
                   reads=[bk(b), ('xT', j)], writes=[('xT', j)])
        rmsnorm(l, 'ln2')
        def aT(f):
            t = Ft[f // 2]
            v = t[:].bitcast(BF16)
            return v[:, (f % 2) * BLK:(f % 2) * BLK + BLK], f'F{f // 2}'
        sgt = Bt[0]
        for f in range(NF):
            si = load_slab(wgu_d[l, f], 2 * KD * 128)
            av, akey = aT(f)
            for j in range(NT):
                ts = slice(j * TT, (j + 1) * TT)
                bg, bu = bank(), bank()
                for gu, b in ((0, bg), (1, bu)):
                    for kk in range(KD):
                        off = (gu * KD + kk) * 128
                        op('pe', lambda e, b=b, kk=kk, ts=ts, si=si, off=off: e.matmul(banks[b][:, 0:TT], lhsT=slabs[si][:, off:off + 128],
                                                                                       rhs=hT[:, kk, ts], start=(kk == 0), stop=(kk == KD - 1)),
                           reads=[f'slab{si}', ('hT', j)], writes=[bk(b)])
                wk_ = [('B0f', j)] + (['B0'] if f == 0 else [])
                rk_ = [('B0f', j)] + (['B0'] if f == NF - 1 else [])
                op('act', lambda e, bg=bg, ts=ts: e.activation(out=sgt[:, ts], in_=banks[bg][:, 0:TT], func=AF.Silu), reads=[bk(bg)], writes=wk_)
                op('dve', lambda e, bu=bu, ts=ts, av=av: e.tensor_tensor(out=av[:, ts], in0=sgt[:, ts], in1=banks[bu][:, 0:TT], op=ALU.mult),
                   reads=rk_ + [bk(bu)], writes=[akey])
        for o in range(KD):
            sis = [load_slab(wdn_d[l, o][:, 0:NFH * 128], NFH * 128), load_slab(wdn_d[l, o][:, NFH * 128:NF * 128], (NF - NFH) * 128)]
            for j in range(NT):
                ts = slice(j * TT, (j + 1) * TT)
                b = bank()
                for f in range(NF):
                    av, akey = aT(f)
                    si = sis[f // NFH]
                    fo_ = (f % NFH) * 128
                    op('pe', lambda e, b=b, f=f, ts=ts, si=si, av=av, fo_=fo_: e.matmul(banks[b][:, 0:TT], lhsT=slabs[si][:, fo_:fo_ + 128],
                                                                               rhs=av[:, ts], start=(f == 0), stop=(f == NF - 1)),
                       reads=[f'slab{si}', akey], writes=[bk(b)])
                op('dve', lambda e, b=b, o=o, ts=ts: e.tensor_tensor(out=xT[:, o, ts], in0=xT[:, o, ts], in1=banks[b][:, 0:TT], op=ALU.add),
                   reads=[bk(b), ('xT', j)], writes=[('xT', j)])

    TPT = TT // 128
    for s in range(c.NSEQ):
        for blk in range(c.NBLK):
            first = (blk == 0)
            t0 = blk * BLK
            for tt in range(BLK // 128):
                dma('sp', xin[:], x_d[s, t0 + tt * 128:t0 + (tt + 1) * 128, :], writes=['xin'])
                for k4 in range(0, KD, 4):
                    b = bank()
                    n4 = min(4, KD - k4)
                    for q in range(n4):
                        op('pe', lambda e, b=b, q=q, k4=k4: e.transpose(out=banks[b][:, q * 128:(q + 1) * 128], in_=xin[:, (k4 + q) * 128:(k4 + q + 1) * 128],
                                                                         identity=C('ident', 128)), reads=['xin', 'cst'], writes=[bk(b)])
                    op('act', lambda e, b=b, k4=k4, n4=n4, tt=tt: e.activation(out=xT[:, k4:k4 + n4, tt * 128:(tt + 1) * 128],
                                                                               in_=banks[b][:, 0:n4 * 128].rearrange("p (a b) -> p a b", b=128), func=AF.Copy),
                       reads=[bk(b)], writes=[('xT', tt // TPT)])
            for l in range(c.DEPTH):
                PH = getattr(c, 'phases', 'nmrwf')
                if 'n' in PH:
                    rmsnorm(l, 'ln1')
                if 'm' in PH:
                    mlstm(l, first)
                if 'r' in PH:
                    retention(l, first, blk)
                if 'w' in PH:
                    rwkv(l, first)
                if 'f' in PH:
                    wout_ffn(l)
            fo = Ft[0]
            for n in range(BLK // TN):
                t0w = n * TN
                j = t0w // TT
                rstd, rkey = rstd_tile(t0w)
                for t8 in range(TN // 128):
                    tsl = slice(t0w + t8 * 128, t0w + (t8 + 1) * 128)
                    for kk in range(KD):
                        op('dve', lambda e, kk=kk, tsl=tsl, t8=t8: e.scalar_tensor_tensor(out=fo[:, kk * 128:(kk + 1) * 128], in0=xT[:, kk, tsl], scalar=C('lnf', 1, kk),
                                                                                          in1=rstd[:, t8 * 128:(t8 + 1) * 128], op0=ALU.mult, op1=ALU.mult),
                           reads=[('xT', j), rkey, 'cst'], writes=['F0'])
                    for k4 in range(0, KD, 4):
                        b2 = bank()
                        n4 = min(4, KD - k4)
                        for q in range(n4):
                            op('pe', lambda e, b2=b2, q=q, k4=k4: e.transpose(out=banks[b2][:, q * 128:(q + 1) * 128], in_=fo[:, (k4 + q) * 128:(k4 + q + 1) * 128],
                                                                               identity=C('ident', 128)), reads=['F0', 'cst'], writes=[bk(b2)])
                        op('act', lambda e, b2=b2, k4=k4, n4=n4: e.activation(out=xin[:, k4 * 128:(k4 + n4) * 128], in_=banks[b2][:, 0:n4 * 128], func=AF.Copy),
                           reads=[bk(b2)], writes=['xin'])
                    dma('sp', out_d[s, t0 + tsl.start:t0 + tsl.stop, :], xin[:], reads=['xin'], is_output=True)
    k.emit()
    return nc


_CACHE = {}


def kernel(**inputs):
    cfg = Cfg()
    packed = pack_host(cfg, inputs)
    if 'nc' not in _CACHE:
        _CACHE['nc'] = build(cfg)
    nc = _CACHE['nc']
    x = np.asarray(inputs['x'], np.float32)
    in_maps = []
    for i in range(8):
        m = dict(packed)
        m['x'] = np.ascontiguousarray(x[i * cfg.NSEQ:(i + 1) * cfg.NSEQ])
        m['win'] = packed['win'].reshape(cfg.DEPTH, cfg.NG, 128, cfg.KD * 128)
        m['wout'] = packed['wout'].reshape(cfg.DEPTH, cfg.KD, 128, cfg.KM * 128)
        m['wgu'] = packed['wgu'].reshape(cfg.DEPTH, cfg.NF, 128, 2 * cfg.KD * 128)
        m['wdn'] = packed['wdn'].reshape(cfg.DEPTH, cfg.KD, 128, cfg.NF * 128)
        in_maps.append(m)
    res = run_bass_kernel_spmd(nc, in_maps, core_ids=list(range(8)))
    return np.concatenate([r['out'] for r in res.results], axis=0).astype(np.float32)
```

```python
import math
DBG = 99
VAR = ''
from contextlib import ExitStack
import numpy as np
import concourse.bass as bass
import concourse.mybir as mybir
from concourse.alu_op_type import AluOpType as ALU
from concourse.bass_utils import run_bass_kernel_spmd

F32 = mybir.dt.float32
BF16 = mybir.dt.bfloat16
AF = mybir.ActivationFunctionType
AX = mybir.AxisListType

ENGS = ['pe', 'dve', 'act', 'pool', 'sp']
NDMASEM = 8
HALO = 4
DMASCR = 16384
SAME_ENGINE_SYNC = True
NO_SELF_SYNC = ('act',)
PIPE_OFF = 9
L = 128


class _Rec:
    def __init__(self):
        self.call = None

    def __getattr__(self, name):
        def f(*a, **kw):
            self.call = (name, a, kw)
            return self
        return f


class TS:
    def __init__(self):
        object.__setattr__(self, 'sets', [{}, {}])
        object.__setattr__(self, 'par', 0)

    def add(self, name, t0, t1):
        self.sets[0][name] = t0
        self.sets[1][name] = t1

    def set(self, par):
        object.__setattr__(self, 'par', par)

    def __getattr__(self, name):
        return self.sets[self.par][name]

    def k(self, name):
        return f'{name}@{self.par}'


class KB:
    def __init__(self, nc):
        self.nc = nc
        self.q = {e: [] for e in ENGS}
        self.cnt = {e: 0 for e in ENGS}
        self.lastw = {}
        self.readers = {}
        self.seen = {e: {} for e in ENGS}
        self.dma_i = {e: 0 for e in ENGS}
        self.dma_cnt = {}
        self.out_events = []
        self.stack = ExitStack()
        self.ntile = 0
        self.bank_i = 0

    def sb(self, shape, dt=F32, name=None):
        self.ntile += 1
        return self.stack.enter_context(self.nc.sbuf_tensor(name or f"t{self.ntile}", list(shape), dt))

    def ps(self, shape, dt=F32, name=None):
        self.ntile += 1
        return self.stack.enter_context(self.nc.psum_tensor(name or f"p{self.ntile}", list(shape), dt))

    def _deps(self, reads, writes):
        ev = []
        for k in reads:
            if k in self.lastw:
                ev.append(self.lastw[k])
        for k in writes:
            if k in self.lastw:
                ev.append(self.lastw[k])
            ev.extend(self.readers.get(k, []))
        return ev

    def _filter(self, eng, evs):
        best = {}
        for (s, v) in evs:
            if s[0] == 'eng' and s[1] == eng and (eng == 'pe' or eng in NO_SELF_SYNC or not SAME_ENGINE_SYNC):
                continue
            if self.seen[eng].get(s, 0) >= v:
                continue
            if best.get(s, 0) < v:
                best[s] = v
        for s, v in best.items():
            self.seen[eng][s] = v
        return list(best.items())

    def _record(self, ev, reads, writes):
        for k in writes:
            self.lastw[k] = ev
            self.readers[k] = []
        for k in reads:
            self.readers.setdefault(k, []).append(ev)

    def op(self, eng, fn, reads=(), writes=()):
        waits = self._filter(eng, self._deps(reads, writes))
        self.cnt[eng] += 1
        ev = (('eng', eng), self.cnt[eng])
        rec = _Rec()
        fn(rec)
        call = rec.call
        self.q[eng].append(('op', waits, lambda e, call=call: getattr(e, call[0])(*call[1], **call[2])))
        self._record(ev, reads, writes)
        return ev

    def dma(self, eng, out, in_, reads=(), writes=(), is_output=False):
        i = self.dma_i[eng] % NDMASEM
        self.dma_i[eng] += 1
        s = ('dma', eng, i)
        prev = self.dma_cnt.get(s, 0)
        evs = self._deps(reads, writes)
        if prev:
            evs.append((s, prev * 16))
        waits = self._filter(eng, evs)
        self.dma_cnt[s] = prev + 1
        ev = (s, (prev + 1) * 16)
        self.q[eng].append(('dma', waits, (out, in_), s))
        self._record(ev, reads, writes)
        if is_output:
            self.out_events.append(ev)
        return ev

    def emit(self):
        nc = self.nc
        semh = {}
        with ExitStack() as st:
            for e in ENGS:
                semh[('eng', e)] = st.enter_context(nc.semaphore(f"s_{e}"))
            for s in self.dma_cnt:
                semh[s] = st.enter_context(nc.semaphore(f"d_{s[1]}_{s[2]}"))
            fin = self._filter('sp', self.out_events)
            with nc.Block() as block:
                def run(eng_name, engine):
                    me = semh[('eng', eng_name)]
                    for item in self.q[eng_name]:
                        if item[0] == 'op':
                            _, waits, fn = item
                            for s, v in waits:
                                engine.wait_ge(semh[s], v)
                            fn(engine).then_inc(me, 1)
                        else:
                            _, waits, (out, in_), s = item
                            for ss, v in waits:
                                engine.wait_ge(semh[ss], v)
                            engine.dma_start(out=out, in_=in_).then_inc(semh[s], 16)
                    if eng_name == 'sp':
                        for s, v in fin:
                            engine.wait_ge(semh[s], v)

                @block.tensor
                def _(e):
                    run('pe', e)

                @block.vector
                def _(e):
                    run('dve', e)

                @block.scalar
                def _(e):
                    run('act', e)

                @block.gpsimd
                def _(e):
                    run('pool', e)

                @block.sync
                def _(e):
                    run('sp', e)
        self.stack.close()


class Cfg:
    def __init__(s, D=1024, SEQ=2048, NSEQ=2, BLK=1024, DEPTH=2, MH=4, RH=6, WH=6, DFF=2816,
                 WL=64, AL=64, GL=128):
        s.D, s.SEQ, s.NSEQ, s.BLK, s.DEPTH = D, SEQ, NSEQ, BLK, DEPTH
        s.MH, s.RH, s.WH, s.DFF, s.WL, s.AL, s.GL = MH, RH, WH, DFF, WL, AL, GL
        s.KD = D // 128
        s.NBLK = SEQ // BLK
        s.NCH = BLK // L
        s.TT = min(512, BLK)
        s.NT = BLK // s.TT
        s.MP, s.RP, s.WP = MH // 2, RH // 2, WH // 2
        s.NF = DFF // 128
        s.MW, s.RW, s.WW = MH * 64, RH * 64, WH * 64
        s.MIX = s.MW + s.RW + s.WW
        s.KM = s.MIX // 128
        s.MCOLS = 4 * s.MW + 2 * MH
        s.RCOLS = 4 * s.RW
        s.WCOLS = 3 * s.WW + WL + AL + GL
        g = []
        mb = 0
        for p in range(s.MP):
            for nm, base in (('mq', 0), ('mk', s.MW), ('mv', 2 * s.MW), ('mo', 3 * s.MW)):
                g.append(((nm, p), [mb + base + 128 * p + i for i in range(128)]))
        g.append((('mgi', 0), [mb + 4 * s.MW + i for i in range(MH)]))
        g.append((('mgf', 0), [mb + 4 * s.MW + MH + i for i in range(MH)]))
        rb = s.MCOLS
        swap = [h * 64 + ((j + 32) % 64) for h in range(2) for j in range(64)]
        for p in range(s.RP):
            for nm, base in (('rq', 0), ('rk', s.RW), ('rv', 2 * s.RW), ('rg', 3 * s.RW)):
                g.append(((nm, p), [rb + base + 128 * p + i for i in range(128)]))
                if nm in ('rq', 'rk'):
                    g.append(((nm + 's', p), [rb + base + 128 * p + swap[i] for i in range(128)]))
        wb = s.MCOLS + s.RCOLS
        g.append((('wwl', 0), [wb + 3 * s.WW + i for i in range(WL)]))
        g.append((('wal', 0), [wb + 3 * s.WW + WL + i for i in range(AL)]))
        g.append((('wgl', 0), [wb + 3 * s.WW + WL + AL + i for i in range(GL)]))
        for p in range(s.WP):
            for nm, base in (('wr', 0), ('wk', s.WW), ('wv', 2 * s.WW)):
                g.append(((nm, p), [wb + base + 128 * p + i for i in range(128)]))
        s.groups = g
        s.gidx = {k: i for i, (k, _) in enumerate(g)}
        s.NG = len(g)
        pc = {}
        n = 0

        def add(name, w=1):
            nonlocal n
            pc[name] = n
            n += w
        add('ln1', s.KD)
        add('ln2', s.KD)
        for p in range(s.MP):
            add(('cq', p), 4)
            add(('ck', p), 4)
            add(('mlg', p))
        add('bi')
        add('bf')
        for key, _ in g:
            if key[0][0] == 'w':
                add(('mu', key))
        for p in range(s.WP):
            for nm in ('w0', 'a0', 'kk', 'ka', 'rk', 'lg', 'lb'):
                add((nm, p))
        s.pc = pc
        s.NPAR = n
        cc = {}
        n = 0

        def addc(name, w):
            nonlocal n
            cc[name] = n
            n += w
        addc('ident', 128)
        addc('mincl', 128)
        addc('mstrict', 128)
        addc('mN', 128)
        addc('bones', 128)
        addc('es_r', RH)
        addc('ft_r', RH)
        addc('dec_r', s.RP)
        addc('sel', s.MP * 128)
        addc('lnf', s.KD)
        s.cc = cc
        s.NC = n


def pack_host(cfg, inp):
    c = cfg
    f32 = np.float32
    D, KD = c.D, c.KD
    w_in = np.asarray(inp['w_in'], f32)
    win = np.zeros((c.DEPTH, c.NG, 128, KD, 128), f32)
    for gi, (key, cols) in enumerate(c.groups):
        sub = w_in[:, :, cols]
        sub = sub.reshape(c.DEPTH, KD, 128, len(cols)).transpose(0, 2, 1, 3)
        win[:, gi, :, :, :len(cols)] = sub
    wout = np.asarray(inp['w_out'], f32).reshape(c.DEPTH, c.KM, 128, KD, 128).transpose(0, 3, 2, 1, 4)
    wg = np.asarray(inp['w_gate'], f32).reshape(c.DEPTH, KD, 128, c.NF, 128).transpose(0, 3, 2, 1, 4)
    wu = np.asarray(inp['w_up'], f32).reshape(c.DEPTH, KD, 128, c.NF, 128).transpose(0, 3, 2, 1, 4)
    wgu = np.stack([wg, wu], axis=3)
    wdn = np.asarray(inp['w_down'], f32).reshape(c.DEPTH, c.NF, 128, KD, 128).transpose(0, 3, 2, 1, 4)
    lora = np.zeros((c.DEPTH, 3, 128, c.WW), f32)
    lora[:, 0, :c.WL] = inp['rw_w_up']
    lora[:, 1, :c.AL] = inp['rw_a_up']
    lora[:, 2, :c.GL] = inp['rw_g_up']
    par = np.zeros((c.DEPTH, 128, c.NPAR), f32)
    pc = c.pc
    par[:, :, pc['ln1']:pc['ln1'] + KD] = np.asarray(inp['ln1_g'], f32).reshape(c.DEPTH, KD, 128).transpose(0, 2, 1)
    par[:, :, pc['ln2']:pc['ln2'] + KD] = np.asarray(inp['ln2_g'], f32).reshape(c.DEPTH, KD, 128).transpose(0, 2, 1)
    mconv = np.asarray(inp['m_conv'], f32)
    for p in range(c.MP):
        par[:, :, pc[('cq', p)]:pc[('cq', p)] + 4] = mconv[:, :, 128 * p:128 * p + 128].transpose(0, 2, 1)
        par[:, :, pc[('ck', p)]:pc[('ck', p)] + 4] = mconv[:, :, c.MW + 128 * p:c.MW + 128 * p + 128].transpose(0, 2, 1)
        par[:, :, pc[('mlg', p)]] = np.asarray(inp['m_ln_g'], f32)[:, 128 * p:128 * p + 128]
    par[:, :c.MH, pc['bi']] = inp['m_b_i']
    par[:, :c.MH, pc['bf']] = inp['m_b_f']
    mu = np.asarray(inp['rw_mu'], f32)
    wbase = c.MCOLS + c.RCOLS
    for key, cols in c.groups:
        if key[0][0] == 'w':
            par[:, :len(cols), pc[('mu', key)]] = mu[:, [cc - wbase for cc in cols]]
    for p in range(c.WP):
        sl = slice(128 * p, 128 * p + 128)
        for nm, src in (('w0', 'rw_w0'), ('a0', 'rw_a0'), ('kk', 'rw_k_k'), ('ka', 'rw_k_a'),
                        ('rk', 'rw_r_k'), ('lg', 'rw_ln_g'), ('lb', 'rw_ln_b')):
            par[:, :, pc[(nm, p)]] = np.asarray(inp[src], f32)[:, sl]
    cst = np.zeros((128, c.NC), f32)
    cc = c.cc
    i = np.arange(128)
    cst[:, cc['ident']:cc['ident'] + 128] = np.eye(128)
    cst[:, cc['mincl']:cc['mincl'] + 128] = (i[:, None] <= i[None, :])
    cst[:, cc['mstrict']:cc['mstrict'] + 128] = (i[:, None] < i[None, :])
    cst[:, cc['mN']:cc['mN'] + 128] = (i[None, :] < i[:, None])
    cst[:, cc['bones']:cc['bones'] + 128] = (i[:, None] // 64 == i[None, :] // 64)
    gam = 1.0 - 2.0 ** (-5.0 - np.arange(c.RH, dtype=np.float64))
    cst[:, cc['es_r']:cc['es_r'] + c.RH] = gam[None, :] ** (L - 1.0 - i[:, None])
    cst[:, cc['ft_r']:cc['ft_r'] + c.RH] = gam[None, :] ** (i[:, None] - (L - 1.0))
    for p in range(c.RP):
        cst[:64, cc['dec_r'] + p] = gam[2 * p] ** L
        cst[64:, cc['dec_r'] + p] = gam[2 * p + 1] ** L
    for p in range(c.MP):
        for m in range(128):
            cst[2 * p + m // 64, cc['sel'] + p * 128 + m] = 1.0
    cst[:, cc['lnf']:cc['lnf'] + KD] = np.asarray(inp['lnf_g'], f32).reshape(KD, 128).T
    theta = 1.0 / (10000.0 ** np.linspace(0.0, 1.0, 32, dtype=np.float32))
    ang = np.arange(c.SEQ, dtype=np.float32)[None, :] * theta.astype(np.float32)[:, None]
    cs, sn = np.cos(ang).astype(f32), np.sin(ang).astype(f32)
    rot = np.zeros((2, 128, c.SEQ), f32)
    for h in range(2):
        rot[0, h * 64:h * 64 + 32] = cs
        rot[0, h * 64 + 32:h * 64 + 64] = cs
        rot[1, h * 64:h * 64 + 32] = -sn
        rot[1, h * 64 + 32:h * 64 + 64] = sn
    return dict(win=win, wout=np.ascontiguousarray(wout), wgu=np.ascontiguousarray(wgu),
                wdn=np.ascontiguousarray(wdn), lora=lora, par=par, cst=cst, rot=rot)


def build(cfg):
    c = cfg
    nc = bass.Bass("TRN2", target_bir_lowering=False, dynamic_dma_scratch_size=DMASCR)
    KD, BLK, TT, NT, NCH, NF, KM = c.KD, c.BLK, c.TT, c.NT, c.NCH, c.NF, c.KM
    x_d = nc.dram_tensor("x", [c.NSEQ, c.SEQ, c.D], F32, kind="ExternalInput").ap()
    win_d = nc.dram_tensor("win", [c.DEPTH, c.NG, 128, KD * 128], F32, kind="ExternalInput").ap()
    wout_d = nc.dram_tensor("wout", [c.DEPTH, KD, 128, KM * 128], F32, kind="ExternalInput").ap()
    wgu_d = nc.dram_tensor("wgu", [c.DEPTH, NF, 128, 2 * KD * 128], F32, kind="ExternalInput").ap()
    wdn_d = nc.dram_tensor("wdn", [c.DEPTH, KD, 128, NF * 128], F32, kind="ExternalInput").ap()
    lora_d = nc.dram_tensor("lora", [c.DEPTH, 3, 128, c.WW], F32, kind="ExternalInput").ap()
    par_d = nc.dram_tensor("par", [c.DEPTH, 128, c.NPAR], F32, kind="ExternalInput").ap()
    cst_d = nc.dram_tensor("cst", [128, c.NC], F32, kind="ExternalInput").ap()
    rot_d = nc.dram_tensor("rot", [2, 128, c.SEQ], F32, kind="ExternalInput").ap()
    out_d = nc.dram_tensor("out", [c.NSEQ, c.SEQ, c.D], F32, kind="ExternalOutput").ap()

    k = KB(nc)
    op, dma = k.op, k.dma
    xT = k.sb([128, KD, BLK], F32, "xT")
    hT = k.sb([128, KD, BLK], BF16, "hT")
    yT = k.sb([128, KM, BLK], BF16, "yT")
    NFH = (NF + 1) // 2
    SLABW = max(KD * 128, KM * 128, 2 * KD * 128, NFH * 128)
    NSLAB = 5
    slabs = [k.sb([128, SLABW], BF16, f"slab{i}") for i in range(NSLAB)]
    NFT = 12
    FW = max(HALO + BLK, c.D)
    Ft = [k.sb([128, FW], F32, f"F{i}") for i in range(NFT)]
    Bt = {i: k.sb([128, BLK], BF16, f"B{i}") for i in (0, 3, 4, 9, 10, 11)}
    cst = k.sb([128, c.NC], F32, "cst_sb")
    par = [k.sb([128, c.NPAR], F32, f"par_sb{l}") for l in range(c.DEPTH)]
    lorab = [k.sb([128, 3, c.WW], BF16, f"lora_sb{l}") for l in range(c.DEPTH)]
    identb = k.sb([128, 128], BF16, "identb")
    bonesb = k.sb([128, 128], BF16, "bonesb")
    mask4 = k.sb([128, 4, 128], BF16, "mask4")
    xin = k.sb([128, c.D], F32, "xin")
    TN = min(256, TT)
    rstd2 = [k.sb([128, TN], F32, f"rstd{i}") for i in range(2)]
    tok3 = k.sb([128, 3, 128], BF16, "tok3")
    vaug = k.sb([128, 2, 66], BF16, "vaug")
    ATt = [k.sb([128, 128], BF16, f"AT{h}") for h in range(2)]
    SC2 = k.sb([128, 2, 4, 128], BF16, "SC2")
    Q0t = k.sb([128, 2, 128], BF16, "Q0t")
    PQ = [k.sb([128, 2, 2, 128], BF16, f"PQ{i}") for i in range(2)]
    Tb2 = [k.sb([128, 2, 128], BF16, f"Tb2_{i}") for i in range(2)]
    mN2 = k.sb([128, 2, 128], BF16, "mN2")
    id2 = k.sb([128, 2, 128], BF16, "id2")
    XU = k.sb([128, 2, 2, 64], BF16, "XU")
    hpre = k.sb([128, 2, 64], F32, "hpre")
    ndsb = k.sb([128, 2, 66], F32, "ndsb")
    hn = k.sb([128, 128], BF16, "hn")
    st6 = k.sb([128, 2, 6], F32, "st6")
    mv = k.sb([128, 2, 2], F32, "mv")
    sm = k.sb([128, 8], F32, "sm")
    ytmp = k.sb([128, 128], F32, "ytmp")
    omka = k.sb([128, c.WP], F32, "omka")
    Cf = [[k.sb([128, 65], F32, f"Cf{l}_{p}") for p in range(c.MP)] for l in range(c.DEPTH)]
    Rf = [[k.sb([128, 64], F32, f"Rf{l}_{p}") for p in range(c.RP)] for l in range(c.DEPTH)]
    Mf = [[k.sb([128, 64], F32, f"Mf{l}_{p}") for p in range(c.WP)] for l in range(c.DEPTH)]
    Sz = k.sb([128, 2, 66], BF16, "Sz")
    halo_keys = [key for key, _ in c.groups if key[0] in ('mq', 'mk') or key[0][0] == 'w']
    halo = {(l, key): k.sb([128, HALO], F32, f"halo{l}_{key[0]}{key[1]}") for l in range(c.DEPTH) for key in halo_keys}
    gcar = [k.sb([c.MH, 2], F32, f"gcar{l}") for l in range(c.DEPTH)]
    Rall = k.sb([c.MH, NCH + 1], F32, "Rall")
    decrow = k.sb([c.MH, NCH], F32, "decrow")
    decb = k.sb([128, NCH], F32, "decb")
    est = k.sb([128, NCH, c.MH], F32, "est")
    thrt = k.sb([128, NCH, c.MH], F32, "thrt")
    WLt = k.sb([128, NCH], F32, "WLt")
    S = TS()
    _shapes = dict(tok3=([128, 3, 128], BF16), Sz=([128, 2, 66], BF16), SC2=([128, 2, 4, 128], BF16), Q0t=([128, 2, 128], BF16),
                   XU=([128, 2, 2, 64], BF16), hpre=([128, 2, 64], F32), hn=([128, 128], BF16), st6=([128, 2, 6], F32),
                   mv=([128, 2, 2], F32), sm=([128, 8], F32), ytmp=([128, 128], F32), vaug=([128, 2, 66], BF16), ndsb=([128, 2, 66], F32))
    _first = dict(tok3=tok3, Sz=Sz, SC2=SC2, Q0t=Q0t, XU=XU, hpre=hpre, hn=hn, st6=st6, mv=mv, sm=sm, ytmp=ytmp, vaug=vaug, ndsb=ndsb)
    for _n, (_sh, _dt) in _shapes.items():
        S.add(_n, _first[_n], k.sb(_sh, _dt, _n + "_b"))
    S.add('PQ', PQ, [k.sb([128, 2, 2, 128], BF16, f"PQb{i}") for i in range(2)])
    S.add('Tb2', Tb2, [k.sb([128, 2, 128], BF16, f"Tb2b_{i}") for i in range(2)])
    S.add('ATt', ATt, [k.sb([128, 128], BF16, f"ATb{h}") for h in range(2)])
    banks = [k.ps([128, 512], F32, f"bank{i}") for i in range(8)]

    def bank():
        i = k.bank_i % 8
        k.bank_i += 1
        return i

    def bk(i):
        return ('bank', i)

    def bbf(i):
        return banks[i][:].bitcast(BF16)

    cc = c.cc
    pc = c.pc

    def C(name, w=1, off=0):
        return cst[:, cc[name] + off:cc[name] + off + w]

    dma('sp', cst[:], cst_d, writes=['cst'])
    for l in range(c.DEPTH):
        dma('sp', par[l][:], par_d[l], writes=[f'par{l}'])
        dma('pool', lorab[l][:], lora_d[l].rearrange("a p w -> p a w"), writes=[f'lora{l}'])
    op('dve', lambda e: e.tensor_copy(out=identb[:], in_=C('ident', 128)), reads=['cst'], writes=['identb'])
    op('dve', lambda e: e.tensor_copy(out=bonesb[:], in_=C('bones', 128)), reads=['cst'], writes=['bonesb'])
    for i, nm in enumerate(('mstrict', 'mincl', 'mstrict', 'mincl')):
        op('dve', lambda e, i=i, nm=nm: e.tensor_copy(out=mask4[:, i, :], in_=C(nm, 128)), reads=['cst'], writes=['mask4'])

    for h in range(2):
        op('dve', lambda e, h=h: e.tensor_copy(out=mN2[:, h, :], in_=C('mN', 128)), reads=['cst'], writes=['mN2'])
        op('dve', lambda e, h=h: e.tensor_copy(out=id2[:, h, :], in_=C('ident', 128)), reads=['cst'], writes=['id2'])
    slab_i = [0]

    def load_slab(src_ap, width):
        i = slab_i[0] % NSLAB
        slab_i[0] += 1
        dma('pool', slabs[i][:, 0:width], src_ap, writes=[f'slab{i}'])
        return i

    def P(l, name, w=1):
        return par[l][:, pc[name]:pc[name] + w]

    def rstd_tile(t0w):
        j = t0w // TT
        ri = (t0w // TN) % 2
        rstd, rkey = rstd2[ri], f'rstd{ri}'
        sq = Ft[11][:].bitcast(BF16)[:, 0:KD * TN].rearrange("p (a b) -> p a b", b=TN)
        op('act', lambda e: e.activation(out=sq, in_=xT[:, :, t0w:t0w + TN], func=AF.Square),
           reads=[('xT', j)], writes=['F11'])
        b = bank()
        for kk in range(KD):
            op('pe', lambda e, b=b, kk=kk: e.matmul(banks[b][:, 0:TN], lhsT=bonesall[:], rhs=sq[:, kk, :],
                                                     start=(kk == 0), stop=(kk == KD - 1)),
               reads=['F11', 'bonesall'], writes=[bk(b)])
        op('act', lambda e, b=b: e.activation(out=rstd[:], in_=banks[b][:, 0:TN], func=AF.Sqrt,
                                              scale=1.0 / c.D, bias=epsc[:, 0:1]),
           reads=[bk(b), 'epsc'], writes=[rkey])
        op('dve', lambda e: e.reciprocal(out=rstd[:], in_=rstd[:]), reads=[rkey], writes=[rkey])
        return rstd, rkey

    def rmsnorm(l, gname):
        for n in range(BLK // TN):
            t0w = n * TN
            j = t0w // TT
            ts = slice(t0w, t0w + TN)
            rstd, rkey = rstd_tile(t0w)
            for kk in range(KD):
                gap = par[l][:, pc[gname] + kk:pc[gname] + kk + 1]
                op('dve', lambda e, kk=kk, ts=ts, gap=gap, rstd=rstd: e.scalar_tensor_tensor(
                    out=hT[:, kk, ts], in0=xT[:, kk, ts], scalar=gap, in1=rstd[:], op0=ALU.mult, op1=ALU.mult),
                   reads=[('xT', j), rkey, f'par{l}'], writes=[('hT', j)])

    bonesall = k.sb([128, 128], BF16, "bonesall")
    epsc = k.sb([128, 4], F32, "epsc")
    op('dve', lambda e: e.memset(bonesall[:], 1.0), writes=['bonesall'])
    op('dve', lambda e: e.memset(epsc[:, 0:1], 1e-6), writes=['epsc'])
    op('dve', lambda e: e.memset(epsc[:, 1:2], 1e-5), writes=['epsc'])
    op('dve', lambda e: e.memset(epsc[:, 2:3], 64e-5), writes=['epsc'])
    op('dve', lambda e: e.memset(epsc[:, 3:4], 1.0), writes=['epsc'])

    def project(l, key, evac):
        gi = c.gidx[key]
        si = load_slab(win_d[l, gi], KD * 128)
        for j in range(NT):
            b = bank()
            for kk in range(KD):
                op('pe', lambda e, b=b, kk=kk, j=j, si=si: e.matmul(
                    banks[b][:, 0:TT], lhsT=slabs[si][:, kk * 128:(kk + 1) * 128], rhs=hT[:, kk, j * TT:(j + 1) * TT],
                    start=(kk == 0), stop=(kk == KD - 1)),
                   reads=[f'slab{si}', ('hT', j)], writes=[bk(b)])
            evac(b, j)

    def raw_evac(dst, dkey, l, key, first):
        def prep():
            if first:
                op('dve', lambda e: e.memset(dst[:, 0:HALO], 0.0), writes=[dkey])
            else:
                op('dve', lambda e: e.tensor_copy(out=dst[:, 0:HALO], in_=halo[(l, key)][:]),
                   reads=[('halo', l, key)], writes=[dkey])

        def ev(b, j):
            op('act', lambda e: e.activation(out=dst[:, HALO + j * TT:HALO + (j + 1) * TT], in_=banks[b][:, 0:TT],
                                             func=AF.Copy), reads=[bk(b)], writes=[dkey])

        def fin():
            op('dve', lambda e: e.tensor_copy(out=halo[(l, key)][:], in_=dst[:, BLK:BLK + HALO]),
               reads=[dkey], writes=[('halo', l, key)])
        return prep, ev, fin

    def proj_raw(l, key, dst, dkey, first):
        prep, ev, fin = raw_evac(dst, dkey, l, key, first)
        prep()
        project(l, key, ev)
        fin()

    def proj_simple(l, key, dst_ap_fn, dkey, func=AF.Copy):
        def ev(b, j):
            op('act', lambda e: e.activation(out=dst_ap_fn(j), in_=banks[b][:, 0:TT], func=func),
               reads=[bk(b)], writes=[dkey])
        project(l, key, ev)

    def tok_ln(eps_col):
        for h in range(2):
            op('dve', lambda e, h=h: e.bn_stats(out=S.st6[:, h, :], in_=S.hpre[:, h, :]), reads=[S.k('hpre')], writes=[S.k('st6')])
            op('dve', lambda e, h=h: e.bn_aggr(out=S.mv[:, h, :], in_=S.st6[:, h, :]), reads=[S.k('st6')], writes=[S.k('mv')])
        op('act', lambda e: e.activation(out=S.sm[:, 0:2], in_=S.mv[:, :, 1], func=AF.Sqrt, bias=epsc[:, eps_col:eps_col + 1]),
           reads=[S.k('mv'), 'epsc'], writes=[S.k('sm')])
        op('dve', lambda e: e.reciprocal(out=S.sm[:, 2:4], in_=S.sm[:, 0:2]), reads=[S.k('sm')], writes=[S.k('sm')])
        for h in range(2):
            op('dve', lambda e, h=h: e.tensor_scalar(out=S.hn[:, h * 64:(h + 1) * 64], in0=S.hpre[:, h, :],
                                                      scalar1=S.mv[:, h, 0:1], scalar2=S.sm[:, 2 + h:3 + h],
                                                      op0=ALU.subtract, op1=ALU.mult),
               reads=[S.k('hpre'), S.k('mv'), S.k('sm')], writes=[S.k('hn')])

    def transpose_to(bi, col, src_ap, rkeys):
        op('pe', lambda e: e.transpose(out=bbf(bi)[:, col:col + 128], in_=src_ap, identity=identb[:]),
           reads=list(rkeys) + ['identb'], writes=[bk(bi)])

    def linattn_chunk(ci, qc, qkey, kz, kzkey, vT, vkey, es_fn, es_keys, Sf, Sfkey, dec_ap, dec_keys, naug,
                      post):
        cs = slice(ci * L, (ci + 1) * L)
        W = 64 + naug
        bt_ = bank()
        transpose_to(bt_, 0, vT[:, cs], [vkey])
        transpose_to(bt_, 128, kz[:, 0, cs], [kzkey])
        transpose_to(bt_, 256, kz[:, 1, cs], [kzkey])
        op('act', lambda e: e.activation(out=S.tok3[:].rearrange("p a b -> p (a b)"), in_=bbf(bt_)[:, 0:384], func=AF.Copy),
           reads=[bk(bt_)], writes=[S.k('tok3')])
        for h in range(2):
            op('dve', lambda e, h=h: e.tensor_scalar(out=S.vaug[:, h, 0:64], in0=S.tok3[:, 0, h * 64:(h + 1) * 64],
                                                      scalar1=es_fn(h), scalar2=None, op0=ALU.mult),
               reads=[S.k('tok3')] + es_keys, writes=[S.k('vaug')])
            if naug:
                op('act', lambda e, h=h: e.activation(out=S.vaug[:, h, 64:65], in_=es_fn(h), func=AF.Copy),
                   reads=es_keys, writes=[S.k('vaug')])
        if DBG <= 2:
            return
        op('dve', lambda e: e.tensor_scalar(out=Sf[:, 0:W], in0=Sf[:, 0:W], scalar1=dec_ap, scalar2=None, op0=ALU.mult),
           reads=[Sfkey] + dec_keys, writes=[Sfkey])
        for h in range(2):
            op('act', lambda e, h=h: e.activation(out=S.Sz[h * 64:(h + 1) * 64, h, 0:W], in_=Sf[h * 64:(h + 1) * 64, 0:W],
                                                  func=AF.Copy), reads=[Sfkey], writes=[S.k('Sz')])
        if DBG <= 3:
            return
        for h in range(2):
            bs = bank()
            op('pe', lambda e, h=h, bs=bs: e.matmul(banks[bs][:, 0:128], lhsT=kz[:, h, cs], rhs=qc[:, cs], start=True, stop=True),
               reads=[kzkey, qkey], writes=[bk(bs)])
            op('dve', lambda e, h=h, bs=bs: e.tensor_tensor(out=S.ATt[h][:], in0=banks[bs][:, 0:128], in1=C('mincl', 128), op=ALU.mult),
               reads=[bk(bs), 'cst'], writes=[S.k(f'AT{h}')])
        bo = bank()
        for h in range(2):
            op('pe', lambda e, h=h: e.matmul(banks[bo][:, h * 128:h * 128 + W], lhsT=S.ATt[h][:], rhs=S.vaug[:, h, 0:W], start=True, stop=False),
               reads=[S.k(f'AT{h}'), S.k('vaug')], writes=[bk(bo)])
            op('pe', lambda e, h=h: e.matmul(banks[bo][:, h * 128:h * 128 + W], lhsT=qc[:, cs], rhs=S.Sz[:, h, 0:W], start=False, stop=True),
               reads=[qkey, S.k('Sz')], writes=[bk(bo)])
        if DBG <= 4 or DBG in (41, 42):
            return
        bu = bank()
        for h in range(2):
            op('pe', lambda e, h=h: e.matmul(banks[bu][:, 0:W], lhsT=S.tok3[:, 1 + h, :], rhs=S.vaug[:, h, 0:W], start=(h == 0), stop=(h == 1)),
               reads=[S.k('tok3'), S.k('vaug')], writes=[bk(bu)])
        op('dve', lambda e: e.tensor_tensor(out=Sf[:, 0:W], in0=Sf[:, 0:W], in1=banks[bu][:, 0:W], op=ALU.add),
           reads=[Sfkey, bk(bu)], writes=[Sfkey])
        if DBG <= 5:
            return
        post(bo)

    def finish_chunk(ci, pair_idx, eps_col, fin_fn):
        tok_ln(eps_col)
        bt2 = bank()
        transpose_to(bt2, 0, S.hn[:], [S.k('hn')])
        fin_fn(bt2, slice(ci * L, (ci + 1) * L))

    def mlstm(l, first):
        onesF = Ft[11]
        MH = c.MH
        ipre, lt, Bc, gt, Gt, esr, thr = Ft[4], Ft[5], Ft[6], Ft[7], Ft[8], Ft[9], Ft[10]

        def ev_gi(b, j):
            op('act', lambda e: e.activation(out=ipre[0:MH, j * TT:(j + 1) * TT], in_=banks[b][0:MH, 0:TT], func=AF.Identity,
                                             bias=par[l][0:MH, pc['bi']:pc['bi'] + 1]), reads=[bk(b), f'par{l}'], writes=['F4'])
        project(l, ('mgi', 0), ev_gi)

        def ev_gf(b, j):
            sl = slice(j * TT, (j + 1) * TT)
            op('act', lambda e: e.activation(out=lt[0:MH, sl], in_=banks[b][0:MH, 0:TT], func=AF.Identity,
                                             bias=par[l][0:MH, pc['bf']:pc['bf'] + 1]), reads=[bk(b), f'par{l}'], writes=['F5'])
            op('act', lambda e: e.activation(out=lt[0:MH, sl], in_=lt[0:MH, sl], func=AF.Exp, scale=-1.0), reads=['F5'], writes=['F5'])
            op('act', lambda e: e.activation(out=lt[0:MH, sl], in_=lt[0:MH, sl], func=AF.Ln, bias=epsc[0:MH, 3:4]),
               reads=['F5', 'epsc'], writes=['F5'])
        project(l, ('mgf', 0), ev_gf)
        if first:
            op('dve', lambda e: e.memset(gcar[l][:, 0:1], 0.0), writes=[f'gcar{l}'])
            op('dve', lambda e: e.memset(gcar[l][:, 1:2], -1e30), writes=[f'gcar{l}'])
        op('dve', lambda e: e.memset(onesF[0:MH, 0:BLK], 1.0), writes=['F11'])
        op('dve', lambda e: e.tensor_tensor_scan(out=Bc[0:MH, 0:BLK], data0=onesF[0:MH, 0:BLK], data1=lt[0:MH, 0:BLK],
                                                 initial=gcar[l][:, 0:1], op0=ALU.mult, op1=ALU.subtract),
           reads=['F11', 'F5', f'gcar{l}'], writes=['F6'])
        op('dve', lambda e: e.tensor_tensor(out=gt[0:MH, 0:BLK], in0=ipre[0:MH, 0:BLK], in1=Bc[0:MH, 0:BLK], op=ALU.subtract),
           reads=['F4', 'F6'], writes=['F7'])
        op('dve', lambda e: e.tensor_tensor_scan(out=Gt[0:MH, 0:BLK], data0=gt[0:MH, 0:BLK], data1=gt[0:MH, 0:BLK],
                                                 initial=gcar[l][:, 1:2], op0=ALU.max, op1=ALU.max),
           reads=['F7', f'gcar{l}'], writes=['F8'])
        op('dve', lambda e: e.tensor_copy(out=Rall[:, 0:1], in_=gcar[l][:, 1:2]), reads=[f'gcar{l}'], writes=['Rall'])
        op('dve', lambda e: e.tensor_copy(out=Rall[:, 1:NCH + 1], in_=Gt[0:MH, L - 1:BLK:L]), reads=['F8'], writes=['Rall'])
        op('dve', lambda e: e.tensor_copy(out=gcar[l][:, 0:1], in_=Bc[0:MH, BLK - 1:BLK]), reads=['F6'], writes=[f'gcar{l}'])
        op('dve', lambda e: e.tensor_copy(out=gcar[l][:, 1:2], in_=Gt[0:MH, BLK - 1:BLK]), reads=['F8'], writes=[f'gcar{l}'])
        op('dve', lambda e: e.tensor_tensor(out=decrow[:], in0=Rall[:, 0:NCH], in1=Rall[:, 1:NCH + 1], op=ALU.subtract),
           reads=['Rall'], writes=['decrow'])
        op('act', lambda e: e.activation(out=decrow[:], in_=decrow[:], func=AF.Exp), reads=['decrow'], writes=['decrow'])
        rcb = Rall[:, 1:NCH + 1].unsqueeze(2).to_broadcast([MH, NCH, L])
        op('dve', lambda e: e.tensor_tensor(out=esr[0:MH, 0:BLK].rearrange("p (a b) -> p a b", b=L),
                                            in0=gt[0:MH, 0:BLK].rearrange("p (a b) -> p a b", b=L), in1=rcb, op=ALU.subtract),
           reads=['F7', 'Rall'], writes=['F9'])
        op('act', lambda e: e.activation(out=esr[0:MH, 0:BLK], in_=esr[0:MH, 0:BLK], func=AF.Exp), reads=['F9'], writes=['F9'])
        op('dve', lambda e: e.tensor_tensor(out=thr[0:MH, 0:BLK].rearrange("p (a b) -> p a b", b=L),
                                            in0=Bc[0:MH, 0:BLK].rearrange("p (a b) -> p a b", b=L), in1=rcb, op=ALU.add),
           reads=['F6', 'Rall'], writes=['F10'])
        op('act', lambda e: e.activation(out=thr[0:MH, 0:BLK], in_=thr[0:MH, 0:BLK], func=AF.Exp, scale=-1.0), reads=['F10'], writes=['F10'])
        for src, skey, dst, dkey in ((esr, 'F9', est, 'est'), (thr, 'F10', thrt, 'thrt')):
            b = bank()
            for ci in range(NCH):
                op('pe', lambda e, ci=ci, src=src, b=b: e.transpose(out=banks[b][:, ci * MH:(ci + 1) * MH], in_=src[0:MH, ci * L:(ci + 1) * L],
                                                                     identity=C('ident', MH)[0:MH, :]),
                   reads=[skey, 'cst'], writes=[bk(b)])
            op('act', lambda e, b=b, dst=dst: e.activation(out=dst[:].rearrange("p a b -> p (a b)"), in_=banks[b][:, 0:NCH * MH], func=AF.Copy),
               reads=[bk(b)], writes=[dkey])
        for p in range(c.MP):
            praw_q, praw_k, acc = Ft[0], Ft[1], Ft[3]
            qc, vT_, sgo = Bt[0], Bt[3], Bt[4]
            kz = kzt_view[0]
            b = bank()
            op('pe', lambda e, b=b, p=p: e.matmul(banks[b][:, 0:NCH], lhsT=C('sel', 128, p * 128)[0:MH, :], rhs=decrow[:], start=True, stop=True),
               reads=['cst', 'decrow'], writes=[bk(b)])
            op('act', lambda e, b=b: e.activation(out=decb[:], in_=banks[b][:, 0:NCH], func=AF.Copy), reads=[bk(b)], writes=['decb'])
            for nm, praw, pk, cname in (('mq', praw_q, 'F0', 'cq'), ('mk', praw_k, 'F1', 'ck')):
                proj_raw(l, (nm, p), praw, pk, first)
                cw = pc[(cname, p)]
                op('dve', lambda e, praw=praw, cw=cw: e.tensor_scalar(out=acc[:, 0:BLK], in0=praw[:, HALO - 3:HALO - 3 + BLK],
                                                                     scalar1=par[l][:, cw:cw + 1], scalar2=None, op0=ALU.mult),
                   reads=[pk, f'par{l}'], writes=['F3'])
                for jj in range(1, 4):
                    op('dve', lambda e, praw=praw, cw=cw, jj=jj: e.scalar_tensor_tensor(
                        out=acc[:, 0:BLK], in0=praw[:, HALO - 3 + jj:HALO - 3 + jj + BLK], scalar=par[l][:, cw + jj:cw + jj + 1],
                        in1=acc[:, 0:BLK], op0=ALU.mult, op1=ALU.add), reads=[pk, f'par{l}', 'F3'], writes=['F3'])
                if nm == 'mq':
                    op('act', lambda e: e.activation(out=qc[:], in_=acc[:, 0:BLK], func=AF.Silu), reads=['F3'], writes=['B0'])
                else:
                    op('act', lambda e: e.activation(out=acc[:, 0:BLK], in_=acc[:, 0:BLK], func=AF.Silu), reads=['F3'], writes=['F3'])
                    for h in range(2):
                        op('dve', lambda e, h=h: e.tensor_scalar(out=kz[h * 64:(h + 1) * 64, h, :], in0=acc[h * 64:(h + 1) * 64, 0:BLK],
                                                                  scalar1=0.125, scalar2=None, op0=ALU.mult),
                           reads=['F3'], writes=['BZ'])
            proj_simple(l, ('mv', p), lambda j: vT_[:, j * TT:(j + 1) * TT], 'B3')
            proj_simple(l, ('mo', p), lambda j: sgo[:, j * TT:(j + 1) * TT], 'B4', func=AF.Sigmoid)
            if first:
                op('dve', lambda e, p=p: e.memset(Cf[l][p][:], 0.0), writes=[f'Cf{l}_{p}'])
            for ci in range(NCH):
                def post(bo, ci=ci, p=p):
                    op('act', lambda e: e.activation(out=S.ndsb[:, :, 0:65], in_=banks[bo][:, 0:256].rearrange("p (a b) -> p a b", a=2)[:, :, 0:65], func=AF.Copy),
                       reads=[bk(bo)], writes=[S.k('ndsb')])
                    op('act', lambda e: e.activation(out=S.sm[:, 4:6], in_=S.ndsb[:, :, 64], func=AF.Abs), reads=[S.k('ndsb')], writes=[S.k('sm')])
                    op('dve', lambda e: e.tensor_tensor(out=S.sm[:, 4:6], in0=S.sm[:, 4:6], in1=thrt[:, ci, 2 * p:2 * p + 2], op=ALU.max),
                       reads=[S.k('sm'), 'thrt'], writes=[S.k('sm')])
                    op('dve', lambda e: e.reciprocal(out=S.sm[:, 6:8], in_=S.sm[:, 4:6]), reads=[S.k('sm')], writes=[S.k('sm')])
                    for h in range(2):
                        op('dve', lambda e, h=h: e.tensor_scalar(out=S.hpre[:, h, :], in0=S.ndsb[:, h, 0:64],
                                                                  scalar1=S.sm[:, 6 + h:7 + h], scalar2=None, op0=ALU.mult),
                           reads=[S.k('ndsb'), S.k('sm')], writes=[S.k('hpre')])

                    def fin(bt2, cs):
                        op('dve', lambda e: e.scalar_tensor_tensor(out=yT[:, p, cs], in0=bbf(bt2)[:, 0:128],
                                                                   scalar=par[l][:, pc[('mlg', p)]:pc[('mlg', p)] + 1],
                                                                   in1=sgo[:, cs], op0=ALU.mult, op1=ALU.mult),
                           reads=[bk(bt2), f'par{l}', 'B4'], writes=[('yT', p)])
                    finish_chunk(ci, p, 1, fin)
                linattn_chunk(ci, qc, 'B0', kz, 'BZ', vT_, 'B3',
                              lambda h, ci=ci, p=p: est[:, ci, 2 * p + h:2 * p + h + 1], ['est'],
                              Cf[l][p], f'Cf{l}_{p}', decb[:, ci:ci + 1], ['decb'], 1, post)

    kzt_view = [None]

    def retention(l, first, blk):
        cosT, sinT = Ft[11], Ft[10]
        pos = slice(blk * BLK, (blk + 1) * BLK)
        dma('sp', cosT[:, 0:BLK], rot_d[0][:, pos], writes=['F11'])
        dma('sp', sinT[:, 0:BLK], rot_d[1][:, pos], writes=['F10'])
        kz = kzt_view[0]
        for p in range(c.RP):
            t1, t2 = Ft[0], Ft[1]
            qr, vT_, sg = Bt[0], Bt[3], Bt[4]
            for nm in ('rq', 'rk'):
                def ev1(b, j):
                    sl = slice(j * TT, (j + 1) * TT)
                    op('dve', lambda e: e.tensor_tensor(out=t1[:, sl], in0=banks[b][:, 0:TT], in1=cosT[:, sl], op=ALU.mult),
                       reads=[bk(b), 'F11'], writes=['F0'])
                project(l, (nm, p), ev1)

                def ev2(b, j):
                    sl = slice(j * TT, (j + 1) * TT)
                    op('dve', lambda e: e.tensor_tensor(out=t2[:, sl], in0=banks[b][:, 0:TT], in1=sinT[:, sl], op=ALU.mult),
                       reads=[bk(b), 'F10'], writes=['F1'])
                project(l, (nm + 's', p), ev2)
                if nm == 'rq':
                    op('dve', lambda e: e.tensor_tensor(out=qr[:], in0=t1[:, 0:BLK], in1=t2[:, 0:BLK], op=ALU.add),
                       reads=['F0', 'F1'], writes=['B0'])
                else:
                    op('dve', lambda e: e.tensor_tensor(out=t1[:, 0:BLK], in0=t1[:, 0:BLK], in1=t2[:, 0:BLK], op=ALU.add),
                       reads=['F0', 'F1'], writes=['F0'])
                    for h in range(2):
                        op('dve', lambda e, h=h: e.tensor_scalar(out=kz[h * 64:(h + 1) * 64, h, :], in0=t1[h * 64:(h + 1) * 64, 0:BLK],
                                                                  scalar1=0.125, scalar2=None, op0=ALU.mult),
                           reads=['F0'], writes=['BZ'])
            proj_simple(l, ('rv', p), lambda j: vT_[:, j * TT:(j + 1) * TT], 'B3')
            proj_simple(l, ('rg', p), lambda j: sg[:, j * TT:(j + 1) * TT], 'B4', func=AF.Silu)
            if first:
                op('dve', lambda e, p=p: e.memset(Rf[l][p][:], 0.0), writes=[f'Rf{l}_{p}'])
            for ci in range(NCH if DBG > 1 else 0):
                def post(bo, ci=ci, p=p):
                    for h in range(2):
                        op('dve', lambda e, h=h: e.tensor_scalar(out=S.hpre[:, h, :], in0=banks[bo][:, h * 128:h * 128 + 64],
                                                                  scalar1=C('ft_r', 1, 2 * p + h), scalar2=None, op0=ALU.mult),
                           reads=[bk(bo), 'cst'], writes=[S.k('hpre')])

                    def fin(bt2, cs):
                        op('dve', lambda e: e.tensor_tensor(out=yT[:, c.MP + p, cs], in0=bbf(bt2)[:, 0:128], in1=sg[:, cs], op=ALU.mult),
                           reads=[bk(bt2), 'B4'], writes=[('yT', c.MP + p)])
                    finish_chunk(ci, c.MP + p, 1, fin)
                linattn_chunk(ci, qr, 'B0', kz, 'BZ', vT_, 'B3',
                              lambda h, p=p: C('es_r', 1, 2 * p + h), ['cst'],
                              Rf[l][p], f'Rf{l}_{p}', C('dec_r', 1, p), ['cst'], 0, post)

    def rwkv(l, first):
        tw, alb, sgl = Bt[9], Bt[10], Bt[11]
        tmp = Ft[3]

        def shifted(key, dst, dkey):
            proj_raw(l, key, dst, dkey, first)
            mcol = pc[('mu', key)]
            op('dve', lambda e: e.tensor_tensor(out=tmp[:, 0:BLK], in0=dst[:, HALO - 1:HALO - 1 + BLK], in1=dst[:, HALO:HALO + BLK], op=ALU.subtract),
               reads=[dkey], writes=['F3'])
            op('dve', lambda e: e.scalar_tensor_tensor(out=dst[:, HALO:HALO + BLK], in0=tmp[:, 0:BLK], scalar=par[l][:, mcol:mcol + 1],
                                                       in1=dst[:, HALO:HALO + BLK], op0=ALU.mult, op1=ALU.add),
               reads=['F3', dkey, f'par{l}'], writes=[dkey])
        shifted(('wwl', 0), Ft[0], 'F0')
        op('act', lambda e: e.activation(out=tw[:], in_=Ft[0][:, HALO:HALO + BLK], func=AF.Tanh), reads=['F0'], writes=['B9'])
        shifted(('wal', 0), Ft[0], 'F0')
        op('act', lambda e: e.activation(out=alb[:], in_=Ft[0][:, HALO:HALO + BLK], func=AF.Copy), reads=['F0'], writes=['B10'])
        shifted(('wgl', 0), Ft[0], 'F0')
        op('act', lambda e: e.activation(out=sgl[:], in_=Ft[0][:, HALO:HALO + BLK], func=AF.Sigmoid), reads=['F0'], writes=['B11'])
        for p in range(c.WP):
            ka = pc[('ka', p)]
            op('dve', lambda e, p=p, ka=ka: e.tensor_scalar(out=omka[:, p:p + 1], in0=par[l][:, ka:ka + 1], scalar1=-1.0, scalar2=1.0,
                                                             op0=ALU.mult, op1=ALU.add), reads=[f'par{l}'], writes=['omka'])
        def pair_body(p):
            rs, ks, vs = Ft[0], Ft[1], Ft[2]
            lw, cl, at, kap, kmod, ak, Et, gT, bv = Ft[4], Ft[5], Ft[6], Ft[7], Ft[8], Ft[9], Ft[10], Ft[11], Ft[2]
            vTb, bh, kh = Bt[0], Bt[3], Bt[4]
            AR = ARv[0]
            BZ = BZv[0]
            KZ = KZv[0]
            H0 = slice(HALO, HALO + BLK)
            if p == 0:
                shifted(('wr', p), rs, 'F0')
                shifted(('wk', p), ks, 'F1')
            shifted(('wv', p), vs, 'F2')
            op('act', lambda e: e.activation(out=vTb[:], in_=vs[:, H0], func=AF.Copy), reads=['F2'], writes=['B0'])
            cols = slice(p * 128, (p + 1) * 128)
            for j in range(NT):
                sl = slice(j * TT, (j + 1) * TT)
                b1 = bank()
                op('pe', lambda e, b1=b1, sl=sl: e.matmul(banks[b1][:, 0:TT], lhsT=lorab[l][:, 0, cols], rhs=tw[:, sl], start=True, stop=True),
                   reads=[f'lora{l}', 'B9'], writes=[bk(b1)])
                op('act', lambda e, b1=b1, sl=sl: e.activation(out=lw[:, sl], in_=banks[b1][:, 0:TT], func=AF.Sigmoid,
                                                               bias=P(l, ('w0', p))), reads=[bk(b1), f'par{l}'], writes=['F4'])
                b2 = bank()
                op('pe', lambda e, b2=b2, sl=sl: e.matmul(banks[b2][:, 0:TT], lhsT=lorab[l][:, 1, cols], rhs=alb[:, sl], start=True, stop=True),
                   reads=[f'lora{l}', 'B10'], writes=[bk(b2)])
                op('act', lambda e, b2=b2, sl=sl: e.activation(out=at[:, sl], in_=banks[b2][:, 0:TT], func=AF.Sigmoid,
                                                               bias=P(l, ('a0', p))), reads=[bk(b2), f'par{l}'], writes=['F6'])
                b3 = bank()
                op('pe', lambda e, b3=b3, sl=sl: e.matmul(banks[b3][:, 0:TT], lhsT=lorab[l][:, 2, cols], rhs=sgl[:, sl], start=True, stop=True),
                   reads=[f'lora{l}', 'B11'], writes=[bk(b3)])
                op('act', lambda e, b3=b3, sl=sl: e.activation(out=gT[:, sl], in_=banks[b3][:, 0:TT], func=AF.Copy), reads=[bk(b3)], writes=['F11'])
            op('dve', lambda e: e.tensor_scalar(out=lw[:, 0:BLK], in0=lw[:, 0:BLK], scalar1=-math.exp(-0.5), scalar2=None, op0=ALU.mult),
               reads=['F4'], writes=['F4'])
            op('dve', lambda e: e.tensor_scalar(out=kap[:, 0:BLK], in0=ks[:, H0], scalar1=P(l, ('kk', p)), scalar2=None, op0=ALU.mult),
               reads=['F1', f'par{l}'], writes=['F7'])
            op('act', lambda e: e.activation(out=bh[:], in_=kap[:, 0:BLK], func=AF.Square), reads=['F7'], writes=['B3'])
            for j in range(NT):
                sl = slice(j * TT, (j + 1) * TT)
                b1 = bank()
                op('pe', lambda e, b1=b1, sl=sl: e.matmul(banks[b1][:, 0:TT], lhsT=bonesb[:], rhs=bh[:, sl], start=True, stop=True),
                   reads=['bonesb', 'B3'], writes=[bk(b1)])
                op('act', lambda e, b1=b1, sl=sl: e.activation(out=tmp[:, sl], in_=banks[b1][:, 0:TT], func=AF.Sqrt), reads=[bk(b1)], writes=['F3'])
            op('dve', lambda e: e.tensor_scalar(out=tmp[:, 0:BLK], in0=tmp[:, 0:BLK], scalar1=1e-12, scalar2=None, op0=ALU.max), reads=['F3'], writes=['F3'])
            op('dve', lambda e: e.reciprocal(out=tmp[:, 0:BLK], in_=tmp[:, 0:BLK]), reads=['F3'], writes=['F3'])
            op('dve', lambda e: e.tensor_tensor(out=kap[:, 0:BLK], in0=kap[:, 0:BLK], in1=tmp[:, 0:BLK], op=ALU.mult), reads=['F7', 'F3'], writes=['F7'])
            op('dve', lambda e: e.tensor_scalar(out=tmp[:, 0:BLK], in0=at[:, 0:BLK], scalar1=P(l, ('ka', p)), scalar2=omka[:, p:p + 1],
                                                op0=ALU.mult, op1=ALU.add), reads=['F6', f'par{l}', 'omka'], writes=['F3'])
            op('dve', lambda e: e.tensor_tensor(out=kmod[:, 0:BLK], in0=ks[:, H0], in1=tmp[:, 0:BLK], op=ALU.mult), reads=['F1', 'F3'], writes=['F8'])
            op('dve', lambda e: e.scalar_tensor_tensor(out=kh[:], in0=rs[:, H0], scalar=P(l, ('rk', p)), in1=kmod[:, 0:BLK],
                                                       op0=ALU.mult, op1=ALU.mult), reads=['F0', 'F8', f'par{l}'], writes=['B4'])
            for j in range(NT):
                sl = slice(j * TT, (j + 1) * TT)
                b1 = bank()
                op('pe', lambda e, b1=b1, sl=sl: e.matmul(banks[b1][:, 0:TT], lhsT=bonesb[:], rhs=kh[:, sl], start=True, stop=True),
                   reads=['bonesb', 'B4'], writes=[bk(b1)])
                op('dve', lambda e, b1=b1, j=j: e.tensor_tensor(out=bv[:, HALO + j * TT:HALO + (j + 1) * TT], in0=banks[b1][:, 0:TT], in1=vs[:, HALO + j * TT:HALO + (j + 1) * TT], op=ALU.mult),
                   reads=[bk(b1), 'F2'], writes=['F2'])
            op('dve', lambda e: e.tensor_tensor_scan(out=cl[:, 0:BLK], data0=resetm[:], data1=lw[:, 0:BLK], initial=0.0,
                                                     op0=ALU.mult, op1=ALU.add), reads=['resetm', 'F4'], writes=['F5'])
            op('dve', lambda e: e.tensor_tensor(out=ak[:, 0:BLK], in0=at[:, 0:BLK], in1=kap[:, 0:BLK], op=ALU.mult), reads=['F6', 'F7'], writes=['F9'])
            op('dve', lambda e: e.tensor_tensor(out=tmp[:, 0:BLK], in0=cl[:, 0:BLK], in1=lw[:, 0:BLK], op=ALU.subtract), reads=['F5', 'F4'], writes=['F3'])
            op('act', lambda e: e.activation(out=Et[:, 0:BLK], in_=tmp[:, 0:BLK], func=AF.Exp), reads=['F3'], writes=['F10'])
            op('dve', lambda e: e.scalar_tensor_tensor(out=AR[:, :, 0, :], in0=kap[:, 0:BLK].rearrange("p (a b) -> p a b", b=L), scalar=-1.0,
                                                       in1=Et[:, 0:BLK].rearrange("p (a b) -> p a b", b=L), op0=ALU.mult, op1=ALU.mult),
               reads=['F7', 'F10'], writes=['AR'])
            op('act', lambda e: e.activation(out=Et[:, 0:BLK], in_=cl[:, 0:BLK], func=AF.Exp), reads=['F5'], writes=['F10'])
            op('dve', lambda e: e.tensor_tensor(out=AR[:, :, 1, :], in0=rs[:, H0].rearrange("p (a b) -> p a b", b=L),
                                                in1=Et[:, 0:BLK].rearrange("p (a b) -> p a b", b=L), op=ALU.mult),
               reads=['F0', 'F10'], writes=['AR'])
            op('dve', lambda e: e.tensor_copy(out=WLt[:], in_=Et[:, L - 1:BLK:L]), reads=['F10'], writes=['WLt'])
            op('act', lambda e: e.activation(out=Et[:, 0:BLK], in_=cl[:, 0:BLK], func=AF.Exp, scale=-1.0), reads=['F5'], writes=['F10'])
            for h in range(2):
                hs = slice(h * 64, (h + 1) * 64)
                op('dve', lambda e, h=h, hs=hs: e.tensor_tensor(out=BZ[hs, h, :], in0=ak[hs, 0:BLK], in1=Et[hs, 0:BLK], op=ALU.mult),
                   reads=['F9', 'F10'], writes=['BZ'])
                op('dve', lambda e, h=h, hs=hs: e.tensor_tensor(out=KZ[hs, h, :], in0=kmod[hs, 0:BLK], in1=Et[hs, 0:BLK], op=ALU.mult),
                   reads=['F8', 'F10'], writes=['KZ'])
            clL = cl[:, L - 1:BLK:L].unsqueeze(2).to_broadcast([128, NCH, L])
            op('dve', lambda e: e.tensor_tensor(out=tmp[:, 0:BLK].rearrange("p (a b) -> p a b", b=L), in0=clL,
                                                in1=cl[:, 0:BLK].rearrange("p (a b) -> p a b", b=L), op=ALU.subtract), reads=['F5'], writes=['F3'])
            op('act', lambda e: e.activation(out=Et[:, 0:BLK], in_=tmp[:, 0:BLK], func=AF.Exp), reads=['F3'], writes=['F10'])
            op('dve', lambda e: e.tensor_tensor(out=bh[:], in0=ak[:, 0:BLK], in1=Et[:, 0:BLK], op=ALU.mult), reads=['F9', 'F10'], writes=['B3'])
            op('dve', lambda e: e.tensor_tensor(out=kh[:], in0=kmod[:, 0:BLK], in1=Et[:, 0:BLK], op=ALU.mult), reads=['F8', 'F10'], writes=['B4'])
            if first:
                op('dve', lambda e, p=p: e.memset(Mf[l][p][:], 0.0), writes=[f'Mf{l}_{p}'])
            Mfp, Mkey = Mf[l][p], f'Mf{l}_{p}'
            def chunk_body(ci):
                par = ci % 2
                S.set(par)
                cs = slice(ci * L, (ci + 1) * L)
                bt_ = bank()
                transpose_to(bt_, 0, vTb[:, cs], ['B0'])
                transpose_to(bt_, 128, bh[:, cs], ['B3'])
                transpose_to(bt_, 256, kh[:, cs], ['B4'])
                op('act', lambda e: e.activation(out=S.tok3[:].rearrange("p a b -> p (a b)"), in_=bbf(bt_)[:, 0:384], func=AF.Copy),
                   reads=[bk(bt_)], writes=[S.k('tok3')])
                yield
                S.set(par)
                ARc = AR[:, ci, :, :].rearrange("p a b -> p (a b)")
                bn = bank()
                for h in range(2):
                    bs = bank()
                    op('pe', lambda e, h=h, bs=bs: e.matmul(banks[bs][:, 0:256], lhsT=BZ[:, h, cs], rhs=ARc, start=True, stop=True),
                       reads=['BZ', 'AR'], writes=[bk(bs)])
                    op('pe', lambda e, h=h, bs=bs: e.matmul(banks[bs][:, 256:512], lhsT=KZ[:, h, cs], rhs=ARc, start=True, stop=True),
                       reads=['KZ', 'AR'], writes=[bk(bs)])
                    op('dve', lambda e, h=h, bs=bs: e.tensor_tensor(out=S.SC2[:, h, :, :].rearrange("p a b -> p (a b)"), in0=banks[bs][:, 0:512],
                                                                     in1=mask4[:].rearrange("p a b -> p (a b)"), op=ALU.mult),
                       reads=[bk(bs), 'mask4'], writes=[S.k('SC2')])
                    op('pe', lambda e, h=h: e.matmul(banks[bn][:, h * 128:(h + 1) * 128], lhsT=AR[:, ci, 0, :], rhs=BZ[:, h, cs], start=True, stop=True),
                       reads=['AR', 'BZ'], writes=[bk(bn)])
                op('dve', lambda e: e.tensor_tensor(out=S.Q0t[:].rearrange("p a b -> p (a b)"), in0=banks[bn][:, 0:256],
                                                    in1=mN2[:].rearrange("p a b -> p (a b)"), op=ALU.mult),
                   reads=[bk(bn), 'mN2'], writes=[S.k('Q0t')])
                op('dve', lambda e: e.tensor_tensor(out=S.Tb2[0][:], in0=S.SC2[:, :, 0, :], in1=id2[:], op=ALU.add),
                   reads=[S.k('SC2'), 'id2'], writes=[S.k('Tb2_0')])
                yield
                S.set(par)
                NLEV = 7
                tcur = 0
                for lev in range(NLEV - 1):
                    nxt = lev % 2
                    bp = bank()
                    for h in range(2):
                        if lev == 0:
                            pc_, pk_ = S.SC2[:, h, 0, :], S.k('SC2')
                            qc_, qk_ = S.Q0t[:, h, :], S.k('Q0t')
                        else:
                            pc_, pk_ = S.PQ[1 - nxt][:, h, 0, :], S.k(f'PQ{1 - nxt}')
                            qc_, qk_ = S.PQ[1 - nxt][:, h, 1, :], S.k(f'PQ{1 - nxt}')
                        if lev < NLEV - 2:
                            op('pe', lambda e, h=h, bp=bp, pc_=pc_, qc_=qc_: e.matmul(banks[bp][:, h * 256:h * 256 + 128], lhsT=qc_, rhs=pc_, start=True, stop=True),
                               reads=[pk_, qk_], writes=[bk(bp)])
                        op('pe', lambda e, h=h, bp=bp, pc_=pc_, qc_=qc_: e.matmul(banks[bp][:, h * 256 + 128:h * 256 + 256], lhsT=pc_, rhs=qc_, start=True, stop=True),
                           reads=[pk_, qk_], writes=[bk(bp)])
                    if lev < NLEV - 2:
                        op('act', lambda e, bp=bp, nxt=nxt: e.activation(out=S.PQ[nxt][:].rearrange("p a b c -> p (a b c)"), in_=banks[bp][:, 0:512], func=AF.Copy),
                           reads=[bk(bp)], writes=[S.k(f'PQ{nxt}')])
                    else:
                        op('act', lambda e, bp=bp, nxt=nxt: e.activation(out=S.PQ[nxt][:, :, 1, :], in_=banks[bp][:, 0:512].rearrange("p (a b c) -> p a b c", a=2, b=2)[:, :, 1, :], func=AF.Copy),
                           reads=[bk(bp)], writes=[S.k(f'PQ{nxt}')])
                    yield
                    S.set(par)
                    bt3 = bank()
                    for h in range(2):
                        op('pe', lambda e, h=h, bt3=bt3, nxt=nxt, tcur=tcur: e.matmul(banks[bt3][:, h * 128:(h + 1) * 128], lhsT=S.PQ[nxt][:, h, 1, :],
                                                                                 rhs=S.Tb2[tcur][:, h, :], start=True, stop=True),
                           reads=[S.k(f'PQ{nxt}'), S.k(f'Tb2_{tcur}')], writes=[bk(bt3)])
                    op('dve', lambda e, bt3=bt3, tcur=tcur: e.tensor_tensor(out=S.Tb2[1 - tcur][:].rearrange("p a b -> p (a b)"),
                                                                           in0=S.Tb2[tcur][:].rearrange("p a b -> p (a b)"), in1=banks[bt3][:, 0:256], op=ALU.add),
                       reads=[S.k(f'Tb2_{tcur}'), bk(bt3)], writes=[S.k(f'Tb2_{1 - tcur}')])
                    tcur = 1 - tcur
                    yield
                    S.set(par)
                tfin = tcur
                for h in range(2):
                    op('act', lambda e, h=h: e.activation(out=S.Sz[h * 64:(h + 1) * 64, h, 0:64], in_=Mfp[h * 64:(h + 1) * 64, :], func=AF.Copy),
                       reads=[Mkey], writes=[S.k('Sz')])
                bx = bank()
                for h in range(2):
                    op('pe', lambda e, h=h: e.matmul(banks[bx][:, h * 64:(h + 1) * 64], lhsT=AR[:, ci, 0, :], rhs=S.Sz[:, h, 0:64], start=True, stop=False),
                       reads=['AR', S.k('Sz')], writes=[bk(bx)])
                    op('pe', lambda e, h=h: e.matmul(banks[bx][:, h * 64:(h + 1) * 64], lhsT=S.SC2[:, h, 2, :], rhs=S.tok3[:, 0, h * 64:(h + 1) * 64], start=False, stop=True),
                       reads=[S.k('SC2'), S.k('tok3')], writes=[bk(bx)])
                op('act', lambda e: e.activation(out=S.XU[:, 0, :, :].rearrange("p a b -> p (a b)"), in_=banks[bx][:, 0:128], func=AF.Copy), reads=[bk(bx)], writes=[S.k('XU')])
                yield
                S.set(par)
                bu_ = bank()
                for h in range(2):
                    op('pe', lambda e, h=h: e.matmul(banks[bu_][:, h * 64:(h + 1) * 64], lhsT=S.Tb2[tfin][:, h, :], rhs=S.XU[:, 0, h, :], start=True, stop=True),
                       reads=[S.k(f'Tb2_{tfin}'), S.k('XU')], writes=[bk(bu_)])
                op('act', lambda e: e.activation(out=S.XU[:, 1, :, :].rearrange("p a b -> p (a b)"), in_=banks[bu_][:, 0:128], func=AF.Copy), reads=[bk(bu_)], writes=[S.k('XU')])
                yield
                S.set(par)
                by = bank()
                for h in range(2):
                    op('pe', lambda e, h=h: e.matmul(banks[by][:, h * 64:(h + 1) * 64], lhsT=AR[:, ci, 1, :], rhs=S.Sz[:, h, 0:64], start=True, stop=False),
                       reads=['AR', S.k('Sz')], writes=[bk(by)])
                    op('pe', lambda e, h=h: e.matmul(banks[by][:, h * 64:(h + 1) * 64], lhsT=S.SC2[:, h, 1, :], rhs=S.XU[:, 1, h, :], start=False, stop=False),
                       reads=[S.k('SC2'), S.k('XU')], writes=[bk(by)])
                    op('pe', lambda e, h=h: e.matmul(banks[by][:, h * 64:(h + 1) * 64], lhsT=S.SC2[:, h, 3, :], rhs=S.tok3[:, 0, h * 64:(h + 1) * 64], start=False, stop=True),
                       reads=[S.k('SC2'), S.k('tok3')], writes=[bk(by)])
                op('act', lambda e: e.activation(out=S.hpre[:].rearrange("p a b -> p (a b)"), in_=banks[by][:, 0:128], func=AF.Copy), reads=[bk(by)], writes=[S.k('hpre')])
                yield
                S.set(par)
                bm = bank()
                op('pe', lambda e: e.matmul(banks[bm][:, 0:128], lhsT=S.tok3[:, 1, :], rhs=S.XU[:, 1, :, :].rearrange("p a b -> p (a b)"), start=True, stop=False),
                   reads=[S.k('tok3'), S.k('XU')], writes=[bk(bm)])
                op('pe', lambda e: e.matmul(banks[bm][:, 0:128], lhsT=S.tok3[:, 2, :], rhs=S.tok3[:, 0, :], start=False, stop=True),
                   reads=[S.k('tok3')], writes=[bk(bm)])
                for h in range(2):
                    hs = slice(h * 64, (h + 1) * 64)
                    op('dve', lambda e, h=h, hs=hs: e.scalar_tensor_tensor(out=Mfp[hs, :], in0=Mfp[hs, :], scalar=WLt[hs, ci:ci + 1],
                                                                           in1=banks[bm][hs, h * 64:(h + 1) * 64], op0=ALU.mult, op1=ALU.add),
                       reads=[Mkey, 'WLt', bk(bm)], writes=[Mkey])

                def fin(bt2, cs):
                    op('dve', lambda e: e.tensor_scalar(out=S.ytmp[:], in0=bbf(bt2)[:, 0:128], scalar1=P(l, ('lg', p)), scalar2=P(l, ('lb', p)),
                                                        op0=ALU.mult, op1=ALU.add), reads=[bk(bt2), f'par{l}'], writes=[S.k('ytmp')])
                    op('dve', lambda e: e.tensor_tensor(out=S.ytmp[:], in0=S.ytmp[:], in1=bv[:, HALO + cs.start:HALO + cs.stop], op=ALU.add), reads=[S.k('ytmp'), 'F2'], writes=[S.k('ytmp')])
                    op('dve', lambda e: e.tensor_tensor(out=yT[:, c.MP + c.RP + p, cs], in0=S.ytmp[:], in1=gT[:, cs], op=ALU.mult),
                       reads=[S.k('ytmp'), 'F11'], writes=[('yT', c.MP + c.RP + p)])
                finish_chunk(ci, c.MP + c.RP + p, 2, fin)

            if p + 1 < c.WP:
                shifted(('wr', p + 1), rs, 'F0')
                shifted(('wk', p + 1), ks, 'F1')
            gens = [chunk_body(ci) for ci in range(NCH)]
            fin_ = [False] * NCH
            OFF = PIPE_OFF
            t_ = 0
            while not all(fin_):
                for ci in range(NCH):
                    if ci * OFF <= t_ and not fin_[ci]:
                        try:
                            next(gens[ci])
                        except StopIteration:
                            fin_[ci] = True
                t_ += 1
            S.set(0)

        for p in range(c.WP):
            pair_body(p)

    ARv = [k.sb([128, NCH, 2, L], BF16, "AR")]
    BZv = [k.sb([128, 2, BLK], BF16, "BZ")]
    kzt_view[0] = BZv[0]
    KZv = [k.sb([128, 2, BLK], BF16, "KZ")]
    resetm = k.sb([128, BLK], BF16, "resetm")
    op('dve', lambda e: e.memset(resetm[:], 1.0), writes=['resetm'])
    op('dve', lambda e: e.memset(resetm[:, 0:BLK:L], 0.0), writes=['resetm'])
    op('dve', lambda e: e.memset(BZv[0][:], 0.0), writes=['BZ'])
    op('dve', lambda e: e.memset(KZv[0][:], 0.0), writes=['KZ'])
    for _p in range(2):
        S.set(_p)
        op('dve', lambda e: e.memset(S.Sz[:], 0.0), writes=[S.k('Sz')])
    S.set(0)

    def wout_ffn(l):
        for o in range(KD):
            si = load_slab(wout_d[l, o], KM * 128)
            for j in range(NT):
                ts = slice(j * TT, (j + 1) * TT)
                b = bank()
                for kk in range(KM):
                    op('pe', lambda e, b=b, kk=kk, ts=ts, si=si: e.matmul(banks[b][:, 0:TT], lhsT=slabs[si][:, kk * 128:(kk + 1) * 128],
                                                                          rhs=yT[:, kk, ts], start=(kk == 0), stop=(kk == KM - 1)),
                       reads=[f'slab{si}', ('yT', kk)], writes=[bk(b)])
                op('dve', lambda e, b=b, o=o, ts=ts: e.tensor_tensor(out=xT[:, o, ts], in0=xT[:, o, ts], in1=banks[b][:, 0:TT], op=ALU.add),
                   reads=[bk(b), ('xT', j)], writes=[('xT', j)])
        rmsnorm(l, 'ln2')
        def aT(f):
            t = Ft[f // 2]
            v = t[:].bitcast(BF16)
            return v[:, (f % 2) * BLK:(f % 2) * BLK + BLK], f'F{f // 2}'
        sgt = Bt[0]
        for f in range(NF):
            si = load_slab(wgu_d[l, f], 2 * KD * 128)
            av, akey = aT(f)
            for j in range(NT):
                ts = slice(j * TT, (j + 1) * TT)
                bg, bu = bank(), bank()
                for gu, b in ((0, bg), (1, bu)):
                    for kk in range(KD):
                        off = (gu * KD + kk) * 128
                        op('pe', lambda e, b=b, kk=kk, ts=ts, si=si, off=off: e.matmul(banks[b][:, 0:TT], lhsT=slabs[si][:, off:off + 128],
                                                                                       rhs=hT[:, kk, ts], start=(kk == 0), stop=(kk == KD - 1)),
                           reads=[f'slab{si}', ('hT', j)], writes=[bk(b)])
                op('act', lambda e, bg=bg, ts=ts: e.activation(out=sgt[:, ts], in_=banks[bg][:, 0:TT], func=AF.Silu), reads=[bk(bg)], writes=['B0'])
                op('dve', lambda e, bu=bu, ts=ts, av=av: e.tensor_tensor(out=av[:, ts], in0=sgt[:, ts], in1=banks[bu][:, 0:TT], op=ALU.mult),
                   reads=['B0', bk(bu)], writes=[akey])
        for o in range(KD):
            sis = [load_slab(wdn_d[l, o][:, 0:NFH * 128], NFH * 128), load_slab(wdn_d[l, o][:, NFH * 128:NF * 128], (NF - NFH) * 128)]
            for j in range(NT):
                ts = slice(j * TT, (j + 1) * TT)
                b = bank()
                for f in range(NF):
                    av, akey = aT(f)
                    si = sis[f // NFH]
                    fo_ = (f % NFH) * 128
                    op('pe', lambda e, b=b, f=f, ts=ts, si=si, av=av, fo_=fo_: e.matmul(banks[b][:, 0:TT], lhsT=slabs[si][:, fo_:fo_ + 128],
                                                                               rhs=av[:, ts], start=(f == 0), stop=(f == NF - 1)),
                       reads=[f'slab{si}', akey], writes=[bk(b)])
                op('dve', lambda e, b=b, o=o, ts=ts: e.tensor_tensor(out=xT[:, o, ts], in0=xT[:, o, ts], in1=banks[b][:, 0:TT], op=ALU.add),
                   reads=[bk(b), ('xT', j)], writes=[('xT', j)])

    TPT = TT // 128
    for s in range(c.NSEQ):
        for blk in range(c.NBLK):
            first = (blk == 0)
            t0 = blk * BLK
            for tt in range(BLK // 128):
                dma('sp', xin[:], x_d[s, t0 + tt * 128:t0 + (tt + 1) * 128, :], writes=['xin'])
                for k4 in range(0, KD, 4):
                    b = bank()
                    n4 = min(4, KD - k4)
                    for q in range(n4):
                        op('pe', lambda e, b=b, q=q, k4=k4: e.transpose(out=banks[b][:, q * 128:(q + 1) * 128], in_=xin[:, (k4 + q) * 128:(k4 + q + 1) * 128],
                                                                         identity=C('ident', 128)), reads=['xin', 'cst'], writes=[bk(b)])
                    op('act', lambda e, b=b, k4=k4, n4=n4, tt=tt: e.activation(out=xT[:, k4:k4 + n4, tt * 128:(tt + 1) * 128],
                                                                               in_=banks[b][:, 0:n4 * 128].rearrange("p (a b) -> p a b", b=128), func=AF.Copy),
                       reads=[bk(b)], writes=[('xT', tt // TPT)])
            for l in range(c.DEPTH):
                PH = getattr(c, 'phases', 'nmrwf')
                if 'n' in PH:
                    rmsnorm(l, 'ln1')
                if 'm' in PH:
                    mlstm(l, first)
                if 'r' in PH:
                    retention(l, first, blk)
                if 'w' in PH:
                    rwkv(l, first)
                if 'f' in PH:
                    wout_ffn(l)
            fo = Ft[0]
            for n in range(BLK // TN):
                t0w = n * TN
                j = t0w // TT
                rstd, rkey = rstd_tile(t0w)
                for t8 in range(TN // 128):
                    tsl = slice(t0w + t8 * 128, t0w + (t8 + 1) * 128)
                    for kk in range(KD):
                        op('dve', lambda e, kk=kk, tsl=tsl, t8=t8: e.scalar_tensor_tensor(out=fo[:, kk * 128:(kk + 1) * 128], in0=xT[:, kk, tsl], scalar=C('lnf', 1, kk),
                                                                                          in1=rstd[:, t8 * 128:(t8 + 1) * 128], op0=ALU.mult, op1=ALU.mult),
                           reads=[('xT', j), rkey, 'cst'], writes=['F0'])
                    for k4 in range(0, KD, 4):
                        b2 = bank()
                        n4 = min(4, KD - k4)
                        for q in range(n4):
                            op('pe', lambda e, b2=b2, q=q, k4=k4: e.transpose(out=banks[b2][:, q * 128:(q + 1) * 128], in_=fo[:, (k4 + q) * 128:(k4 + q + 1) * 128],
                                                                               identity=C('ident', 128)), reads=['F0', 'cst'], writes=[bk(b2)])
                        op('act', lambda e, b2=b2, k4=k4, n4=n4: e.activation(out=xin[:, k4 * 128:(k4 + n4) * 128], in_=banks[b2][:, 0:n4 * 128], func=AF.Copy),
                           reads=[bk(b2)], writes=['xin'])
                    dma('sp', out_d[s, t0 + tsl.start:t0 + tsl.stop, :], xin[:], reads=['xin'], is_output=True)
    k.emit()
    return nc


_CACHE = {}


def kernel(**inputs):
    cfg = Cfg()
    packed = pack_host(cfg, inputs)
    if 'nc' not in _CACHE:
        _CACHE['nc'] = build(cfg)
    nc = _CACHE['nc']
    x = np.asarray(inputs['x'], np.float32)
    in_maps = []
    for i in range(8):
        m = dict(packed)
        m['x'] = np.ascontiguousarray(x[i * cfg.NSEQ:(i + 1) * cfg.NSEQ])
        m['win'] = packed['win'].reshape(cfg.DEPTH, cfg.NG, 128, cfg.KD * 128)
        m['wout'] = packed['wout'].reshape(cfg.DEPTH, cfg.KD, 128, cfg.KM * 128)
        m['wgu'] = packed['wgu'].reshape(cfg.DEPTH, cfg.NF, 128, 2 * cfg.KD * 128)
        m['wdn'] = packed['wdn'].reshape(cfg.DEPTH, cfg.KD, 128, cfg.NF * 128)
        in_maps.append(m)
    res = run_bass_kernel_spmd(nc, in_maps, core_ids=list(range(8)))
    return np.concatenate([r['out'] for r in res.results], axis=0).astype(np.float32)
```
